# Optimizing a Trainium2 kernel written in Bass

```python
import math
import jax, jax.numpy as jnp
from jax import lax
import numpy as np

D_MODEL = 1024
BATCH = 8
SEQ = 4096
DEPTH = 2

CTX_LEN = 256
GRID_W = 64
HEAD_DIM = 64
ATTN_WIDTH = D_MODEL // 2
N_HEADS = ATTN_WIDTH // HEAD_DIM
N_KV_HEADS = N_HEADS // 4
WINDOW = 128
ATTN_BLOCK = 128
ROPE_BASE = 10000.0
POOL_WINDOWS = (2, 4, 8, 16)
POOL_WIDTH = D_MODEL // 4
POOL_GROUP = POOL_WIDTH // 4
RET_HEADS = 4
RET_WIDTH = D_MODEL // 4
RET_DV = RET_WIDTH // RET_HEADS
RET_DK = RET_DV // 2
RET_CHUNK = 128
MIX_WIDTH = ATTN_WIDTH + POOL_WIDTH + RET_WIDTH
IN_SPLITS = (ATTN_WIDTH, N_KV_HEADS * HEAD_DIM, N_KV_HEADS * HEAD_DIM, POOL_WIDTH,
             RET_HEADS * RET_DK, RET_HEADS * RET_DK, RET_WIDTH, RET_WIDTH)
IN_WIDTH = ATTN_WIDTH + 2 * N_KV_HEADS * HEAD_DIM + POOL_WIDTH + 2 * RET_HEADS * RET_DK + 2 * RET_WIDTH
FFN_DIM = ((8 * D_MODEL // 3 + 127) // 128) * 128
N_EXPERTS = 8
TOP_K = 2
LN_EPS = 1e-5
NEG_INF = -1e30

kernel_name = "hybrid_flow_block"


def layer_norm(x, g, b):
    xf = x.astype(jnp.float32)
    mu = jnp.mean(xf, -1, keepdims=True)
    var = jnp.mean(jnp.square(xf - mu), -1, keepdims=True)
    y = (xf - mu) * lax.rsqrt(var + LN_EPS) * g.astype(jnp.float32) + b.astype(jnp.float32)
    return y.astype(x.dtype)


def head_norm(o):
    mu = jnp.mean(o, -1, keepdims=True)
    var = jnp.mean(jnp.square(o - mu), -1, keepdims=True)
    return (o - mu) * lax.rsqrt(var + LN_EPS)


def axial_rope(x, row, col):
    half = HEAD_DIM // 2
    quarter = half // 2
    inv = ROPE_BASE ** (-jnp.arange(quarter, dtype=jnp.float32) / quarter)

    def rot(xp, pos):
        ang = pos.astype(jnp.float32)[:, None] * inv[None, :]
        cos = jnp.cos(ang)[None, :, None, :]
        sin = jnp.sin(ang)[None, :, None, :]
        x1, x2 = xp[..., :quarter], xp[..., quarter:]
        return jnp.concatenate([x1 * cos - x2 * sin, x1 * sin + x2 * cos], -1)

    xf = x.astype(jnp.float32)
    return jnp.concatenate([rot(xf[..., :half], row), rot(xf[..., half:], col)], -1).astype(x.dtype)


def sink_softmax(sink, scores):
    B, KV, G, Q, _ = scores.shape
    sk = jnp.broadcast_to(sink.astype(jnp.float32).reshape(1, KV, G, 1, 1), (B, KV, G, Q, 1))
    p = jax.nn.softmax(jnp.concatenate([sk, scores], -1), axis=-1)
    return p[..., 1:]


def band_attention(q, k, v, kc, vc, sink):
    B, L, H, hd = q.shape
    KV = k.shape[2]
    G = H // KV
    nb = L // ATTN_BLOCK
    span = 3 * ATTN_BLOCK
    scale = hd ** -0.5
    qb = jnp.moveaxis(q.reshape(B, nb, ATTN_BLOCK, KV, G, hd), 1, 0)
    pad = ((0, 0), (ATTN_BLOCK, ATTN_BLOCK), (0, 0), (0, 0))
    kp = jnp.pad(k, pad)
    vp = jnp.pad(v, pad)

    def block(args):
        i, qi = args
        start = i * ATTN_BLOCK
        ki = lax.dynamic_slice_in_dim(kp, start, span, axis=1)
        vi = lax.dynamic_slice_in_dim(vp, start, span, axis=1)
        qpos = start + jnp.arange(ATTN_BLOCK)
        kpos = start - ATTN_BLOCK + jnp.arange(span)
        ok = (kpos[None, :] >= 0) & (kpos[None, :] < L) & (jnp.abs(qpos[:, None] - kpos[None, :]) <= WINDOW)
        s_loc = jnp.einsum('bqkgd,bjkd->bkgqj', qi, ki).astype(jnp.float32) * scale
        s_loc = jnp.where(ok, s_loc, NEG_INF)
        s_ctx = jnp.einsum('bqkgd,bjkd->bkgqj', qi, kc).astype(jnp.float32) * scale
        p = sink_softmax(sink, jnp.concatenate([s_loc, s_ctx], -1)).astype(v.dtype)
        return (jnp.einsum('bkgqj,bjkd->bqkgd', p[..., :span], vi)
                + jnp.einsum('bkgqj,bjkd->bqkgd', p[..., span:], vc))

    o = lax.map(block, (jnp.arange(nb), qb))
    return jnp.moveaxis(o, 0, 1).reshape(B, L, H * hd)


def ctx_attention(qc, kc, vc, sink):
    B, Lc, H, hd = qc.shape
    KV = kc.shape[2]
    G = H // KV
    qr = qc.reshape(B, Lc, KV, G, hd)
    s = jnp.einsum('bqkgd,bjkd->bkgqj', qr, kc).astype(jnp.float32) * hd ** -0.5
    p = sink_softmax(sink, s).astype(vc.dtype)
    return jnp.einsum('bkgqj,bjkd->bqkgd', p, vc).reshape(B, Lc, H * hd)


def pool_mix(u, pool_w, pool_scale):
    B, L, _ = u.shape
    t = jnp.arange(L)
    uf = u.astype(jnp.float32)
    cs = jnp.pad(jnp.cumsum(uf, axis=1), ((0, 0), (1, 0), (0, 0)))
    outs = []
    for g, w in enumerate(POOL_WINDOWS):
        sl = slice(g * POOL_GROUP, (g + 1) * POOL_GROUP)
        ug, csg = uf[..., sl], cs[..., sl]
        lo = jnp.clip(t - w // 2, 0, L)
        hi = jnp.clip(t + w - w // 2, 0, L)
        cnt = (hi - lo).astype(jnp.float32)[None, :, None]
        mean = (jnp.take(csg, hi, axis=1) - jnp.take(csg, lo, axis=1)) / cnt
        outs.append(jnp.einsum('blc,cd->bld', (mean - ug).astype(u.dtype), pool_w[g]))
    return jnp.concatenate(outs, -1) * pool_scale


def retention_scan(q, k, v, log_g, state0):
    B, L, H, dk = q.shape
    dv = v.shape[-1]
    C = RET_CHUNK
    nc = L // C
    qc = q.astype(jnp.float32).reshape(B, nc, C, H, dk)
    kc = k.astype(jnp.float32).reshape(B, nc, C, H, dk)
    vc = v.astype(jnp.float32).reshape(B, nc, C, H, dv)
    n = jnp.arange(C, dtype=jnp.float32)
    rel = n[:, None] - n[None, :]
    dmask = jnp.where(rel >= 0, jnp.exp(log_g[:, None, None] * jnp.maximum(rel, 0.0)), 0.0)
    intra = jnp.einsum('bcnhd,bcmhd->bchnm', qc, kc) * dmask
    o_intra = jnp.einsum('bchnm,bcmhe->bcnhe', intra, vc)
    w_state = jnp.exp(log_g[None, :] * (C - 1 - n)[:, None])
    kv = jnp.einsum('bcmhd,bcmhe->cbhde', kc * w_state[:, :, None], vc)
    chunk_decay = jnp.exp(log_g * C)[None, :, None, None]

    def step(S, kv_i):
        return chunk_decay * S + kv_i, S

    s_fin, s_prev = lax.scan(step, state0, kv)
    q_dec = qc * jnp.exp(log_g[None, :] * (n + 1)[:, None])[:, :, None]
    o_cross = jnp.einsum('bcnhd,cbhde->bcnhe', q_dec, s_prev)
    return (o_intra + o_cross).reshape(B, L, H, dv), s_fin


def retention_state(k, v, log_g):
    L = k.shape[1]
    t = jnp.arange(L, dtype=jnp.float32)
    w = jnp.exp(log_g[None, :] * (L - 1 - t)[:, None])
    return jnp.einsum('blhd,blhe->bhde', k.astype(jnp.float32) * w[None, :, :, None], v.astype(jnp.float32))


def flip(t):
    return jnp.flip(t, axis=1)


def heads(t, n):
    return t.reshape(t.shape[0], t.shape[1], n, -1)


def mixer(h, hc, w_in, sink, pool_w, pool_scale, lg_f, lg_b, w_out, need_ctx):
    B, L, _ = h.shape
    ROWS = L // GRID_W
    row = jnp.repeat(jnp.arange(ROWS), GRID_W)
    col = jnp.tile(jnp.arange(GRID_W), ROWS)
    cuts = [int(s) for s in np.cumsum(IN_SPLITS)[:-1]]
    q, k, v, u, rq, rk, rv, rg = jnp.split(h @ w_in, cuts, axis=-1)
    qc, kc, vc, uc, rqc, rkc, rvc, rgc = jnp.split(hc @ w_in, cuts, axis=-1)

    q = axial_rope(heads(q, N_HEADS), row, col)
    k = axial_rope(heads(k, N_KV_HEADS), row, col)
    v = heads(v, N_KV_HEADS)
    kc, vc = heads(kc, N_KV_HEADS), heads(vc, N_KV_HEADS)
    attn = band_attention(q, k, v, kc, vc, sink)

    pool = pool_mix(u, pool_w, pool_scale)

    lg_f = lg_f.astype(jnp.float32)
    lg_b = lg_b.astype(jnp.float32)
    rscale = RET_DK ** -0.5
    rq, rk, rv = heads(rq, RET_HEADS), heads(rk, RET_HEADS) * rscale, heads(rv, RET_HEADS)
    rqc, rkc, rvc = heads(rqc, RET_HEADS), heads(rkc, RET_HEADS) * rscale, heads(rvc, RET_HEADS)
    if need_ctx:
        zero = jnp.zeros((B, RET_HEADS, RET_DK, RET_DV), jnp.float32)
        ocf, s_f = retention_scan(rqc, rkc, rvc, lg_f, zero)
        ocb, s_b = retention_scan(flip(rqc), flip(rkc), flip(rvc), lg_b, zero)
    else:
        s_f = retention_state(rkc, rvc, lg_f)
        s_b = retention_state(flip(rkc), flip(rvc), lg_b)
    olf, _ = retention_scan(rq, rk, rv, lg_f, s_f)
    olb, _ = retention_scan(flip(rq), flip(rk), flip(rv), lg_b, s_b)
    ret = jax.nn.silu(rg) * head_norm(olf + flip(olb)).reshape(B, L, RET_WIDTH).astype(rg.dtype)

    y = jnp.concatenate([attn, pool, ret], -1) @ w_out
    if not need_ctx:
        return y, None
    Lc = hc.shape[1]
    attn_c = ctx_attention(heads(qc, N_HEADS), kc, vc, sink)
    pool_c = pool_mix(uc, pool_w, pool_scale)
    ret_c = jax.nn.silu(rgc) * head_norm(ocf + flip(ocb)).reshape(B, Lc, RET_WIDTH).astype(rgc.dtype)
    yc = jnp.concatenate([attn_c, pool_c, ret_c], -1) @ w_out
    return y, yc


def swiglu(t, wg, wu, wd):
    return (jax.nn.silu(t @ wg) * (t @ wu)) @ wd


def moe_ffn(t, router, wg, wu, wd):
    shape = t.shape
    tf = t.reshape(-1, shape[-1])
    logits = (tf @ router).astype(jnp.float32)
    top_v, top_i = lax.top_k(logits, TOP_K)
    top_w = jax.nn.softmax(top_v, axis=-1)
    gates = jnp.sum(jax.nn.one_hot(top_i, N_EXPERTS, dtype=jnp.float32) * top_w[..., None], axis=1)
    gates = gates.astype(t.dtype)
    y = jnp.zeros_like(tf)
    for e in range(N_EXPERTS):
        y = y + gates[:, e:e + 1] * swiglu(tf, wg[e], wu[e], wd[e])
    return y.reshape(shape)


def channel_mixer(t, i, ffn_w_gate, ffn_w_up, ffn_w_down, moe_router, moe_w_gate, moe_w_up, moe_w_down):
    j = i // 2
    if i % 2 == 0:
        return swiglu(t, ffn_w_gate[j], ffn_w_up[j], ffn_w_down[j])
    return moe_ffn(t, moe_router[j], moe_w_gate[j], moe_w_up[j], moe_w_down[j])


def setup_inputs(seed: int = 0) -> dict:
    key = jax.random.key(seed)
    ks = jax.random.split(key, 32)
    f32 = jnp.float32

    def nrm(k, shape, s):
        return jax.random.normal(k, shape, f32) * s

    beta = (8.0 * DEPTH) ** -0.25
    nd, nm = (DEPTH + 1) // 2, DEPTH // 2
    base_decay = jnp.log(1.0 - 2.0 ** (-5.0 - jnp.arange(RET_HEADS, dtype=f32)))
    D = D_MODEL
    return {
        "x": nrm(ks[0], (BATCH, SEQ, D), 1.0),
        "c": nrm(ks[1], (BATCH, D), 1.0),
        "ctx": nrm(ks[2], (BATCH, CTX_LEN, D), 1.0),
        "c_ctx": nrm(ks[3], (D,), 1.0),
        "w_mod": nrm(ks[4], (DEPTH, D, 6 * D), D ** -0.5),
        "b_mod": nrm(ks[5], (DEPTH, 6 * D), 0.02),
        "w_in": nrm(ks[6], (DEPTH, D, IN_WIDTH), D ** -0.5),
        "attn_sink": nrm(ks[7], (DEPTH, N_HEADS), 0.5),
        "pool_w": nrm(ks[8], (DEPTH, len(POOL_WINDOWS), POOL_GROUP, POOL_GROUP), POOL_GROUP ** -0.5),
        "pool_scale": 1.0 + nrm(ks[9], (DEPTH, POOL_WIDTH), 0.1),
        "ret_log_decay_fwd": base_decay[None, :] * jnp.exp(nrm(ks[10], (DEPTH, RET_HEADS), 0.1)),
        "ret_log_decay_bwd": base_decay[None, :] * jnp.exp(nrm(ks[11], (DEPTH, RET_HEADS), 0.1)),
        "w_out": nrm(ks[12], (DEPTH, MIX_WIDTH, D), beta * MIX_WIDTH ** -0.5),
        "ln1_g": 1.0 + nrm(ks[13], (DEPTH, D), 0.02),
        "ln1_b": nrm(ks[14], (DEPTH, D), 0.02),
        "ln2_g": 1.0 + nrm(ks[15], (DEPTH, D), 0.02),
        "ln2_b": nrm(ks[16], (DEPTH, D), 0.02),
        "ffn_w_gate": nrm(ks[17], (nd, D, FFN_DIM), D ** -0.5),
        "ffn_w_up": nrm(ks[18], (nd, D, FFN_DIM), D ** -0.5),
        "ffn_w_down": nrm(ks[19], (nd, FFN_DIM, D), beta * FFN_DIM ** -0.5),
        "moe_router": nrm(ks[20], (nm, D, N_EXPERTS), D ** -0.5),
        "moe_w_gate": nrm(ks[21], (nm, N_EXPERTS, D, FFN_DIM), D ** -0.5),
        "moe_w_up": nrm(ks[22], (nm, N_EXPERTS, D, FFN_DIM), D ** -0.5),
        "moe_w_down": nrm(ks[23], (nm, N_EXPERTS, FFN_DIM, D), beta * FFN_DIM ** -0.5),
    }


def reference(x, c, ctx, c_ctx, w_mod, b_mod, w_in, attn_sink, pool_w, pool_scale,
              ret_log_decay_fwd, ret_log_decay_bwd, w_out, ln1_g, ln1_b, ln2_g, ln2_b,
              ffn_w_gate, ffn_w_up, ffn_w_down, moe_router, moe_w_gate, moe_w_up, moe_w_down):
    alpha = (2.0 * DEPTH) ** 0.25
    xc = ctx
    for i in range(DEPTH):
        last = i == DEPTH - 1
        mod = (jax.nn.silu(c) @ w_mod[i] + b_mod[i])[:, None, :]
        modc = (jax.nn.silu(c_ctx) @ w_mod[i] + b_mod[i])[None, None, :]
        sh1, sc1, g1, sh2, sc2, g2 = jnp.split(mod, 6, axis=-1)
        sh1c, sc1c, g1c, sh2c, sc2c, g2c = jnp.split(modc, 6, axis=-1)

        y, yc = mixer(x * (1 + sc1) + sh1, xc * (1 + sc1c) + sh1c, w_in[i], attn_sink[i],
                      pool_w[i], pool_scale[i], ret_log_decay_fwd[i], ret_log_decay_bwd[i],
                      w_out[i], not last)
        x = layer_norm(alpha * x + g1 * y, ln1_g[i], ln1_b[i])
        f = channel_mixer(x * (1 + sc2) + sh2, i, ffn_w_gate, ffn_w_up, ffn_w_down,
                          moe_router, moe_w_gate, moe_w_up, moe_w_down)
        x = layer_norm(alpha * x + g2 * f, ln2_g[i], ln2_b[i])
        if not last:
            xc = layer_norm(alpha * xc + g1c * yc, ln1_g[i], ln1_b[i])
            fc = channel_mixer(xc * (1 + sc2c) + sh2c, i, ffn_w_gate, ffn_w_up, ffn_w_down,
                               moe_router, moe_w_gate, moe_w_up, moe_w_down)
            xc = layer_norm(alpha * xc + g2c * fc, ln2_g[i], ln2_b[i])
    return x
```

```python
import contextlib
import math
import numpy as np
import concourse.bass as bass
import concourse.mybir as mybir
from concourse.bass_utils import run_bass_kernel_spmd

F32 = mybir.dt.float32
BF16 = mybir.dt.bfloat16
AF = mybir.ActivationFunctionType
ALU = mybir.AluOpType
AX = mybir.AxisListType

SAME_ENGINE_SYNC = "raw"
ENGS = ["pe", "act", "dve", "pool", "sp"]

D = 1024
L = 4096
LC = 256
NT = (L + LC) // 128
NCT = LC // 128
FF = 2816
NCH = FF // 128
NEXP = 8
ALPHA = 4.0 ** 0.25
EPS = 1e-5
RSCALE = 32 ** -0.5
NEG = -30000.0


_GS = {}


class Buf:
    __slots__ = ("name", "lw", "rd", "last_dma", "sem", "cnt", "doff")

    def __init__(self, name):
        self.name = name
        self.lw = None
        self.rd = []
        self.last_dma = None
        self.sem = None
        self.cnt = 0


class Op:
    __slots__ = ("eng", "fn", "deps", "signal", "val", "is_dma", "idx", "semkey", "raw")


class Sched:
    def __init__(self, nc, name="p"):
        self.nc = nc
        self.name = name
        self.ops = {e: [] for e in ENGS}
        self.dma_keys = []
        self.bufs = set()

    def op(self, eng, fn, reads=(), writes=(), dma_key=None):
        o = Op()
        o.eng = eng
        o.fn = fn
        o.is_dma = dma_key is not None
        o.signal = False
        o.val = None
        o.semkey = dma_key
        deps = []
        raw = set()
        for b in reads:
            if b.lw is not None:
                deps.append(b.lw)
                raw.add(id(b.lw))
        for b in writes:
            if b.lw is not None:
                deps.append(b.lw)
            deps.extend(b.rd)
        o.raw = raw
        if o.is_dma:
            if dma_key.last_dma is not None:
                deps.append(dma_key.last_dma)
            dma_key.last_dma = o
            if dma_key.sem is None:
                dma_key.sem = True
                self.dma_keys.append(dma_key)
        o.deps = [d for d in deps if d is not o]
        for b in reads:
            self.bufs.add(b)
        for b in writes:
            self.bufs.add(b)
        for b in reads:
            if not o.is_dma:
                b.rd = [r for r in b.rd if r.is_dma or r.eng != eng]
            b.rd.append(o)
        for b in writes:
            b.lw = o
            b.rd = []
        o.idx = len(self.ops[eng])
        self.ops[eng].append(o)
        return o

    def emit(self):
        nc = self.nc
        import os as _os2
        _only = _os2.environ.get("K_ONLY")
        if _only and not self.name.endswith(_only):
            for k in self.dma_keys:
                k.sem = None; k.cnt = 0; k.last_dma = None
            for b in self.bufs:
                b.lw = None; b.rd = []
            return
        last = {}
        for e in ENGS:
            for o in reversed(self.ops[e]):
                if o.fn is not None and not o.is_dma:
                    o.signal = True
                    last[e] = o
                    break
        for e in ENGS:
            for o in self.ops[e]:
                nd = []
                for d in o.deps:
                    if d.is_dma:
                        nd.append(d)
                        continue
                    if d.eng == o.eng and not o.is_dma:
                        if d.eng == "pe" or not SAME_ENGINE_SYNC:
                            continue
                        if SAME_ENGINE_SYNC == "raw" and id(d) not in o.raw:
                            continue
                    d.signal = True
                    nd.append(d)
                o.deps = nd
        for e in ENGS:
            c = 0
            for o in self.ops[e]:
                if o.is_dma:
                    o.semkey.cnt += 16
                    o.val = o.semkey.cnt
                elif o.signal:
                    c += 1
                    o.val = c
        import os as _os
        if _os.environ.get("K_DBG"):
            print(self.name, {e: (len(self.ops[e]), max([o.val or 0 for o in self.ops[e] if not o.is_dma] + [0])) for e in ENGS}, "dma", len(self.dma_keys), max([k.cnt for k in self.dma_keys] + [0]), flush=True)
        gs = _GS
        if gs.get("nc") is not nc:
            gs.clear()
            gs["nc"] = nc
            gs["esem"] = {e: nc.semaphore("g_s_" + e).__enter__() for e in ENGS}
            gs["eoff"] = {e: 0 for e in ENGS}
            gs["dsem"] = []
            gs["doff"] = []
        kind = {}
        for e in ENGS:
            for o in self.ops[e]:
                if o.is_dma and id(o.semkey) not in kind:
                    kind[id(o.semkey)] = 1 if e == "pool" else 0
        self.dma_keys.sort(key=lambda k: kind.get(id(k), 0))
        npool = sum(1 for k in self.dma_keys if kind.get(id(k), 0) == 1)
        nsp = len(self.dma_keys) - npool
        for nm, need in (("dsem_sp", nsp), ("dsem_pl", npool)):
            gs.setdefault(nm, [])
            gs.setdefault(nm + "_off", [])
            while len(gs[nm]) < need:
                gs[nm].append(nc.semaphore("g_%s%d" % (nm, len(gs[nm]))).__enter__())
                gs[nm + "_off"].append(0)
        gs["dsem"] = gs["dsem_sp"][:nsp] + gs["dsem_pl"][:npool]
        gs["doff"] = gs["dsem_sp_off"][:nsp] + gs["dsem_pl_off"][:npool]
        eoff = gs["eoff"]
        for e in ENGS:
            for o in self.ops[e]:
                if o.is_dma:
                    continue
                if o.val is not None:
                    o.val += eoff[e]
        for i, k in enumerate(self.dma_keys):
            k.doff = gs["doff"][i]
        for e in ENGS:
            for o in self.ops[e]:
                if o.is_dma:
                    o.val += o.semkey.doff
        for k in self.dma_keys:
            k.cnt += k.doff
        with contextlib.ExitStack() as st:
            esem = gs["esem"]
            for i, k in enumerate(self.dma_keys):
                k.sem = gs["dsem"][i]
            block = st.enter_context(nc.Block())

            def run(e, engobj):
                known = {}
                for o in self.ops[e]:
                    for d in o.deps:
                        sem = d.semkey.sem if d.is_dma else esem[d.eng]
                        key = id(sem)
                        if known.get(key, 0) < d.val:
                            engobj.wait_ge(sem, d.val)
                            known[key] = d.val
                    if o.fn is None:
                        continue
                    ins = o.fn(engobj)
                    if o.is_dma:
                        ins.then_inc(o.semkey.sem, 16)
                    elif o.signal:
                        ins.then_inc(esem[e], 1)
                for f in ENGS:
                    if f in last and known.get(id(esem[f]), 0) < last[f].val:
                        engobj.wait_ge(esem[f], last[f].val)
                for k in self.dma_keys:
                    if known.get(id(k.sem), 0) < k.cnt:
                        engobj.wait_ge(k.sem, k.cnt)

            @block.tensor
            def _(t):
                run("pe", t)

            @block.scalar
            def _(t):
                run("act", t)

            @block.vector
            def _(t):
                run("dve", t)

            @block.gpsimd
            def _(t):
                run("pool", t)

            @block.sync
            def _(t):
                run("sp", t)
        for e in ENGS:
            if e in last:
                gs["eoff"][e] = last[e].val
        for i, k in enumerate(self.dma_keys):
            if i < nsp:
                gs["dsem_sp_off"][i] = k.cnt
            else:
                gs["dsem_pl_off"][i - nsp] = k.cnt
        for k in self.dma_keys:
            k.sem = None
            k.cnt = 0
            k.last_dma = None
        for b in self.bufs:
            b.lw = None
            b.rd = []


class T:
    def __init__(self, h, name):
        self.h = h
        self.b = Buf(name)


C_MPREV, C_MNEXT = 0, 512
C_POOLA = 1024
C_R1 = C_POOLA + 20 * 128
C_M1 = C_R1 + 128
C_R2 = C_M1 + 128
C_M2 = C_R2 + 128
C_NP1 = C_M2 + 128
C_NB = C_NP1 + 128
C_MASKS = C_NB + 128
C_MASKQ = C_MASKS + 256
C_IDENT = C_MASKQ + 512
C_COLS = C_IDENT + 128
C_TOT = C_COLS + 8
POOL_WINDOWS = (2, 4, 8, 16)


def make_consts():
    c = np.zeros((128, C_TOT), np.float32)
    kl = np.arange(128)[:, None]
    ql = np.arange(128)[None, :]
    mprev = np.where(ql <= kl, 0.0, NEG).astype(np.float32)
    mnext = np.where(kl <= ql, 0.0, NEG).astype(np.float32)
    c[:, C_MPREV:C_MPREV + 512] = np.tile(mprev, (1, 4))
    c[:, C_MNEXT:C_MNEXT + 512] = np.tile(mnext, (1, 4))
    Ls = 384
    for g, w in enumerate(POOL_WINDOWS):
        t = np.arange(Ls)
        lo = np.clip(t - w // 2, 0, Ls)
        hi = np.clip(t + w - w // 2, 0, Ls)
        A = np.zeros((Ls, Ls), np.float64)
        for i in range(Ls):
            A[i, lo[i]:hi[i]] = 1.0 / (hi[i] - lo[i])
        A -= np.eye(Ls)
        blk = lambda ti, si: A[ti * 128:(ti + 1) * 128, si * 128:(si + 1) * 128].T
        mats = [blk(0, 0), blk(1, 1), blk(2, 2), blk(1, 0), blk(1, 2)]
        for v, m in enumerate(mats):
            o = C_POOLA + (g * 5 + v) * 128
            c[:, o:o + 128] = m
    m = np.arange(128)[:, None].astype(np.float64)
    n = np.arange(128)[None, :].astype(np.float64)
    c[:, C_R1:C_R1 + 128] = np.maximum(n - m, 0)
    c[:, C_M1:C_M1 + 128] = (n >= m) * RSCALE
    c[:, C_R2:C_R2 + 128] = np.maximum(m - n, 0)
    c[:, C_M2:C_M2 + 128] = (m >= n) * RSCALE
    c[:, C_NP1:C_NP1 + 128] = np.broadcast_to(n + 1, (128, 128))
    c[:, C_NB:C_NB + 128] = np.broadcast_to(128 - n, (128, 128))
    p = np.arange(128)[:, None]
    c[:, C_MASKS:C_MASKS + 256] = (p // 32 == np.arange(256)[None, :] // 64)
    c[:, C_MASKQ:C_MASKQ + 512] = (p // 32 == np.arange(512)[None, :] // 128)
    c[:, C_IDENT:C_IDENT + 128] = np.eye(128)
    c[:, C_COLS + 0] = 127 - np.arange(128)
    c[:, C_COLS + 1] = np.arange(128)
    c[:, C_COLS + 2] = EPS
    c[:, C_COLS + 3] = math.log(RSCALE)
    c[:, C_COLS + 4] = EPS / (ALPHA * ALPHA)
    return c


NSLOT = 24 * 512
RC_TOK, RC_BG, RC_BD, RC_U, RC_ONES = 0, 32, 64, 86, 214
RC_TOT = 342


def make_rconst():
    c = np.zeros((128, RC_TOT), np.float32)
    p = np.arange(128)[:, None]
    c[:, RC_TOK:RC_TOK + 32] = p + 128 * np.arange(32)[None, :] + NCT * 128
    c[:, RC_BG:RC_BG + 32] = p + 128 * (np.arange(32)[None, :] % 4)
    c[:, RC_BD:RC_BD + 22] = p + 128 * np.arange(22)[None, :]
    c[:, RC_U:RC_U + 128] = (p < np.arange(128)[None, :])
    c[:, RC_ONES:RC_ONES + 128] = 1.0
    return c


def make_slot_init():
    a = np.zeros((NSLOT, 4), np.float32)
    a[:, 2] = 2 * L + (np.arange(NSLOT) % 128)
    return a


def make_rope():
    pos = np.arange(L)
    row = (pos // 64).astype(np.float64)
    col = (pos % 64).astype(np.float64)
    inv = 10000.0 ** (-np.arange(16, dtype=np.float64) / 16)
    ar = row[:, None] * inv[None, :]
    ac = col[:, None] * inv[None, :]
    C = np.concatenate([np.cos(ar), np.cos(ar), np.cos(ac), np.cos(ac)], 1)
    S = np.concatenate([-np.sin(ar), np.sin(ar), -np.sin(ac), np.sin(ac)], 1)
    return np.concatenate([C, S], 1).astype(np.float32)


def win_perm():
    q = []
    for j in range(4):
        q += list(range(j * 64, (j + 1) * 64)) + list(range((4 + j) * 64, (5 + j) * 64))
    k = list(range(512, 640)); v = list(range(640, 768)); u = list(range(768, 1024))
    rq = list(range(1024, 1152)); rk = list(range(1152, 1280)); rv = list(range(1280, 1536)); rg = list(range(1536, 1792))
    return np.array(q + k + rq + rk + v + u + rv + rg)


def build(n_layers=2, moe_experts=NEXP, dbg=False, stop=None):
    nc = bass.Bass("TRN2", target_bir_lowering=False)

    def din(name, shape, dt=F32):
        return nc.dram_tensor(name, list(shape), dt, kind="ExternalInput").ap()

    xall = din("xall", [NT * 128, D])
    cvec = din("cvec", [128, 16])
    w_mod = din("w_mod", [2, D, 6 * D])
    b_modT = din("b_modT", [2, 128, 48])
    b_modr = din("b_modr", [2, 1, 6 * D])
    w_in = din("w_in", [2, D, 1792])
    sink = din("sink", [2, 1, 8])
    pool_w = din("pool_w", [2, 4, 64, 64])
    pool_scale = din("pool_scale", [2, 1, 256])
    lgp = din("lgp", [2, 128, 2])
    lgrow = din("lgrow", [2, 1, 256])
    lgb = din("lgb", [2, 1, 8])
    w_out = din("w_out", [2, D, D])
    lnp = din("lnp", [2, 4, D])
    ffn_wg = din("ffn_wg", [1, D, FF])
    ffn_wu = din("ffn_wu", [1, D, FF])
    ffn_wd = din("ffn_wd", [1, FF, D])
    if n_layers > 1:
        router = din("router", [D, NEXP])
        moe_wg = din("moe_wg", [NEXP * 4 * 128, 8 * 768])
        moe_wu = din("moe_wu", [NEXP * 4 * 128, 8 * 768])
        moe_wd = din("moe_wd", [NEXP * 4 * 128, 6 * D])
    import os as _os
    SKIP = set(_os.environ.get("K_SKIP", "").split(","))
    SPARSE = n_layers > 1 and _os.environ.get("K_DENSE", "") == ""
    if n_layers > 1:
        rconst = din("rconst", [128, RC_TOT])
        slot_init = din("slot_init", [NSLOT, 4])
        slotinfo = nc.dram_tensor("slotinfo", [NSLOT, 4], F32).ap()
        Fsc = nc.dram_tensor("Fsc", [2 * L + 128, D], F32).ap()
        wg2d = nc.dram_tensor("wg16", [NEXP * 4 * 128, 8 * 768], BF16).ap()
        wu2d = nc.dram_tensor("wu16", [NEXP * 4 * 128, 8 * 768], BF16).ap()
        wd2d = nc.dram_tensor("wd16", [NEXP * 4 * 128, 6 * D], BF16).ap()
    consts = din("consts", [128, C_TOT])
    ropecs = din("ropecs", [L, 128])
    out = nc.dram_tensor("out", [L, D], F32, kind="ExternalOutput").ap()
    kind = "ExternalOutput" if dbg else "Internal"
    xs1 = nc.dram_tensor("xs1", [NT * 128, D], F32, kind=kind).ap()
    xs2 = nc.dram_tensor("xs2", [NT * 128, D], F32, kind=kind).ap()
    qbun = nc.dram_tensor("qbun", [NT, 128, 1024], BF16).ap()
    gscr = nc.dram_tensor("gscr", [2, 2048], F32).ap()
    if dbg:
        dbg_modT = nc.dram_tensor("dbg_modT", [128, 96], F32, kind="ExternalOutput").ap()

    xs1_b = [Buf("xs1_%d" % t) for t in range(NT)]
    xs2_b = [Buf("xs2_%d" % t) for t in range(NT)]
    qbun_b = [Buf("qbun_%d" % t) for t in range(NT)]
    out_b = [Buf("out_%d" % t) for t in range(NT)]
    gscr_b = Buf("gscr")

    G = contextlib.ExitStack()
    with G:
        cnt = [0]

        def sb(st, name, shape, dt):
            cnt[0] += 1
            return T(st.enter_context(nc.sbuf_tensor("%s_%d" % (name, cnt[0]), list(shape), dt)), name)

        def ps(st, name, shape, dt):
            cnt[0] += 1
            return T(st.enter_context(nc.psum_tensor("%s_%d" % (name, cnt[0]), list(shape), dt)), name)

        cb = sb(G, "cb", [128, C_R1], BF16)
        cf = sb(G, "cf", [128, C_MASKS - C_R1], F32)
        cm = sb(G, "cm", [128, 768 + 128], BF16)
        cc = sb(G, "cc", [128, 8], F32)
        ones_t = sb(G, "ones", [128, 8], F32)
        ident = cm.h[:, 768:896]
        maskS = cm.h[:, 0:256]
        maskQ = cm.h[:, 256:768]
        mprev = cb.h[:, C_MPREV:C_MPREV + 512]
        mnext = cb.h[:, C_MNEXT:C_MNEXT + 512]

        def PA(g, v):
            o = C_POOLA + (g * 5 + v) * 128
            return cb.h[:, o:o + 128]

        cfo = lambda c0: cf.h[:, c0 - C_R1:c0 - C_R1 + 128]
        KT_b = [Buf("KT%d" % t) for t in range(NT)]
        VX_b = [Buf("VX%d" % t) for t in range(NT)]
        UR_b = [Buf("UR%d" % t) for t in range(NT)]
        RKT_b = [Buf("RKT%d" % t) for t in range(NT)]
        SBD_b = [Buf("SBD%d" % t) for t in range(NT)]
        modT = sb(G, "modT", [128, 48, 2], F32)
        Gt = [sb(G, "G%d" % i, [128, D], F32) for i in range(4)]
        LNt = [sb(G, "LN%d" % i, [128, D], F32) for i in range(4)]
        DsumT = sb(G, "DsumT", [128, 512], F32)
        DFB = sb(G, "DFB", [128, 256], F32)
        WFB = sb(G, "WFB", [128, 256], F32)
        gC = sb(G, "gC", [128, 2], F32)
        esink = sb(G, "esink", [128, 8], F32)
        PW = sb(G, "PW", [64, 256], BF16)

        S = Sched(nc, "c0")
        S.op("pool", lambda e: e.dma_start(out=cb.h[:], in_=consts[:, 0:C_R1]), writes=[cb.b], dma_key=cb.b)
        S.op("pool", lambda e: e.dma_start(out=cm.h[:], in_=consts[:, C_MASKS:C_COLS]), writes=[cm.b], dma_key=cm.b)
        S.op("sp", lambda e: e.dma_start(out=cf.h[:], in_=consts[:, C_R1:C_MASKS]), writes=[cf.b], dma_key=cf.b)
        S.op("sp", lambda e: e.dma_start(out=cc.h[:], in_=consts[:, C_COLS:C_TOT]), writes=[cc.b], dma_key=cc.b)
        S.op("pool", lambda e: e.memset(ones_t.h[:], 1.0), writes=[ones_t.b])
        S.emit()
        c127m = cc.h[:, 0:1]
        cmcol = cc.h[:, 1:2]
        epsc = cc.h[:, 2:3]
        lnrs = cc.h[:, 3:4]
        epsln = cc.h[:, 4:5]

        for layer in range(n_layers):
            last = layer == n_layers - 1
            xsrc, xsrc_b = (xall, None) if layer == 0 else (xs2, xs2_b)
            xdst, xdst_b = (xs2, xs2_b) if not last else (None, None)
            p2_tiles = list(range(NT)) if not last else list(range(NCT, NT))

            def xsrc_bufs(t):
                return [xsrc_b[t]] if xsrc_b is not None else []

            with contextlib.ExitStack() as P:
                S = Sched(nc, "L%dp0" % layer)
                cv = sb(P, "cv", [128, 16], F32)
                scv = sb(P, "scv", [128, 8, 2], BF16)
                wmb = [sb(P, "wmb%d" % i, [128, 8, 512], BF16) for i in range(2)]
                bmT = sb(P, "bmT", [128, 48], F32)
                brow = sb(P, "brow", [2, 2048], F32)
                grow = sb(P, "grow", [2, 2048], F32)
                lgbT = sb(P, "lgbT", [128, 8], F32)
                lgpT = sb(P, "lgpT", [128, 2], F32)
                lgrT = sb(P, "lgrT", [128, 256], F32)
                sinkb = sb(P, "sinkb", [128, 8], F32)
                pwf = sb(P, "pwf", [64, 4, 64], F32)
                pscb = sb(P, "pscb", [64, 256], F32)
                tmpA = sb(P, "tmpA", [128, 128], F32)
                tmpB = sb(P, "tmpB", [128, 128], F32)
                psM = ps(P, "psM", [128, 96], F32)
                psG = ps(P, "psG", [2, 2048], F32)
                S.op("sp", lambda e: e.dma_start(out=cv.h[:], in_=cvec), writes=[cv.b], dma_key=cv.b)
                S.op("sp", lambda e: e.dma_start(out=bmT.h[:], in_=b_modT[layer]), writes=[bmT.b], dma_key=bmT.b)
                for r in range(2):
                    S.op("sp", lambda e, r=r: e.dma_start(out=brow.h[r:r + 1, 0:1024], in_=b_modr[layer, :, 2048:3072]), writes=[brow.b], dma_key=brow.b)
                    S.op("sp", lambda e, r=r: e.dma_start(out=brow.h[r:r + 1, 1024:2048], in_=b_modr[layer, :, 5120:6144]), writes=[brow.b], dma_key=brow.b)
                S.op("sp", lambda e: e.dma_start(out=lgbT.h[:], in_=lgb[layer].partition_broadcast(128)), writes=[lgbT.b], dma_key=lgbT.b)
                S.op("sp", lambda e: e.dma_start(out=lgpT.h[:], in_=lgp[layer]), writes=[lgpT.b], dma_key=lgpT.b)
                S.op("sp", lambda e: e.dma_start(out=lgrT.h[:], in_=lgrow[layer].partition_broadcast(128)), writes=[lgrT.b], dma_key=lgrT.b)
                S.op("sp", lambda e: e.dma_start(out=sinkb.h[:], in_=sink[layer].partition_broadcast(128)), writes=[sinkb.b], dma_key=sinkb.b)
                S.op("sp", lambda e: e.dma_start(out=pwf.h[:], in_=pool_w[layer].rearrange("g c d -> c g d")), writes=[pwf.b], dma_key=pwf.b)
                S.op("sp", lambda e: e.dma_start(out=pscb.h[:], in_=pool_scale[layer].partition_broadcast(64)), writes=[pscb.b], dma_key=pscb.b)
                for i in range(4):
                    S.op("sp", lambda e, i=i: e.dma_start(out=LNt[i].h[:], in_=lnp[layer, i:i + 1, :].partition_broadcast(128)), writes=[LNt[i].b], dma_key=LNt[i].b)
                S.op("act", lambda e: e.activation(out=scv.h[:].rearrange("p k r -> p r k"), in_=cv.h[:].rearrange("p (r k) -> p r k", r=2), func=AF.Silu), reads=[cv.b], writes=[scv.b])
                wsrc = w_mod[layer].rearrange("(k p) n -> p k n", p=128)
                for blk in range(12):
                    wb_ = wmb[blk % 2]
                    S.op("pool", lambda e, blk=blk, wb_=wb_: e.dma_start(out=wb_.h[:], in_=wsrc[:, :, blk * 512:(blk + 1) * 512]), writes=[wb_.b], dma_key=wb_.b)
                    for fc in range(4):
                        j = blk * 4 + fc
                        for kc in range(8):
                            S.op("pe", lambda e, j=j, fc=fc, kc=kc, wb_=wb_: e.matmul(psM.h[:, 2 * j:2 * j + 2], lhsT=wb_.h[:, kc, fc * 128:(fc + 1) * 128], rhs=scv.h[:, kc, :], start=(kc == 0), stop=(kc == 7)),
                                 reads=[wb_.b, scv.b], writes=[psM.b])
                    if blk in (4, 5, 10, 11):
                        co = {4: 0, 5: 512, 10: 1024, 11: 1536}[blk]
                        for kc in range(8):
                            S.op("pe", lambda e, co=co, kc=kc, wb_=wb_: e.matmul(psG.h[:, co:co + 512], lhsT=scv.h[:, kc, :], rhs=wb_.h[:, kc, :], start=(kc == 0), stop=(kc == 7)),
                                 reads=[wb_.b, scv.b], writes=[psG.b])
                S.op("dve", lambda e: e.tensor_tensor(out=modT.h[:], in0=psM.h[:].rearrange("p (j r) -> p j r", r=2), in1=bmT.h[:].unsqueeze(2).to_broadcast([128, 48, 2]), op=ALU.add),
                     reads=[psM.b, bmT.b], writes=[modT.b])
                S.op("dve", lambda e: e.tensor_scalar_add(out=modT.h[:, 8:16, :], in0=modT.h[:, 8:16, :], scalar1=1.0), reads=[modT.b], writes=[modT.b])
                S.op("dve", lambda e: e.tensor_scalar_add(out=modT.h[:, 32:40, :], in0=modT.h[:, 32:40, :], scalar1=1.0), reads=[modT.b], writes=[modT.b])
                S.op("dve", lambda e: e.tensor_tensor(out=grow.h[:], in0=psG.h[:], in1=brow.h[:], op=ALU.add), reads=[psG.b, brow.b], writes=[grow.b])
                S.op("sp", lambda e: e.dma_start(out=gscr, in_=grow.h[:]), reads=[grow.b], writes=[gscr_b], dma_key=grow.b)
                for i in range(4):
                    r, w = i // 2, i % 2
                    S.op("sp", lambda e, i=i, r=r, w=w: e.dma_start(out=Gt[i].h[:], in_=gscr[r:r + 1, w * 1024:(w + 1) * 1024].partition_broadcast(128)), reads=[gscr_b], writes=[Gt[i].b], dma_key=Gt[i].b)
                    S.op("dve", lambda e, i=i: e.tensor_scalar_mul(out=Gt[i].h[:], in0=Gt[i].h[:], scalar1=1.0 / ALPHA), reads=[Gt[i].b], writes=[Gt[i].b])
                if dbg and layer == 0:
                    S.op("sp", lambda e: e.dma_start(out=dbg_modT, in_=modT.h[:].rearrange("p j r -> p (j r)")), reads=[modT.b], writes=[Buf("dbgm")], dma_key=modT.b)
                for h in range(4):
                    S.op("act", lambda e, h=h: e.activation(out=tmpA.h[:], in_=cfo(C_R1), func=AF.Exp, scale=lgbT.h[:, h:h + 1]), reads=[cf.b, lgbT.b], writes=[tmpA.b])
                    S.op("act", lambda e, h=h: e.activation(out=tmpB.h[:], in_=cfo(C_R2), func=AF.Exp, scale=lgbT.h[:, 4 + h:5 + h]), reads=[cf.b, lgbT.b], writes=[tmpB.b])
                    S.op("dve", lambda e: e.tensor_tensor(out=tmpA.h[:], in0=tmpA.h[:], in1=cfo(C_M1), op=ALU.mult), reads=[tmpA.b, cf.b], writes=[tmpA.b])
                    S.op("dve", lambda e: e.tensor_tensor(out=tmpB.h[:], in0=tmpB.h[:], in1=cfo(C_M2), op=ALU.mult), reads=[tmpB.b, cf.b], writes=[tmpB.b])
                    S.op("dve", lambda e, h=h: e.tensor_tensor(out=DsumT.h[:, h * 128:(h + 1) * 128], in0=tmpA.h[:], in1=tmpB.h[:], op=ALU.add), reads=[tmpA.b, tmpB.b], writes=[DsumT.b])
                S.op("act", lambda e: e.activation(out=DFB.h[:, 0:128], in_=cfo(C_NP1), func=AF.Exp, scale=lgpT.h[:, 0:1]), reads=[cf.b, lgpT.b], writes=[DFB.b])
                S.op("act", lambda e: e.activation(out=DFB.h[:, 128:256], in_=cfo(C_NB), func=AF.Exp, scale=lgpT.h[:, 1:2]), reads=[cf.b, lgpT.b], writes=[DFB.b])
                S.op("act", lambda e: e.activation(out=WFB.h[:, 0:128], in_=lgrT.h[:, 0:128], func=AF.Exp, scale=c127m, bias=lnrs), reads=[lgrT.b, cc.b], writes=[WFB.b])
                S.op("act", lambda e: e.activation(out=WFB.h[:, 128:256], in_=lgrT.h[:, 128:256], func=AF.Exp, scale=cmcol, bias=lnrs), reads=[lgrT.b, cc.b], writes=[WFB.b])
                S.op("act", lambda e: e.activation(out=gC.h[:], in_=lgpT.h[:], func=AF.Exp, scale=128.0), reads=[lgpT.b], writes=[gC.b])
                S.op("act", lambda e: e.activation(out=esink.h[:], in_=sinkb.h[:], func=AF.Exp), reads=[sinkb.b], writes=[esink.b])
                S.op("dve", lambda e: e.tensor_tensor(out=PW.h[:].rearrange("p (g d) -> p g d", g=4), in0=pwf.h[:], in1=pscb.h[:].rearrange("p (g d) -> p g d", g=4), op=ALU.mult), reads=[pwf.b, pscb.b], writes=[PW.b])
                S.emit()

            A1 = lambda kc, r: modT.h[:, 8 + kc, r:r + 1]
            SH1 = lambda kc, r: modT.h[:, 0 + kc, r:r + 1]
            A2 = lambda kc, r: modT.h[:, 32 + kc, r:r + 1]
            SH2 = lambda kc, r: modT.h[:, 24 + kc, r:r + 1]

            if stop == "p0":
                break
            M = contextlib.ExitStack()
            M.__enter__()
            KT = sb(M, "KT", [128, NT, 128], BF16)
            VX = sb(M, "VX", [128, NT, 2, 65], BF16)
            UR = sb(M, "UR", [128, NT, 512], BF16)
            RKT = sb(M, "RKT", [128, NT, 128], BF16)
            SBD = sb(M, "SBD", [128, NT, 256], BF16)
            with contextlib.ExitStack() as P:
                S = Sched(nc, "L%dp1" % layer)
                S.op("pool", lambda e: e.memset(VX.h[:], 1.0), writes=VX_b)
                win = sb(P, "win", [128, 8, 1792], BF16)
                xb = [sb(P, "xb%d" % i, [128, D], BF16) for i in range(2)]
                hT = [sb(P, "hT%d" % i, [128, 8, 128], BF16) for i in range(2)]
                rcs = [sb(P, "rcs%d" % i, [128, 128], F32) for i in range(2)]
                tmp1 = [sb(P, "tmp1_%d" % i, [128, 640], F32) for i in range(2)]
                tmp2 = [sb(P, "tmp2_%d" % i, [128, 640], F32) for i in range(2)]
                trin = [sb(P, "trin%d" % i, [128, 896], BF16) for i in range(2)]
                qst = [sb(P, "qst%d" % i, [128, 1024], BF16) for i in range(2)]
                psX = [ps(P, "psX%d" % i, [128, 8, 128], BF16) for i in range(2)]
                psP = [ps(P, "psP%d" % i, [128, 512], F32) for i in range(4)]
                psT2 = ps(P, "psT2", [128, 7, 128], BF16)
                psPall = None
                S.op("pool", lambda e: e.dma_start(out=win.h[:], in_=w_in[layer].rearrange("(k p) n -> p k n", p=128)), writes=[win.b], dma_key=win.b)
                for t in range(NT):
                    s = t % 2
                    r = 1 if t < NCT else 0
                    lat = t >= NCT
                    S.op("pool", lambda e, t=t, s=s: e.dma_start(out=xb[s].h[:], in_=xsrc[t * 128:(t + 1) * 128, :]), reads=xsrc_bufs(t), writes=[xb[s].b], dma_key=xb[s].b)
                    if lat:
                        S.op("sp", lambda e, t=t, s=s: e.dma_start(out=rcs[s].h[:], in_=ropecs[(t - NCT) * 128:(t - NCT + 1) * 128, :]), writes=[rcs[s].b], dma_key=rcs[s].b)
                    for kc in range(8):
                        S.op("pe", lambda e, kc=kc, s=s: e.transpose(out=psX[s].h[:, kc, :], in_=xb[s].h[:, kc * 128:(kc + 1) * 128], identity=ident), reads=[xb[s].b, cm.b], writes=[psX[s].b])
                    for kc in range(8):
                        if kc % 2 == 0:
                            S.op("act", lambda e, kc=kc, s=s, r=r: e.activation(out=hT[s].h[:, kc, :], in_=psX[s].h[:, kc, :], func=AF.Identity, bias=SH1(kc, r), scale=A1(kc, r)), reads=[psX[s].b, modT.b], writes=[hT[s].b])
                        else:
                            S.op("dve", lambda e, kc=kc, s=s, r=r: e.tensor_scalar(out=hT[s].h[:, kc, :], in0=psX[s].h[:, kc, :], scalar1=A1(kc, r), scalar2=SH1(kc, r), op0=ALU.mult, op1=ALU.add), reads=[psX[s].b, modT.b], writes=[hT[s].b])
                    for nb in range(4):
                        ncol = 512 if nb < 3 else 256
                        for kc in range(8):
                            S.op("pe", lambda e, nb=nb, kc=kc, s=s, ncol=ncol: e.matmul(psP[nb].h[:, 0:ncol], lhsT=hT[s].h[:, kc, :], rhs=win.h[:, kc, nb * 512:nb * 512 + ncol], start=(kc == 0), stop=(kc == 7)),
                                 reads=[hT[s].b, win.b], writes=[psP[nb].b])
                    if lat:
                        Cq = rcs[s].h[:, 0:64].unsqueeze(1).to_broadcast([128, 8, 64])
                        Ck = rcs[s].h[:, 0:64].unsqueeze(1).to_broadcast([128, 2, 64])
                        Sv = rcs[s].h[:, 64:128].rearrange("p (a b d) -> p a b d", a=2, b=2)
                        S.op("dve", lambda e, s=s, Cq=Cq: e.tensor_tensor(out=tmp1[s].h[:, 0:512].rearrange("p (h f) -> p h f", h=8), in0=psP[0].h[:].rearrange("p (h f) -> p h f", h=8), in1=Cq, op=ALU.mult), reads=[psP[0].b, rcs[s].b], writes=[tmp1[s].b])
                        S.op("dve", lambda e, s=s, Ck=Ck: e.tensor_tensor(out=tmp1[s].h[:, 512:640].rearrange("p (h f) -> p h f", h=2), in0=psP[1].h[:, 0:128].rearrange("p (h f) -> p h f", h=2), in1=Ck, op=ALU.mult), reads=[psP[1].b, rcs[s].b], writes=[tmp1[s].b])
                        for b_ in range(2):
                            S.op("dve", lambda e, s=s, b_=b_, Sv=Sv: e.tensor_tensor(
                                out=tmp2[s].h[:, 0:512].rearrange("p (h a b d) -> p h a b d", h=8, a=2, b=2)[:, :, :, b_, :],
                                in0=psP[0].h[:].rearrange("p (h a b d) -> p h a b d", h=8, a=2, b=2)[:, :, :, 1 - b_, :],
                                in1=Sv[:, :, b_, :].unsqueeze(1).to_broadcast([128, 8, 2, 16]), op=ALU.mult), reads=[psP[0].b, rcs[s].b], writes=[tmp2[s].b])
                            S.op("dve", lambda e, s=s, b_=b_, Sv=Sv: e.tensor_tensor(
                                out=tmp2[s].h[:, 512:640].rearrange("p (h a b d) -> p h a b d", h=2, a=2, b=2)[:, :, :, b_, :],
                                in0=psP[1].h[:, 0:128].rearrange("p (h a b d) -> p h a b d", h=2, a=2, b=2)[:, :, :, 1 - b_, :],
                                in1=Sv[:, :, b_, :].unsqueeze(1).to_broadcast([128, 2, 2, 16]), op=ALU.mult), reads=[psP[1].b, rcs[s].b], writes=[tmp2[s].b])
                        S.op("pool", lambda e, s=s: e.tensor_tensor(out=trin[s].h[:, 0:640], in0=tmp1[s].h[:], in1=tmp2[s].h[:], op=ALU.add), reads=[tmp1[s].b, tmp2[s].b], writes=[trin[s].b])
                    else:
                        S.op("act", lambda e, s=s: e.copy(out=trin[s].h[:, 0:512], in_=psP[0].h[:]), reads=[psP[0].b], writes=[trin[s].b])
                        S.op("act", lambda e, s=s: e.copy(out=trin[s].h[:, 512:640], in_=psP[1].h[:, 0:128]), reads=[psP[1].b], writes=[trin[s].b])
                    S.op("act", lambda e, s=s: e.copy(out=trin[s].h[:, 640:896], in_=psP[1].h[:, 128:384]), reads=[psP[1].b], writes=[trin[s].b])
                    S.op("act", lambda e, t=t: e.copy(out=VX.h[:, t, :, 0:64], in_=psP[1].h[:, 384:512].rearrange("p (k d) -> p k d", k=2)), reads=[psP[1].b], writes=[VX_b[t]])
                    S.op("act", lambda e, t=t: e.copy(out=RKT.h[:, t, :], in_=psP[1].h[:, 256:384]), reads=[psP[1].b], writes=[RKT_b[t]])
                    S.op("dve", lambda e, t=t: e.tensor_copy(out=UR.h[:, t, :], in_=psP[2].h[:]), reads=[psP[2].b], writes=[UR_b[t]])
                    S.op("act", lambda e, s=s: e.activation(out=qst[s].h[:, 768:1024], in_=psP[3].h[:, 0:256], func=AF.Silu), reads=[psP[3].b], writes=[qst[s].b])
                    for c in range(7):
                        S.op("pe", lambda e, c=c, s=s: e.transpose(out=psT2.h[:, c, :], in_=trin[s].h[:, c * 128:(c + 1) * 128], identity=ident), reads=[trin[s].b, cm.b], writes=[psT2.b])
                    S.op("dve", lambda e, s=s: e.tensor_copy(out=qst[s].h[:, 0:512], in_=psT2.h[:, 0:4, :].rearrange("p c n -> p (c n)")), reads=[psT2.b], writes=[qst[s].b])
                    S.op("act", lambda e, s=s: e.copy(out=qst[s].h[:, 512:768], in_=psT2.h[:, 5:7, :].rearrange("p c n -> p (c n)")), reads=[psT2.b], writes=[qst[s].b])
                    S.op("dve", lambda e, t=t: e.tensor_copy(out=KT.h[:, t, :], in_=psT2.h[:, 4, :]), reads=[psT2.b], writes=[KT_b[t]])
                    S.op("sp", lambda e, t=t, s=s: e.dma_start(out=qbun[t], in_=qst[s].h[:]), reads=[qst[s].b], writes=[qbun_b[t]], dma_key=qst[s].b)
                S.emit()

            if stop == "p1":
                M.__exit__(None, None, None)
                break
            with contextlib.ExitStack() as P:
                S = Sched(nc, "L%dp2" % layer)
                wout = sb(P, "wout", [128, 8, D], BF16)
                QB = [sb(P, "QB%d" % i, [128, 1024], BF16) for i in range(2)]
                XR = [sb(P, "XR%d" % i, [128, D], F32) for i in range(2)]
                PT = [sb(P, "PT%d" % i, [128, 512], BF16) for i in range(3)]
                mix = [sb(P, "mix%d" % i, [128, D], BF16) for i in range(2)]
                mixT = [sb(P, "mixT%d" % i, [128, 8, 128], BF16) for i in range(2)]
                den = sb(P, "den", [128, 4], F32)
                rec = sb(P, "rec", [128, 4], F32)
                plT = sb(P, "plT", [64, 512], BF16)
                qbd = [sb(P, "qbd%d" % i, [128, 512], BF16) for i in range(2)]
                PTr = [sb(P, "PTr%d" % i, [128, 512], BF16) for i in range(2)]
                qfb = [sb(P, "qfb%d" % i, [128, 256], BF16) for i in range(2)]
                kdf = [sb(P, "kdf%d" % i, [128, 128], BF16) for i in range(2)]
                SFs = sb(P, "SFs", [128, 256], F32)
                SBs = sb(P, "SBs", [128, 256], F32)
                SFbd = [sb(P, "SFbd%d" % i, [128, 256], BF16) for i in range(2)]
                sq = sb(P, "sq", [128, 256], F32)
                ro = sb(P, "ro", [128, 256], F32)
                cen = sb(P, "cen", [128, 256], F32)
                st8 = sb(P, "st8", [128, 24], F32)
                tA = [sb(P, "tA%d" % i, [128, D], F32) for i in range(2)]
                st6 = sb(P, "st6", [128, 2, 6], F32)
                mv = sb(P, "mv", [128, 4], F32)
                psS = [ps(P, "psS%d" % i, [128, 512], F32) for i in range(2)]
                psO = ps(P, "psO", [128, 512], F32)
                psY = [ps(P, "psY%d" % i, [128, 512], F32) for i in range(1)] * 2
                psMT = ps(P, "psMT", [128, 8, 128], BF16)
                psPL = ps(P, "psPL", [128, 512], F32)
                psMa = ps(P, "psMa", [128, 512], F32)
                psR = ps(P, "psR", [128, 512], F32)
                sctr = [0]

                def next_psS():
                    sctr[0] += 1
                    return psS[sctr[0] % 2]

                pctr = [0]

                def next_PT():
                    pctr[0] += 1
                    return PT[pctr[0] % 3]

                S.op("pool", lambda e: e.dma_start(out=wout.h[:], in_=w_out[layer].rearrange("(k p) n -> p k n", p=128)), writes=[wout.b], dma_key=wout.b)
                if layer == 0 and n_layers > 1 and SPARSE:
                    for src_, dst_ in ((moe_wg, wg2d), (moe_wu, wu2d), (moe_wd, wd2d)):
                        for ch in range(8):
                            S.op("pool", lambda e, src_=src_, dst_=dst_, ch=ch: e.dma_start(out=dst_[ch * 512:(ch + 1) * 512, :], in_=src_[ch * 512:(ch + 1) * 512, :]), writes=[], dma_key=Buf("cv"))
                S.op("pool", lambda e: e.memset(SBs.h[:], 0.0), writes=[SBs.b])
                S.op("pool", lambda e: e.memset(SFs.h[:], 0.0), writes=[SFs.b])
                S.op("pool", lambda e: e.memset(SFbd[0].h[:], 0.0), writes=[SFbd[0].b])
                border = [1, 0] + list(range(NT - 1, NCT - 1, -1))
                for i, t in enumerate(border):
                    S.op("pool", lambda e, t=t: e.tensor_tensor(out=SBD.h[:, t, :], in0=SBs.h[:], in1=maskS, op=ALU.mult), reads=[SBs.b, cm.b], writes=[SBD_b[t]])
                    if i == len(border) - 1:
                        break
                    if "bwd" in SKIP:
                        continue
                    kd = kdf[i % 2]
                    S.op("pool", lambda e, t=t, kd=kd: e.tensor_tensor(out=kd.h[:], in0=RKT.h[:, t, :], in1=WFB.h[:, 128:256], op=ALU.mult), reads=[RKT_b[t], WFB.b], writes=[kd.b])
                    pk = next_psS()
                    S.op("pe", lambda e, t=t, kd=kd, pk=pk: e.matmul(pk.h[:, 0:256], lhsT=kd.h[:], rhs=UR.h[:, t, 256:512], start=True, stop=True), reads=[kd.b, UR_b[t]], writes=[pk.b])
                    S.op("dve", lambda e, pk=pk: e.scalar_tensor_tensor(out=SBs.h[:], in0=SBs.h[:], scalar=gC.h[:, 1:2], in1=pk.h[:, 0:256], op0=ALU.mult, op1=ALU.add), reads=[SBs.b, gC.b, pk.b], writes=[SBs.b])
                sf_cur = 0
                for t in range(NT):
                    s = t % 2
                    is_ctx = t < NCT
                    r = 1 if is_ctx else 0
                    do_out = t in p2_tiles
                    qb_ = QB[s]
                    if do_out:
                        S.op("sp", lambda e, t=t, qb_=qb_: e.dma_start(out=qb_.h[:], in_=qbun[t]), reads=[qbun_b[t]], writes=[qb_.b], dma_key=qb_.b)
                        S.op("sp", lambda e, t=t, s=s: e.dma_start(out=XR[s].h[:], in_=xsrc[t * 128:(t + 1) * 128, :]), reads=xsrc_bufs(t), writes=[XR[s].b], dma_key=XR[s].b)
                        mx = mix[s]
                        if is_ctx:
                            blocks = [(0, None), (1, None)]
                        else:
                            blocks = []
                            if t > NCT:
                                blocks.append((t - 1, mprev))
                            blocks.append((t, None))
                            if t < NT - 1:
                                blocks.append((t + 1, mnext))
                            blocks += [(0, None), (1, None)]
                        first = t in (0, NCT)
                        lastt = t in (NCT - 1, NT - 1)
                        qd = qbd[s]
                        S.op("dve", lambda e, qd=qd, qb_=qb_: e.tensor_tensor(out=qd.h[:].rearrange("p (h n) -> p h n", h=4), in0=maskQ.rearrange("p (h n) -> p h n", h=4), in1=qb_.h[:, 512:640].unsqueeze(1).to_broadcast([128, 4, 128]), op=ALU.mult),
                             reads=[qb_.b, cm.b], writes=[qd.b])
                        qf = qfb[s]
                        S.op("dve", lambda e, qf=qf, qb_=qb_: e.tensor_tensor(out=qf.h[:].rearrange("p (a n) -> p a n", a=2), in0=DFB.h[:].rearrange("p (a n) -> p a n", a=2), in1=qb_.h[:, 512:640].unsqueeze(1).to_broadcast([128, 2, 128]), op=ALU.mult),
                             reads=[qb_.b, DFB.b], writes=[qf.b])
                        for g in range(4):
                            srcs = []
                            if not first:
                                srcs.append((t - 1, 3))
                            srcs.append((t, 0 if first else (2 if lastt else 1)))
                            if not lastt:
                                srcs.append((t + 1, 4))
                            for si, (j, v) in enumerate(srcs):
                                S.op("pe", lambda e, g=g, j=j, v=v, si=si, ns=len(srcs): e.matmul(psPL.h[0:64, g * 128:(g + 1) * 128], lhsT=UR.h[:, j, g * 64:(g + 1) * 64], rhs=PA(g, v), start=(si == 0), stop=(si == ns - 1)),
                                     reads=[UR_b[j], cb.b], writes=[psPL.b])
                        S.op("act", lambda e: e.copy(out=plT.h[:], in_=psPL.h[0:64, :]), reads=[psPL.b], writes=[plT.b])
                        pA = next_psS()
                        S.op("pe", lambda e, pA=pA, qd=qd, qb_=qb_: e.matmul(pA.h[:], lhsT=qb_.h[:, 640:768], rhs=qd.h[:], start=True, stop=True), reads=[qb_.b, qd.b], writes=[pA.b])
                        ptr = PTr[s]
                        S.op("dve", lambda e, pA=pA, ptr=ptr: e.tensor_tensor(out=ptr.h[:], in0=pA.h[:], in1=DsumT.h[:], op=ALU.mult), reads=[pA.b, DsumT.b], writes=[ptr.b])
                    sfc = SFbd[sf_cur]
                    if t < NT - 1:
                        kd = kdf[t % 2]
                        S.op("pool", lambda e, t=t, kd=kd: e.tensor_tensor(out=kd.h[:], in0=RKT.h[:, t, :], in1=WFB.h[:, 0:128], op=ALU.mult), reads=[RKT_b[t], WFB.b], writes=[kd.b])
                        pk = next_psS()
                        S.op("pe", lambda e, t=t, kd=kd, pk=pk: e.matmul(pk.h[:, 0:256], lhsT=kd.h[:], rhs=UR.h[:, t, 256:512], start=True, stop=True), reads=[kd.b, UR_b[t]], writes=[pk.b])
                        S.op("dve", lambda e, pk=pk: e.scalar_tensor_tensor(out=SFs.h[:], in0=SFs.h[:], scalar=gC.h[:, 0:1], in1=pk.h[:, 0:256], op0=ALU.mult, op1=ALU.add), reads=[SFs.b, gC.b, pk.b], writes=[SFs.b])
                        sf_cur = 1 - sf_cur
                        sfn = SFbd[sf_cur]
                        S.op("pool", lambda e, sfn=sfn: e.tensor_tensor(out=sfn.h[:], in0=SFs.h[:], in1=maskS, op=ALU.mult), reads=[SFs.b, cm.b], writes=[sfn.b])
                    if do_out:
                        seq = [(kv, bi, j, mk) for kv in range(2) for bi, (j, mk) in enumerate(blocks)]
                        nb_ = len(blocks)

                        def emit_score(kv, bi, j, mk):
                            pS = next_psS()
                            S.op("pe", lambda e, kv=kv, j=j, mk=mk, pS=pS, qb_=qb_: e.matmul(pS.h[:], lhsT=KT.h[64 * kv:64 * kv + 64, j, :], rhs=qb_.h[64 * kv:64 * kv + 64, 0:512], start=True, stop=(mk is None)),
                                 reads=[KT_b[j], qb_.b], writes=[pS.b])
                            if mk is not None:
                                S.op("pe", lambda e, mk=mk, pS=pS: e.matmul(pS.h[:], lhsT=ident, rhs=mk, start=False, stop=True), reads=[cm.b, cb.b], writes=[pS.b])
                            return pS
                        pend = emit_score(*seq[0])
                        for i, (kv, bi, j, mk) in enumerate(seq):
                            pS = pend
                            if i + 1 < len(seq):
                                pend = emit_score(*seq[i + 1])
                            pt = next_PT()
                            S.op("act", lambda e, pS=pS, pt=pt: e.activation(out=pt.h[:], in_=pS.h[:], func=AF.Exp, scale=0.125), reads=[pS.b], writes=[pt.b])
                            for g in range(4):
                                S.op("pe", lambda e, g=g, j=j, kv=kv, pt=pt, bi=bi, nb_=nb_: e.matmul(psO.h[:, g * 65:(g + 1) * 65], lhsT=pt.h[:, g * 128:(g + 1) * 128], rhs=VX.h[:, j, kv, :], start=(bi == 0 and g == 0), stop=(bi == nb_ - 1 and g == 3), skip_group_check=True),
                                     reads=[pt.b, VX_b[j]], writes=[psO.b])
                            if bi == nb_ - 1:
                                pov = psO.h[:, 0:260].rearrange("p (g c) -> p g c", g=4)
                                S.op("dve", lambda e, kv=kv, pov=pov: e.tensor_tensor(out=den.h[:], in0=pov[:, :, 64], in1=esink.h[:, 4 * kv:4 * kv + 4], op=ALU.add), reads=[psO.b, esink.b], writes=[den.b])
                                S.op("dve", lambda e: e.reciprocal(out=rec.h[:], in_=den.h[:]), reads=[den.b], writes=[rec.b])
                                S.op("dve", lambda e, kv=kv, pov=pov, mx=mx: e.tensor_tensor(out=mx.h[:, kv * 256:(kv + 1) * 256].rearrange("p (g d) -> p g d", g=4), in0=pov[:, :, 0:64], in1=rec.h[:].unsqueeze(2).to_broadcast([128, 4, 64]), op=ALU.mult),
                                     reads=[psO.b, rec.b], writes=[mx.b])
                        for g in range(4):
                            S.op("pe", lambda e, g=g: e.matmul(psMa.h[:, g * 64:(g + 1) * 64], lhsT=plT.h[:, g * 128:(g + 1) * 128], rhs=PW.h[:, g * 64:(g + 1) * 64], start=True, stop=True), reads=[plT.b, PW.b], writes=[psMa.b])
                        S.op("act", lambda e, mx=mx: e.copy(out=mx.h[:, 512:768], in_=psMa.h[:, 0:256]), reads=[psMa.b], writes=[mx.b])
                        S.op("pe", lambda e, qf=qf, sfc=sfc: e.matmul(psR.h[:, 0:256], lhsT=qf.h[:, 0:128], rhs=sfc.h[:], start=True, stop=False), reads=[qf.b, sfc.b], writes=[psR.b])
                        S.op("pe", lambda e, qf=qf, t=t: e.matmul(psR.h[:, 0:256], lhsT=qf.h[:, 128:256], rhs=SBD.h[:, t, :], start=False, stop=False), reads=[qf.b, SBD_b[t]], writes=[psR.b])
                        for h in range(4):
                            S.op("pe", lambda e, h=h, ptr=ptr, t=t: e.matmul(psR.h[:, h * 64:(h + 1) * 64], lhsT=ptr.h[:, h * 128:(h + 1) * 128], rhs=UR.h[:, t, 256 + h * 64:256 + (h + 1) * 64], start=False, stop=(h == 3)),
                                 reads=[ptr.b, UR_b[t]], writes=[psR.b])
                    if not do_out:
                        continue
                    if "ret" in SKIP:
                        pass
                    else:
                        S.op("act", lambda e: e.copy(out=ro.h[:], in_=psR.h[:, 0:256]), reads=[psR.b], writes=[ro.b])
                        prv = ro.h[:].rearrange("p (h d) -> p h d", h=4)
                        S.op("dve", lambda e, prv=prv: e.tensor_reduce(out=st8.h[:, 0:4], in_=prv, axis=AX.X, op=ALU.add), reads=[ro.b], writes=[st8.b])
                        S.op("act", lambda e: e.activation(out=sq.h[:], in_=ro.h[:], func=AF.Square), reads=[ro.b], writes=[sq.b])
                        S.op("dve", lambda e: e.tensor_reduce(out=st8.h[:, 4:8], in_=sq.h[:].rearrange("p (h d) -> p h d", h=4), axis=AX.X, op=ALU.add), reads=[sq.b], writes=[st8.b])
                        S.op("dve", lambda e: e.tensor_scalar_mul(out=st8.h[:, 8:12], in0=st8.h[:, 0:4], scalar1=1.0 / 64), reads=[st8.b], writes=[st8.b])
                        S.op("dve", lambda e: e.tensor_tensor(out=st8.h[:, 12:16], in0=st8.h[:, 8:12], in1=st8.h[:, 8:12], op=ALU.mult), reads=[st8.b], writes=[st8.b])
                        S.op("dve", lambda e: e.scalar_tensor_tensor(out=st8.h[:, 16:20], in0=st8.h[:, 4:8], scalar=1.0 / 64, in1=st8.h[:, 12:16], op0=ALU.mult, op1=ALU.subtract), reads=[st8.b], writes=[st8.b])
                        S.op("act", lambda e: e.activation(out=st8.h[:, 20:24], in_=st8.h[:, 16:20], func=AF.Ln, bias=epsc, scale=1.0), reads=[st8.b, cc.b], writes=[st8.b])
                        S.op("act", lambda e: e.activation(out=st8.h[:, 20:24], in_=st8.h[:, 20:24], func=AF.Exp, scale=-0.5), reads=[st8.b], writes=[st8.b])
                        S.op("dve", lambda e, prv=prv: e.tensor_tensor(out=cen.h[:].rearrange("p (h d) -> p h d", h=4), in0=prv, in1=st8.h[:, 8:12].unsqueeze(2).to_broadcast([128, 4, 64]), op=ALU.subtract), reads=[ro.b, st8.b], writes=[cen.b])
                        S.op("dve", lambda e: e.tensor_tensor(out=cen.h[:].rearrange("p (h d) -> p h d", h=4), in0=cen.h[:].rearrange("p (h d) -> p h d", h=4), in1=st8.h[:, 20:24].unsqueeze(2).to_broadcast([128, 4, 64]), op=ALU.mult), reads=[cen.b, st8.b], writes=[cen.b])
                        S.op("pool", lambda e, mx=mx, qb_=qb_: e.tensor_tensor(out=mx.h[:, 768:1024], in0=cen.h[:], in1=qb_.h[:, 768:1024], op=ALU.mult), reads=[cen.b, qb_.b], writes=[mx.b])
                    if "oproj" in SKIP:
                        continue
                    for kc in range(8):
                        S.op("pe", lambda e, kc=kc, mx=mx: e.transpose(out=psMT.h[:, kc, :], in_=mx.h[:, kc * 128:(kc + 1) * 128], identity=ident), reads=[mx.b, cm.b], writes=[psMT.b])
                    mt = mixT[s]
                    S.op("act", lambda e, mt=mt: e.copy(out=mt.h[:, 0:4, :], in_=psMT.h[:, 0:4, :]), reads=[psMT.b], writes=[mt.b])
                    S.op("dve", lambda e, mt=mt: e.tensor_copy(out=mt.h[:, 4:8, :], in_=psMT.h[:, 4:8, :]), reads=[psMT.b], writes=[mt.b])
                    ta = tA[s]
                    gi = 2 if is_ctx else 0
                    for nh in range(2):
                        for kc in range(8):
                            S.op("pe", lambda e, nh=nh, kc=kc, mt=mt: e.matmul(psY[nh].h[:], lhsT=mt.h[:, kc, :], rhs=wout.h[:, kc, nh * 512:(nh + 1) * 512], start=(kc == 0), stop=(kc == 7)), reads=[mt.b, wout.b], writes=[psY[nh].b])
                        S.op("dve", lambda e, nh=nh, ta=ta, gi=gi: e.tensor_tensor(out=ta.h[:, nh * 512:(nh + 1) * 512], in0=psY[nh].h[:], in1=Gt[gi].h[:, nh * 512:(nh + 1) * 512], op=ALU.mult), reads=[psY[nh].b, Gt[gi].b], writes=[ta.b])
                    emit_ln_epilogue(S, ta, XR[s], LNt[0], LNt[1], st6, mv, epsln, cc,
                                     xs1[t * 128:(t + 1) * 128, :], xs1_b[t])
                S.emit()

            M.__exit__(None, None, None)
            if stop == "p2":
                break

            if (layer % 2 == 1) and SPARSE:
                I32 = mybir.dt.int32
                NK = 24
                xlat = xs1[NCT * 128:, :]
                slot_b = Buf("slotinfo")
                F_b = Buf("Fsc")
                IW = sb(G, "IW%d" % layer, [128, NK, 56], I32)
                with contextlib.ExitStack() as P:
                    S = Sched(nc, "L%dr" % layer)
                    rc = sb(P, "rc", [128, RC_TOT], F32)
                    idf = sb(P, "idf", [128, 128], F32)
                    wr = sb(P, "wr", [128, 8, NEXP], F32)
                    XGr = [sb(P, "XGr%d" % i, [128, D], F32) for i in range(2)]
                    t32 = [sb(P, "t32r%d" % i, [128, 8, 128], F32) for i in range(2)]
                    gt = sb(P, "gtr", [128, 64], F32)
                    M12 = sb(P, "M12", [128, 32, 16], F32)
                    W12 = sb(P, "W12", [128, 32, 2], F32)
                    POS = sb(P, "POS", [128, 32, 8], F32)
                    carry = sb(P, "carry", [128, 8], F32)
                    seg = sb(P, "seg", [128, 32], F32)
                    sl = sb(P, "sl", [128, 32, 2], F32)
                    sli = sb(P, "sli", [128, 32, 2], I32)
                    info = sb(P, "info", [128, 32, 2, 4], F32)
                    tmp8 = sb(P, "tmp8", [128, 16], F32)
                    IWf = sb(P, "IWf", [128, NK, 56], F32)
                    psXf = [ps(P, "psXr%d" % i, [128, 4, 128], F32) for i in range(2)]
                    psL = [ps(P, "psL%d" % i, [128, 512], F32) for i in range(2)]
                    psC = [ps(P, "psC%d" % i, [128, 512], F32) for i in range(2)]
                    S.op("sp", lambda e: e.dma_start(out=rc.h[:], in_=rconst), writes=[rc.b], dma_key=rc.b)
                    S.op("sp", lambda e: e.dma_start(out=idf.h[:], in_=consts[:, C_IDENT:C_IDENT + 128]), writes=[idf.b], dma_key=idf.b)
                    S.op("sp", lambda e: e.dma_start(out=wr.h[:], in_=router.rearrange("(k p) n -> p k n", p=128)), writes=[wr.b], dma_key=wr.b)
                    S.op("sp", lambda e: e.dma_start(out=slotinfo, in_=slot_init), writes=[slot_b], dma_key=slot_b)
                    sck = [Buf("sck%d" % i) for i in range(4)]
                    S.op("pool", lambda e: e.memset(carry.h[:], 0.0), writes=[carry.b])
                    S.op("pool", lambda e: e.memset(info.h[:], 0.0), writes=[info.b])
                    Umat = rc.h[:, RC_U:RC_U + 128]
                    Ones = rc.h[:, RC_ONES:RC_ONES + 128]
                    for ti in range(32):
                        t = NCT + ti
                        xg = XGr[ti % 2]
                        t3 = t32[ti % 2]
                        S.op("sp", lambda e, t=t, xg=xg: e.dma_start(out=xg.h[:], in_=xs1[t * 128:(t + 1) * 128, :]), reads=[xs1_b[t]], writes=[xg.b], dma_key=xg.b)
                        for hf in range(2):
                            px = psXf[hf]
                            for k4 in range(4):
                                kc = hf * 4 + k4
                                S.op("pe", lambda e, kc=kc, k4=k4, xg=xg, px=px: e.transpose(out=px.h[:, k4, :], in_=xg.h[:, kc * 128:(kc + 1) * 128], identity=idf.h[:]), reads=[xg.b, idf.b], writes=[px.b])
                            for k4 in range(4):
                                kc = hf * 4 + k4
                                if kc % 2 == 0:
                                    S.op("act", lambda e, kc=kc, k4=k4, px=px, t3=t3: e.activation(out=t3.h[:, kc, :], in_=px.h[:, k4, :], func=AF.Identity, bias=SH2(kc, 0), scale=A2(kc, 0)), reads=[px.b, modT.b], writes=[t3.b])
                                else:
                                    S.op("dve", lambda e, kc=kc, k4=k4, px=px, t3=t3: e.tensor_scalar(out=t3.h[:, kc, :], in0=px.h[:, k4, :], scalar1=A2(kc, 0), scalar2=SH2(kc, 0), op0=ALU.mult, op1=ALU.add), reads=[px.b, modT.b], writes=[t3.b])
                        pr = psL[ti % 2]
                        for kc in range(8):
                            S.op("pe", lambda e, kc=kc, t3=t3, pr=pr: e.matmul(pr.h[:, 0:NEXP], lhsT=t3.h[:, kc, :], rhs=wr.h[:, kc, :], start=(kc == 0), stop=(kc == 7)), reads=[t3.b, wr.b], writes=[pr.b])
                        lg_ = gt.h[:, 0:8]; m1 = gt.h[:, 8:9]; l2 = gt.h[:, 24:32]; m2 = gt.h[:, 9:10]
                        dd = gt.h[:, 10:11]; ee = gt.h[:, 11:12]
                        k1 = M12.h[:, ti, 0:8]; k2 = M12.h[:, ti, 8:16]
                        w1 = W12.h[:, ti, 0:1]; w2 = W12.h[:, ti, 1:2]
                        gb = [gt.b, M12.b, W12.b]
                        S.op("dve", lambda e, pr=pr, lg_=lg_: e.tensor_copy(out=lg_, in_=pr.h[:, 0:NEXP]), reads=[pr.b], writes=gb)
                        S.op("dve", lambda e, lg_=lg_, m1=m1: e.tensor_reduce(out=m1, in_=lg_, axis=AX.X, op=ALU.max), reads=gb, writes=gb)
                        S.op("dve", lambda e, lg_=lg_, m1=m1, k1=k1: e.tensor_scalar(out=k1, in0=lg_, scalar1=m1, scalar2=None, op0=ALU.is_equal), reads=gb, writes=gb)
                        S.op("dve", lambda e, lg_=lg_, k1=k1, l2=l2: e.scalar_tensor_tensor(out=l2, in0=k1, scalar=-1e30, in1=lg_, op0=ALU.mult, op1=ALU.add), reads=gb, writes=gb)
                        S.op("dve", lambda e, l2=l2, m2=m2: e.tensor_reduce(out=m2, in_=l2, axis=AX.X, op=ALU.max), reads=gb, writes=gb)
                        S.op("dve", lambda e, l2=l2, m2=m2, k2=k2: e.tensor_scalar(out=k2, in0=l2, scalar1=m2, scalar2=None, op0=ALU.is_equal), reads=gb, writes=gb)
                        S.op("dve", lambda e, dd=dd, m1=m1, m2=m2: e.tensor_tensor(out=dd, in0=m2, in1=m1, op=ALU.subtract), reads=gb, writes=gb)
                        S.op("act", lambda e, dd=dd, ee=ee: e.activation(out=ee, in_=dd, func=AF.Exp), reads=gb, writes=gb)
                        S.op("dve", lambda e, ee=ee, w1=w1: e.tensor_scalar_add(out=w1, in0=ee, scalar1=1.0), reads=gb, writes=gb)
                        S.op("dve", lambda e, w1=w1: e.reciprocal(out=w1, in_=w1), reads=gb, writes=gb)
                        S.op("dve", lambda e, ee=ee, w1=w1, w2=w2: e.tensor_tensor(out=w2, in0=ee, in1=w1, op=ALU.mult), reads=gb, writes=gb)
                        ma = tmp8.h[:, 0:8]
                        S.op("dve", lambda e, k1=k1, k2=k2, ma=ma: e.tensor_tensor(out=ma, in0=k1, in1=k2, op=ALU.add), reads=gb, writes=[tmp8.b])
                        pc = psC[ti % 2]
                        S.op("pe", lambda e, pc=pc, ma=ma: e.matmul(pc.h[:, 0:8], lhsT=Umat, rhs=ma, start=True, stop=True), reads=[rc.b, tmp8.b], writes=[pc.b])
                        S.op("pe", lambda e, pc=pc, ma=ma: e.matmul(pc.h[:, 8:16], lhsT=Ones, rhs=ma, start=True, stop=True), reads=[rc.b, tmp8.b], writes=[pc.b])
                        S.op("dve", lambda e, pc=pc, ti=ti: e.tensor_tensor(out=POS.h[:, ti, :], in0=pc.h[:, 0:8], in1=carry.h[:], op=ALU.add), reads=[pc.b, carry.b], writes=[POS.b])
                        S.op("dve", lambda e, pc=pc: e.tensor_tensor(out=carry.h[:], in0=pc.h[:, 8:16], in1=carry.h[:], op=ALU.add), reads=[pc.b, carry.b], writes=[carry.b])
                    tl = seg.h[:, 0:8]; se = seg.h[:, 8:16]; ss = seg.h[:, 16:24]
                    sgb = [seg.b]
                    S.op("dve", lambda e: e.tensor_scalar(out=tl, in0=carry.h[:], scalar1=0.0, scalar2=None, op0=ALU.is_gt), reads=[carry.b], writes=sgb)
                    for j in range(1, 8):
                        S.op("dve", lambda e, j=j: e.scalar_tensor_tensor(out=tl, in0=carry.h[:], scalar=512.0 * j, in1=tl, op0=ALU.is_gt, op1=ALU.add), reads=[carry.b] + sgb, writes=sgb)
                    S.op("dve", lambda e: e.tensor_copy(out=se[:, 0:1], in_=tl[:, 0:1]), reads=sgb, writes=sgb)
                    for j in range(1, 8):
                        S.op("dve", lambda e, j=j: e.tensor_tensor(out=se[:, j:j + 1], in0=se[:, j - 1:j], in1=tl[:, j:j + 1], op=ALU.add), reads=sgb, writes=sgb)
                    S.op("dve", lambda e: e.tensor_tensor(out=ss, in0=se, in1=tl, op=ALU.subtract), reads=sgb, writes=sgb)
                    S.op("dve", lambda e: e.tensor_scalar_mul(out=seg.h[:, 8:24], in0=seg.h[:, 8:24], scalar1=512.0), reads=sgb, writes=sgb)
                    for ti in range(32):
                        for k in range(2):
                            tq = tmp8.h[:, 8:16]
                            S.op("dve", lambda e, ti=ti, tq=tq: e.tensor_tensor(out=tq, in0=POS.h[:, ti, :], in1=ss, op=ALU.add), reads=[POS.b] + sgb, writes=[tmp8.b])
                            S.op("dve", lambda e, ti=ti, k=k, tq=tq: e.tensor_tensor(out=tq, in0=tq, in1=M12.h[:, ti, 8 * k:8 * k + 8], op=ALU.mult), reads=[tmp8.b, M12.b], writes=[tmp8.b])
                            S.op("dve", lambda e, ti=ti, k=k, tq=tq: e.tensor_reduce(out=sl.h[:, ti, k:k + 1], in_=tq, axis=AX.X, op=ALU.add), reads=[tmp8.b], writes=[sl.b])
                    S.op("dve", lambda e: e.tensor_copy(out=sli.h[:], in_=sl.h[:]), reads=[sl.b], writes=[sli.b])
                    tokv = rc.h[:, RC_TOK:RC_TOK + 32]
                    for k in range(2):
                        S.op("act", lambda e, k=k: e.copy(out=info.h[:, :, k, 0], in_=tokv), reads=[rc.b], writes=[info.b])
                        S.op("act", lambda e, k=k: e.copy(out=info.h[:, :, k, 1], in_=W12.h[:, :, k]), reads=[W12.b], writes=[info.b])
                        S.op("dve", lambda e, k=k: e.tensor_scalar_add(out=info.h[:, :, k, 2], in0=tokv, scalar1=float(k * L - NCT * 128)), reads=[rc.b], writes=[info.b])
                    for ti in range(32):
                        for k in range(2):
                            S.op("pool", lambda e, ti=ti, k=k: e.indirect_dma_start(out=slotinfo, out_offset=bass.IndirectOffsetOnAxis(ap=sli.h[:, ti, k:k + 1], axis=0), in_=info.h[:, ti, k, :], in_offset=None),
                                 reads=[sli.b, info.b, slot_b], writes=[], dma_key=sck[(2 * ti + k) % 4])
                    for k in range(NK):
                        c8 = tmp8.h[:, 0:8]; ek = tmp8.h[:, 8:9]; e1 = tmp8.h[:, 9:10]; e2 = tmp8.h[:, 10:11]
                        tb = [tmp8.b]
                        S.op("dve", lambda e, k=k, c8=c8: e.tensor_scalar(out=c8, in0=se, scalar1=512.0 * k, scalar2=None, op0=ALU.is_le), reads=sgb, writes=tb)
                        S.op("dve", lambda e, c8=c8, ek=ek: e.tensor_reduce(out=ek, in_=c8, axis=AX.X, op=ALU.add), reads=tb, writes=tb)
                        S.op("dve", lambda e, ek=ek: e.tensor_scalar_min(out=ek, in0=ek, scalar1=7.0), reads=tb, writes=tb)
                        S.op("dve", lambda e, ek=ek, e1=e1: e.tensor_scalar_mul(out=e1, in0=ek, scalar1=512.0), reads=tb, writes=tb)
                        S.op("dve", lambda e, ek=ek, e2=e2: e.tensor_scalar_mul(out=e2, in0=ek, scalar1=2816.0), reads=tb, writes=tb)
                        S.op("dve", lambda e, k=k, e1=e1: e.tensor_scalar(out=IWf.h[:, k, 0:32], in0=rc.h[:, RC_BG:RC_BG + 32], scalar1=e1, scalar2=None, op0=ALU.add), reads=tb + [rc.b], writes=[IWf.b])
                        S.op("dve", lambda e, k=k, e2=e2: e.tensor_scalar(out=IWf.h[:, k, 32:54], in0=rc.h[:, RC_BD:RC_BD + 22], scalar1=e2, scalar2=None, op0=ALU.add), reads=tb + [rc.b], writes=[IWf.b])
                    S.op("pool", lambda e: e.memset(IWf.h[:, :, 54:56], 0.0), writes=[IWf.b])
                    S.op("dve", lambda e: e.tensor_copy(out=IW.h[:], in_=IWf.h[:]), reads=[IWf.b], writes=[IW.b])
                    S.emit()
                with contextlib.ExitStack() as P:
                    S = Sched(nc, "L%dx" % layer)
                    units = []
                    c0 = 0
                    for ncu in (6, 6, 5, 5):
                        units.append((c0, ncu))
                        c0 += ncu
                    WG = [sb(P, "WG%d" % i, [128, 8, 768], BF16) for i in range(2)]
                    WU = [sb(P, "WU%d" % i, [128, 8, 768], BF16) for i in range(2)]
                    WD = [sb(P, "WD%d" % i, [128, 6, D], BF16) for i in range(2)]
                    XG = sb(P, "XGx", [128, 4, D], F32)
                    XG_b = [Buf("XGx%d" % j) for j in range(4)]
                    tTs = [sb(P, "tTx%d" % i, [128, 8, 512], BF16) for i in range(2)]
                    hid = [sb(P, "hidx%d" % i, [128, 6, 512], BF16) for i in range(2)]
                    sg = [sb(P, "sgx%d" % i, [128, 512], BF16) for i in range(2)]
                    accs = [sb(P, "accx%d" % i, [128, 4, D], F32) for i in range(2)]
                    accs_b = [[Buf("accx%d_%d" % (i, j)) for j in range(4)] for i in range(2)]
                    SI = [sb(P, "SI%d" % i, [128, 4, 4], F32) for i in range(2)]
                    TI = [sb(P, "TI%d" % i, [128, 4, 4], I32) for i in range(2)]
                    idf = sb(P, "idfx", [128, 128], F32)
                    psXf = [ps(P, "psXx%d" % i, [128, 4, 128], F32) for i in range(2)]
                    psGa = [ps(P, "psGx%d" % i, [128, 512], F32) for i in range(2)]
                    psUa = [ps(P, "psUx%d" % i, [128, 512], F32) for i in range(2)]
                    psY = [ps(P, "psYx%d" % i, [128, 512], F32) for i in range(2)]
                    S.op("sp", lambda e: e.dma_start(out=idf.h[:], in_=consts[:, C_IDENT:C_IDENT + 128]), writes=[idf.b], dma_key=idf.b)
                    yc = [0]

                    def prep(k):
                        si = SI[k % 2]; tix = TI[k % 2]; tT = tTs[k % 2]
                        S.op("sp", lambda e, k=k, si=si: e.dma_start(out=si.h[:], in_=slotinfo[k * 512:(k + 1) * 512, :].rearrange("(s p) c -> p s c", p=128)), reads=[slot_b], writes=[si.b], dma_key=si.b)
                        S.op("dve", lambda e, si=si, tix=tix: e.tensor_copy(out=tix.h[:], in_=si.h[:]), reads=[si.b], writes=[tix.b])
                        for sub in range(4):
                            S.op("pool", lambda e, sub=sub, tix=tix: e.indirect_dma_start(out=XG.h[:, sub, :], out_offset=None, in_=xs1, in_offset=bass.IndirectOffsetOnAxis(ap=tix.h[:, sub, 0:1], axis=0)),
                                 reads=[tix.b] + xs1_b, writes=[XG_b[sub]], dma_key=XG_b[sub])
                        for sub in range(4):
                            for hf in range(2):
                                px = psXf[hf]
                                for k4 in range(4):
                                    kc = hf * 4 + k4
                                    S.op("pe", lambda e, kc=kc, k4=k4, sub=sub, px=px: e.transpose(out=px.h[:, k4, :], in_=XG.h[:, sub, kc * 128:(kc + 1) * 128], identity=idf.h[:]), reads=[XG_b[sub], idf.b], writes=[px.b])
                                for k4 in range(4):
                                    kc = hf * 4 + k4
                                    if kc % 2 == 0:
                                        S.op("act", lambda e, kc=kc, k4=k4, px=px, sub=sub, tT=tT: e.activation(out=tT.h[:, kc, sub * 128:(sub + 1) * 128], in_=px.h[:, k4, :], func=AF.Identity, bias=SH2(kc, 0), scale=A2(kc, 0)), reads=[px.b, modT.b], writes=[tT.b])
                                    else:
                                        S.op("dve", lambda e, kc=kc, k4=k4, px=px, sub=sub, tT=tT: e.tensor_scalar(out=tT.h[:, kc, sub * 128:(sub + 1) * 128], in0=px.h[:, k4, :], scalar1=A2(kc, 0), scalar2=SH2(kc, 0), op0=ALU.mult, op1=ALU.add), reads=[px.b, modT.b], writes=[tT.b])

                    def gather(k, ui):
                        us = (k * 4 + ui) % 2
                        iw = lambda k=k, ui=ui: bass.IndirectOffsetOnAxis(ap=IW.h[:, k, ui:ui + 1], axis=0)
                        S.op("pool", lambda e, us=us, iw=iw: e.indirect_dma_start(out=WG[us].h[:].rearrange("p k n -> p (k n)"), out_offset=None, in_=wg2d, in_offset=iw()), reads=[IW.b], writes=[WG[us].b], dma_key=WG[us].b)
                        S.op("pool", lambda e, us=us, iw=iw: e.indirect_dma_start(out=WU[us].h[:].rearrange("p k n -> p (k n)"), out_offset=None, in_=wu2d, in_offset=iw()), reads=[IW.b], writes=[WU[us].b], dma_key=WU[us].b)
                        S.op("pool", lambda e, us=us, iw=iw: e.indirect_dma_start(out=WD[us].h[:].rearrange("p c n -> p (c n)"), out_offset=None, in_=wd2d, in_offset=iw()), reads=[IW.b], writes=[WD[us].b], dma_key=WD[us].b)

                    def compute(k, ui):
                        c0, ncu = units[ui]
                        us = (k * 4 + ui) % 2
                        tT = tTs[k % 2]
                        acc = accs[k % 2]; acc_b = accs_b[k % 2]
                        hd = hid[us]
                        for c in range(ncu):
                            pg = psGa[c % 2]
                            pu = psUa[c % 2]
                            for kc in range(8):
                                S.op("pe", lambda e, c=c, kc=kc, us=us, pg=pg, tT=tT: e.matmul(pg.h[:], lhsT=WG[us].h[:, kc, c * 128:(c + 1) * 128], rhs=tT.h[:, kc, :], start=(kc == 0), stop=(kc == 7)), reads=[WG[us].b, tT.b], writes=[pg.b])
                            for kc in range(8):
                                S.op("pe", lambda e, c=c, kc=kc, us=us, pu=pu, tT=tT: e.matmul(pu.h[:], lhsT=WU[us].h[:, kc, c * 128:(c + 1) * 128], rhs=tT.h[:, kc, :], start=(kc == 0), stop=(kc == 7)), reads=[WU[us].b, tT.b], writes=[pu.b])
                            sg_ = sg[c % 2]
                            S.op("act", lambda e, pg=pg, sg_=sg_: e.activation(out=sg_.h[:], in_=pg.h[:], func=AF.Silu), reads=[pg.b], writes=[sg_.b])
                            S.op("dve", lambda e, c=c, pu=pu, sg_=sg_, hd=hd: e.tensor_tensor(out=hd.h[:, c, :], in0=pu.h[:], in1=sg_.h[:], op=ALU.mult), reads=[pu.b, sg_.b], writes=[hd.b])
                        for sub in range(4):
                            for nh in range(2):
                                py = psY[yc[0] % 2]
                                yc[0] += 1
                                for c in range(ncu):
                                    S.op("pe", lambda e, c=c, sub=sub, nh=nh, us=us, hd=hd, py=py, ncu=ncu: e.matmul(py.h[:], lhsT=hd.h[:, c, sub * 128:(sub + 1) * 128], rhs=WD[us].h[:, c, nh * 512:(nh + 1) * 512], start=(c == 0), stop=(c == ncu - 1)), reads=[hd.b, WD[us].b], writes=[py.b])
                                ao = acc.h[:, sub, nh * 512:(nh + 1) * 512]
                                if ui == 0:
                                    S.op("act", lambda e, ao=ao, py=py: e.copy(out=ao, in_=py.h[:]), reads=[py.b], writes=[acc_b[sub]])
                                else:
                                    S.op("dve", lambda e, ao=ao, py=py: e.tensor_tensor(out=ao, in0=py.h[:], in1=ao, op=ALU.add), reads=[py.b, acc_b[sub]], writes=[acc_b[sub]])

                    def fin(k):
                        si = SI[k % 2]; tix = TI[k % 2]
                        acc = accs[k % 2]; acc_b = accs_b[k % 2]
                        for sub in range(4):
                            S.op("act", lambda e, sub=sub, si=si, acc=acc: e.activation(out=acc.h[:, sub, :], in_=acc.h[:, sub, :], func=AF.Identity, scale=si.h[:, sub, 1:2]), reads=[acc_b[sub], si.b], writes=[acc_b[sub]])
                            S.op("pool", lambda e, sub=sub, tix=tix, acc=acc: e.indirect_dma_start(out=Fsc, out_offset=bass.IndirectOffsetOnAxis(ap=tix.h[:, sub, 2:3], axis=0), in_=acc.h[:, sub, :], in_offset=None),
                                 reads=[acc_b[sub], tix.b], writes=[], dma_key=acc_b[sub])

                    prep(0)
                    gather(0, 0)
                    gather(0, 1)
                    for k in range(NK):
                        for ui in range(4):
                            compute(k, ui)
                            nk, nu = (k, ui + 2) if ui + 2 < 4 else (k + 1, ui - 2)
                            if nk < NK:
                                gather(nk, nu)
                            if ui == 1 and k + 1 < NK:
                                prep(k + 1)
                        fin(k)
                    S.emit()
                with contextlib.ExitStack() as P:
                    S = Sched(nc, "L%dz" % layer)
                    F0 = [sb(P, "F0_%d" % i, [128, D], F32) for i in range(2)]
                    F1 = [sb(P, "F1_%d" % i, [128, D], F32) for i in range(2)]
                    XE = [sb(P, "XE%d" % i, [128, D], F32) for i in range(2)]
                    st6 = sb(P, "st6z", [128, 2, 6], F32)
                    mv = sb(P, "mvz", [128, 4], F32)
                    for ti in range(32):
                        t = NCT + ti
                        s_ = ti % 2
                        S.op("sp", lambda e, ti=ti, s_=s_: e.dma_start(out=F0[s_].h[:], in_=Fsc[ti * 128:(ti + 1) * 128, :]), reads=[F_b], writes=[F0[s_].b], dma_key=F0[s_].b)
                        S.op("sp", lambda e, ti=ti, s_=s_: e.dma_start(out=F1[s_].h[:], in_=Fsc[L + ti * 128:L + (ti + 1) * 128, :]), reads=[F_b], writes=[F1[s_].b], dma_key=F1[s_].b)
                        S.op("sp", lambda e, t=t, s_=s_: e.dma_start(out=XE[s_].h[:], in_=xs1[t * 128:(t + 1) * 128, :]), reads=[xs1_b[t]], writes=[XE[s_].b], dma_key=XE[s_].b)
                        S.op("pool", lambda e, s_=s_: e.tensor_tensor(out=F0[s_].h[:], in0=F0[s_].h[:], in1=F1[s_].h[:], op=ALU.add), reads=[F0[s_].b, F1[s_].b], writes=[F0[s_].b])
                        S.op("dve", lambda e, s_=s_: e.tensor_tensor(out=F0[s_].h[:], in0=F0[s_].h[:], in1=Gt[1].h[:], op=ALU.mult), reads=[F0[s_].b, Gt[1].b], writes=[F0[s_].b])
                        if last:
                            dst, dstb = out[ti * 128:(ti + 1) * 128, :], out_b[t]
                        else:
                            dst, dstb = xs2[t * 128:(t + 1) * 128, :], xs2_b[t]
                        emit_ln_epilogue(S, F0[s_], XE[s_], LNt[2], LNt[3], st6, mv, epsln, cc, dst, dstb)
                    S.emit()
                continue
            with contextlib.ExitStack() as P:
                S = Sched(nc, "L%dp3" % layer)
                moe = (layer % 2 == 1)
                nexp = moe_experts if moe else 1
                if moe:
                    wg_src = lambda e_: moe_wg[e_]
                    wu_src = lambda e_: moe_wu[e_]
                    wd_src = lambda e_: moe_wd[e_]
                else:
                    wg_src = lambda e_: ffn_wg[layer // 2]
                    wu_src = lambda e_: ffn_wu[layer // 2]
                    wd_src = lambda e_: ffn_wd[layer // 2]
                units = []
                for e_ in range(nexp):
                    c0 = 0
                    for ncu in (6, 6, 5, 5):
                        units.append((e_, c0, ncu))
                        c0 += ncu
                GT = 4
                tiles = list(range(NT)) if not last else list(range(NCT, NT))
                groups = []
                if not last:
                    groups.append([0, 1])
                for g0 in range(NCT, NT, GT):
                    groups.append(list(range(g0, g0 + GT)))
                WG = [sb(P, "WG%d" % i, [128, 8, 768], BF16) for i in range(2)]
                WU = [sb(P, "WU%d" % i, [128, 8, 768], BF16) for i in range(2)]
                WD = [sb(P, "WD%d" % i, [128, 6, D], BF16) for i in range(2)]
                XG = [sb(P, "XG%d" % i, [128, GT, D], F32) for i in range(1)]
                tT = [sb(P, "tT%d" % i, [128, 8, GT * 128], BF16) for i in range(1)]
                hid = [sb(P, "hid%d" % i, [128, 6, GT * 128], BF16) for i in range(2)]
                sg = [sb(P, "sg%d" % i, [128, GT * 128], BF16) for i in range(2)]
                acc = sb(P, "acc", [128, GT, D], F32)
                acc_b = [Buf("acc%d" % i) for i in range(GT)]
                XG_b = [[Buf("XG%d_%d" % (i, j)) for j in range(GT)] for i in range(1)]
                st6 = sb(P, "st6b", [128, 2, 6], F32)
                mv = sb(P, "mvb", [128, 4], F32)
                psXf = [ps(P, "psXf%d" % i, [128, 4, 128], F32) for i in range(2)]
                psGa = [ps(P, "psGa%d" % i, [128, 512], F32) for i in range(2)]
                psUa = [ps(P, "psUa%d" % i, [128, 512], F32) for i in range(2)]
                psY = [ps(P, "psYb%d" % i, [128, 512], F32) for i in range(2)]
                idf = sb(P, "idf", [128, 128], F32)
                S.op("sp", lambda e: e.dma_start(out=idf.h[:], in_=consts[:, C_IDENT:C_IDENT + 128]), writes=[idf.b], dma_key=idf.b)
                if moe:
                    wr = sb(P, "wr", [128, 8, NEXP], F32)
                    t32 = [sb(P, "t32_%d" % i, [128, 8, 128], F32) for i in range(2)]
                    gates = sb(P, "gates", [128, GT, NEXP], F32)
                    gates_b = [Buf("gates%d" % i) for i in range(GT)]
                    gt = sb(P, "gt", [128, 64], F32)
                    S.op("sp", lambda e: e.dma_start(out=wr.h[:], in_=router.rearrange("(k p) n -> p k n", p=128)), writes=[wr.b], dma_key=wr.b)
                uctr = 0
                yctr = 0
                for gi_, grp in enumerate(groups):
                    gs = 0
                    r = 1 if grp[0] < NCT else 0
                    N = len(grp) * 128
                    for ti, t in enumerate(grp):
                        S.op("sp", lambda e, t=t, ti=ti, gs=gs: e.dma_start(out=XG[gs].h[:, ti, :], in_=xs1[t * 128:(t + 1) * 128, :]), reads=[xs1_b[t]], writes=[XG_b[gs][ti]], dma_key=XG_b[gs][ti])
                        for hf in range(2):
                            px = psXf[hf]
                            for k4 in range(4):
                                kc = hf * 4 + k4
                                S.op("pe", lambda e, kc=kc, k4=k4, ti=ti, gs=gs, px=px: e.transpose(out=px.h[:, k4, :], in_=XG[gs].h[:, ti, kc * 128:(kc + 1) * 128], identity=idf.h[:]), reads=[XG_b[gs][ti], idf.b], writes=[px.b])
                            for k4 in range(4):
                                kc = hf * 4 + k4
                                if moe:
                                    t3 = t32[ti % 2]
                                    S.op("act", lambda e, kc=kc, k4=k4, px=px, t3=t3, r=r: e.activation(out=t3.h[:, kc, :], in_=px.h[:, k4, :], func=AF.Identity, bias=SH2(kc, r), scale=A2(kc, r)), reads=[px.b, modT.b], writes=[t3.b])
                                    S.op("dve", lambda e, kc=kc, ti=ti, gs=gs, t3=t3: e.tensor_copy(out=tT[gs].h[:, kc, ti * 128:(ti + 1) * 128], in_=t3.h[:, kc, :]), reads=[t3.b], writes=[tT[gs].b])
                                elif kc % 2 == 0:
                                    S.op("act", lambda e, kc=kc, k4=k4, px=px, ti=ti, gs=gs, r=r: e.activation(out=tT[gs].h[:, kc, ti * 128:(ti + 1) * 128], in_=px.h[:, k4, :], func=AF.Identity, bias=SH2(kc, r), scale=A2(kc, r)), reads=[px.b, modT.b], writes=[tT[gs].b])
                                else:
                                    S.op("dve", lambda e, kc=kc, k4=k4, px=px, ti=ti, gs=gs, r=r: e.tensor_scalar(out=tT[gs].h[:, kc, ti * 128:(ti + 1) * 128], in0=px.h[:, k4, :], scalar1=A2(kc, r), scalar2=SH2(kc, r), op0=ALU.mult, op1=ALU.add), reads=[px.b, modT.b], writes=[tT[gs].b])
                        if moe:
                            t3 = t32[ti % 2]
                            pr = psY[yctr % 2]
                            yctr += 1
                            for kc in range(8):
                                S.op("pe", lambda e, kc=kc, t3=t3, pr=pr: e.matmul(pr.h[:, 0:NEXP], lhsT=t3.h[:, kc, :], rhs=wr.h[:, kc, :], start=(kc == 0), stop=(kc == 7)), reads=[t3.b, wr.b], writes=[pr.b])
                            lg_ = gt.h[:, 0:8]; m1 = gt.h[:, 8:9]; k1 = gt.h[:, 16:24]; l2 = gt.h[:, 24:32]; m2 = gt.h[:, 9:10]; k2 = gt.h[:, 32:40]
                            dd = gt.h[:, 10:11]; ee = gt.h[:, 11:12]; w1 = gt.h[:, 12:13]; w2 = gt.h[:, 13:14]
                            gb = [gt.b]
                            S.op("dve", lambda e, pr=pr, lg_=lg_: e.tensor_copy(out=lg_, in_=pr.h[:, 0:NEXP]), reads=[pr.b], writes=gb)
                            S.op("dve", lambda e, lg_=lg_, m1=m1: e.tensor_reduce(out=m1, in_=lg_, axis=AX.X, op=ALU.max), reads=gb, writes=gb)
                            S.op("dve", lambda e, lg_=lg_, m1=m1, k1=k1: e.tensor_scalar(out=k1, in0=lg_, scalar1=m1, scalar2=None, op0=ALU.is_equal), reads=gb, writes=gb)
                            S.op("dve", lambda e, lg_=lg_, k1=k1, l2=l2: e.scalar_tensor_tensor(out=l2, in0=k1, scalar=-1e30, in1=lg_, op0=ALU.mult, op1=ALU.add), reads=gb, writes=gb)
                            S.op("dve", lambda e, l2=l2, m2=m2: e.tensor_reduce(out=m2, in_=l2, axis=AX.X, op=ALU.max), reads=gb, writes=gb)
                            S.op("dve", lambda e, l2=l2, m2=m2, k2=k2: e.tensor_scalar(out=k2, in0=l2, scalar1=m2, scalar2=None, op0=ALU.is_equal), reads=gb, writes=gb)
                            S.op("dve", lambda e, dd=dd, m1=m1, m2=m2: e.tensor_tensor(out=dd, in0=m2, in1=m1, op=ALU.subtract), reads=gb, writes=gb)
                            S.op("act", lambda e, dd=dd, ee=ee: e.activation(out=ee, in_=dd, func=AF.Exp), reads=gb, writes=gb)
                            S.op("dve", lambda e, ee=ee, w1=w1: e.tensor_scalar_add(out=w1, in0=ee, scalar1=1.0), reads=gb, writes=gb)
                            S.op("dve", lambda e, w1=w1: e.reciprocal(out=w1, in_=w1), reads=gb, writes=gb)
                            S.op("dve", lambda e, ee=ee, w1=w1, w2=w2: e.tensor_tensor(out=w2, in0=ee, in1=w1, op=ALU.mult), reads=gb, writes=gb)
                            S.op("dve", lambda e, k1=k1, w1=w1: e.tensor_scalar(out=k1, in0=k1, scalar1=w1, scalar2=None, op0=ALU.mult), reads=gb, writes=gb)
                            S.op("dve", lambda e, k1=k1, k2=k2, w2=w2, ti=ti: e.scalar_tensor_tensor(out=gates.h[:, ti, :], in0=k2, scalar=w2, in1=k1, op0=ALU.mult, op1=ALU.add), reads=gb, writes=[gates_b[ti]])
                    for ui, (e_, c0, ncu) in enumerate(units):
                        us = uctr % 2
                        uctr += 1
                        wgs = wg_src(e_).rearrange("(k p) n -> p k n", p=128)
                        wus = wu_src(e_).rearrange("(k p) n -> p k n", p=128)
                        wds = wd_src(e_)[c0 * 128:(c0 + ncu) * 128, :].rearrange("(c p) n -> p c n", p=128)
                        S.op("pool", lambda e, us=us, wgs=wgs, c0=c0, ncu=ncu: e.dma_start(out=WG[us].h[:, :, 0:ncu * 128], in_=wgs[:, :, c0 * 128:(c0 + ncu) * 128]), writes=[WG[us].b], dma_key=WG[us].b)
                        S.op("pool", lambda e, us=us, wus=wus, c0=c0, ncu=ncu: e.dma_start(out=WU[us].h[:, :, 0:ncu * 128], in_=wus[:, :, c0 * 128:(c0 + ncu) * 128]), writes=[WU[us].b], dma_key=WU[us].b)
                        S.op("pool", lambda e, us=us, wds=wds, ncu=ncu: e.dma_start(out=WD[us].h[:, 0:ncu, :], in_=wds), writes=[WD[us].b], dma_key=WD[us].b)
                        hd = hid[us]
                        for c in range(ncu):
                            pg = psGa[c % 2]
                            pu = psUa[c % 2]
                            for kc in range(8):
                                S.op("pe", lambda e, c=c, kc=kc, us=us, gs=gs, pg=pg, N=N: e.matmul(pg.h[:, 0:N], lhsT=WG[us].h[:, kc, c * 128:(c + 1) * 128], rhs=tT[gs].h[:, kc, 0:N], start=(kc == 0), stop=(kc == 7)), reads=[WG[us].b, tT[gs].b], writes=[pg.b])
                            for kc in range(8):
                                S.op("pe", lambda e, c=c, kc=kc, us=us, gs=gs, pu=pu, N=N: e.matmul(pu.h[:, 0:N], lhsT=WU[us].h[:, kc, c * 128:(c + 1) * 128], rhs=tT[gs].h[:, kc, 0:N], start=(kc == 0), stop=(kc == 7)), reads=[WU[us].b, tT[gs].b], writes=[pu.b])
                            sg_ = sg[c % 2]
                            S.op("act", lambda e, pg=pg, sg_=sg_, N=N: e.activation(out=sg_.h[:, 0:N], in_=pg.h[:, 0:N], func=AF.Silu), reads=[pg.b], writes=[sg_.b])
                            S.op("dve", lambda e, c=c, pu=pu, sg_=sg_, hd=hd, N=N: e.tensor_tensor(out=hd.h[:, c, 0:N], in0=pu.h[:, 0:N], in1=sg_.h[:, 0:N], op=ALU.mult), reads=[pu.b, sg_.b], writes=[hd.b])
                        for ti, t in enumerate(grp):
                            for nh in range(2):
                                py = psY[yctr % 2]
                                yctr += 1
                                for c in range(ncu):
                                    S.op("pe", lambda e, c=c, ti=ti, nh=nh, us=us, hd=hd, py=py, ncu=ncu: e.matmul(py.h[:], lhsT=hd.h[:, c, ti * 128:(ti + 1) * 128], rhs=WD[us].h[:, c, nh * 512:(nh + 1) * 512], start=(c == 0), stop=(c == ncu - 1)), reads=[hd.b, WD[us].b], writes=[py.b])
                                ao = acc.h[:, ti, nh * 512:(nh + 1) * 512]
                                eng_ = "dve" if nh == 0 else "pool"
                                if moe:
                                    gsc = gates.h[:, ti, e_:e_ + 1]
                                    if ui == 0:
                                        S.op("dve", lambda e, ao=ao, py=py, gsc=gsc: e.tensor_scalar(out=ao, in0=py.h[:], scalar1=gsc, scalar2=None, op0=ALU.mult), reads=[py.b, gates_b[ti]], writes=[acc_b[ti]])
                                    else:
                                        S.op("dve", lambda e, ao=ao, py=py, gsc=gsc: e.scalar_tensor_tensor(out=ao, in0=py.h[:], scalar=gsc, in1=ao, op0=ALU.mult, op1=ALU.add), reads=[py.b, gates_b[ti], acc_b[ti]], writes=[acc_b[ti]])
                                else:
                                    if ui == 0:
                                        S.op("act", lambda e, ao=ao, py=py: e.copy(out=ao, in_=py.h[:]), reads=[py.b], writes=[acc_b[ti]])
                                    else:
                                        S.op("dve", lambda e, ao=ao, py=py: e.tensor_tensor(out=ao, in0=py.h[:], in1=ao, op=ALU.add), reads=[py.b, acc_b[ti]], writes=[acc_b[ti]])
                    for ti, t in enumerate(grp):
                        gidx = 3 if r == 1 else 1
                        av = acc.h[:, ti, :]
                        S.op("pool", lambda e, av=av, gidx=gidx: e.tensor_tensor(out=av, in0=av, in1=Gt[gidx].h[:], op=ALU.mult), reads=[acc_b[ti], Gt[gidx].b], writes=[acc_b[ti]])
                        if last:
                            dst, dstb = out[(t - NCT) * 128:(t - NCT + 1) * 128, :], out_b[t]
                        else:
                            dst, dstb = xs2[t * 128:(t + 1) * 128, :], xs2_b[t]
                        emit_ln_epilogue(S, T_view(av, acc_b[ti]), T_view(XG[gs].h[:, ti, :], XG_b[gs][ti]), LNt[2], LNt[3], st6, mv, epsln, cc, dst, dstb)
                S.emit()
    return nc


class T_view:
    def __init__(self, ap, b):
        self.ap = ap
        self.b = b


def _ap(x):
    return x.ap if isinstance(x, T_view) else x.h[:]


def emit_ln_epilogue(S, ta, xr, lng, lnb, st6, mv, epsc, cc, dst, dstb):
    a = _ap(ta)
    x = _ap(xr)
    S.op("pool", lambda e: e.tensor_tensor(out=a, in0=x, in1=a, op=ALU.add), reads=[xr.b, ta.b], writes=[ta.b])
    for c in range(2):
        S.op("dve", lambda e, c=c: e.bn_stats(out=st6.h[:, c, :], in_=a[:, c * 512:(c + 1) * 512]), reads=[ta.b], writes=[st6.b])
    S.op("dve", lambda e: e.bn_aggr(out=mv.h[:, 0:2], in_=st6.h[:]), reads=[st6.b], writes=[mv.b])
    S.op("act", lambda e: e.activation(out=mv.h[:, 2:3], in_=mv.h[:, 1:2], func=AF.Ln, bias=epsc, scale=1.0), reads=[mv.b, cc.b], writes=[mv.b])
    S.op("act", lambda e: e.activation(out=mv.h[:, 2:3], in_=mv.h[:, 2:3], func=AF.Exp, scale=-0.5), reads=[mv.b], writes=[mv.b])
    S.op("dve", lambda e: e.scalar_tensor_tensor(out=mv.h[:, 3:4], in0=mv.h[:, 0:1], scalar=-1.0, in1=mv.h[:, 2:3], op0=ALU.mult, op1=ALU.mult), reads=[mv.b], writes=[mv.b])
    S.op("act", lambda e: e.activation(out=a, in_=a, func=AF.Identity, bias=mv.h[:, 3:4], scale=mv.h[:, 2:3]), reads=[ta.b, mv.b], writes=[ta.b])
    S.op("pool", lambda e: e.tensor_tensor(out=a, in0=a, in1=lng.h[:], op=ALU.mult), reads=[ta.b, lng.b], writes=[ta.b])
    S.op("pool", lambda e: e.tensor_tensor(out=a, in0=a, in1=lnb.h[:], op=ALU.add), reads=[ta.b, lnb.b], writes=[ta.b])
    S.op("sp", lambda e: e.dma_start(out=dst, in_=a), reads=[ta.b], writes=[dstb], dma_key=ta.b)


_CACHE = {}


def prep_shared(inp):
    f = lambda a: np.ascontiguousarray(np.asarray(a, dtype=np.float32))
    perm = win_perm()
    sh = {}
    sh["w_mod"] = f(inp["w_mod"])
    bm = f(inp["b_mod"])
    sh["b_modT"] = f(bm.reshape(2, 48, 128).transpose(0, 2, 1))
    sh["b_modr"] = f(bm.reshape(2, 1, 6 * D))
    sh["w_in"] = f(np.asarray(inp["w_in"])[:, :, perm])
    sh["sink"] = f(np.asarray(inp["attn_sink"]).reshape(2, 1, 8))
    sh["pool_w"] = f(inp["pool_w"])
    sh["pool_scale"] = f(np.asarray(inp["pool_scale"]).reshape(2, 1, 256))
    lf = np.asarray(inp["ret_log_decay_fwd"], dtype=np.float32)
    lb = np.asarray(inp["ret_log_decay_bwd"], dtype=np.float32)
    sh["lgp"] = f(np.stack([np.repeat(lf, 32, axis=1), np.repeat(lb, 32, axis=1)], axis=2))
    sh["lgrow"] = f(np.concatenate([np.repeat(lf, 32, axis=1), np.repeat(lb, 32, axis=1)], axis=1).reshape(2, 1, 256))
    sh["lgb"] = f(np.concatenate([lf, lb], axis=1).reshape(2, 1, 8))
    sh["w_out"] = f(inp["w_out"])
    sh["lnp"] = f(np.stack([inp["ln1_g"], inp["ln1_b"], inp["ln2_g"], inp["ln2_b"]], axis=1))
    sh["ffn_wg"] = f(inp["ffn_w_gate"])
    sh["ffn_wu"] = f(inp["ffn_w_up"])
    sh["ffn_wd"] = f(inp["ffn_w_down"])
    sh["router"] = f(np.asarray(inp["moe_router"])[0])
    def unit_layout(w):
        w = np.asarray(w, dtype=np.float32)[0]
        o = np.zeros((NEXP, 4, 128, 8, 768), np.float32)
        wv = w.reshape(NEXP, 8, 128, FF)
        c0 = 0
        for u, ncu in enumerate((6, 6, 5, 5)):
            o[:, u, :, :, 0:ncu * 128] = wv[:, :, :, c0 * 128:(c0 + ncu) * 128].transpose(0, 2, 1, 3)
            c0 += ncu
        return o.reshape(NEXP * 4 * 128, 8 * 768)

    def down_layout(w):
        w = np.asarray(w, dtype=np.float32)[0]
        o = np.zeros((NEXP, 4, 128, 6, D), np.float32)
        wv = w.reshape(NEXP, NCH, 128, D)
        c0 = 0
        for u, ncu in enumerate((6, 6, 5, 5)):
            o[:, u, :, 0:ncu, :] = wv[:, c0:c0 + ncu, :, :].transpose(0, 2, 1, 3)
            c0 += ncu
        return o.reshape(NEXP * 4 * 128, 6 * D)
    sh["moe_wg"] = unit_layout(inp["moe_w_gate"])
    sh["moe_wu"] = unit_layout(inp["moe_w_up"])
    sh["moe_wd"] = down_layout(inp["moe_w_down"])
    sh["consts"] = make_consts()
    sh["ropecs"] = make_rope()
    sh["rconst"] = make_rconst()
    sh["slot_init"] = make_slot_init()
    return sh


def prep_core(inp, b):
    x = np.asarray(inp["x"], dtype=np.float32)
    ctx = np.asarray(inp["ctx"], dtype=np.float32)
    c = np.asarray(inp["c"], dtype=np.float32)
    cc = np.asarray(inp["c_ctx"], dtype=np.float32)
    d = {}
    d["xall"] = np.ascontiguousarray(np.concatenate([ctx[b], x[b]], axis=0))
    d["cvec"] = np.ascontiguousarray(np.concatenate([c[b].reshape(8, 128).T, cc.reshape(8, 128).T], axis=1))
    return d


def kernel(**inputs):
    n = 8
    if "nc" not in _CACHE:
        _CACHE["nc"] = build()
    nc = _CACHE["nc"]
    sh = prep_shared(inputs)
    in_maps = []
    for b in range(n):
        m = dict(sh)
        m.update(prep_core(inputs, b))
        in_maps.append(m)
    res = run_bass_kernel_spmd(nc, in_maps, core_ids=list(range(n)))
    return np.stack([np.asarray(r["out"]) for r in res.results], axis=0).astype(np.float32)
```

```python
import contextlib
import math
import numpy as np
import concourse.bass as bass
import concourse.mybir as mybir
from concourse.bass_utils import run_bass_kernel_spmd

F32 = mybir.dt.float32
BF16 = mybir.dt.bfloat16
AF = mybir.ActivationFunctionType
ALU = mybir.AluOpType
AX = mybir.AxisListType

SAME_ENGINE_SYNC = "raw"
ENGS = ["pe", "act", "dve", "pool", "sp"]

D = 1024
L = 4096
LC = 256
NT = (L + LC) // 128
NCT = LC // 128
FF = 2816
NCH = FF // 128
NEXP = 8
ALPHA = 4.0 ** 0.25
EPS = 1e-5
RSCALE = 32 ** -0.5
NEG = -30000.0


_GS = {}


class Buf:
    __slots__ = ("name", "lw", "rd", "last_dma", "sem", "cnt", "doff")

    def __init__(self, name):
        self.name = name
        self.lw = None
        self.rd = []
        self.last_dma = None
        self.sem = None
        self.cnt = 0


class Op:
    __slots__ = ("eng", "fn", "deps", "signal", "val", "is_dma", "idx", "semkey", "raw")


class Sched:
    def __init__(self, nc, name="p"):
        self.nc = nc
        self.name = name
        self.ops = {e: [] for e in ENGS}
        self.dma_keys = []
        self.bufs = set()

    def op(self, eng, fn, reads=(), writes=(), dma_key=None):
        o = Op()
        o.eng = eng
        o.fn = fn
        o.is_dma = dma_key is not None
        o.signal = False
        o.val = None
        o.semkey = dma_key
        deps = []
        raw = set()
        for b in reads:
            if b.lw is not None:
                deps.append(b.lw)
                raw.add(id(b.lw))
        for b in writes:
            if b.lw is not None:
                deps.append(b.lw)
            deps.extend(b.rd)
        o.raw = raw
        if o.is_dma:
            if dma_key.last_dma is not None:
                deps.append(dma_key.last_dma)
            dma_key.last_dma = o
            if dma_key.sem is None:
                dma_key.sem = True
                self.dma_keys.append(dma_key)
        o.deps = [d for d in deps if d is not o]
        for b in reads:
            self.bufs.add(b)
        for b in writes:
            self.bufs.add(b)
        for b in reads:
            if not o.is_dma:
                b.rd = [r for r in b.rd if r.is_dma or r.eng != eng]
            b.rd.append(o)
        for b in writes:
            b.lw = o
            b.rd = []
        o.idx = len(self.ops[eng])
        self.ops[eng].append(o)
        return o

    def emit(self):
        nc = self.nc
        import os as _os2
        _only = _os2.environ.get("K_ONLY")
        if _only and not self.name.endswith(_only):
            for k in self.dma_keys:
                k.sem = None; k.cnt = 0; k.last_dma = None
            for b in self.bufs:
                b.lw = None; b.rd = []
            return
        last = {}
        for e in ENGS:
            for o in reversed(self.ops[e]):
                if o.fn is not None and not o.is_dma:
                    o.signal = True
                    last[e] = o
                    break
        for e in ENGS:
            for o in self.ops[e]:
                nd = []
                for d in o.deps:
                    if d.is_dma:
                        nd.append(d)
                        continue
                    if d.eng == o.eng and not o.is_dma:
                        if d.eng == "pe" or not SAME_ENGINE_SYNC:
                            continue
                        if SAME_ENGINE_SYNC == "raw" and id(d) not in o.raw:
                            continue
                    d.signal = True
                    nd.append(d)
                o.deps = nd
        for e in ENGS:
            c = 0
            for o in self.ops[e]:
                if o.is_dma:
                    o.semkey.cnt += 16
                    o.val = o.semkey.cnt
                elif o.signal:
                    c += 1
                    o.val = c
        import os as _os
        if _os.environ.get("K_DBG"):
            print(self.name, {e: (len(self.ops[e]), max([o.val or 0 for o in self.ops[e] if not o.is_dma] + [0])) for e in ENGS}, "dma", len(self.dma_keys), max([k.cnt for k in self.dma_keys] + [0]), flush=True)
        gs = _GS
        if gs.get("nc") is not nc:
            gs.clear()
            gs["nc"] = nc
            gs["esem"] = {e: nc.semaphore("g_s_" + e).__enter__() for e in ENGS}
            gs["eoff"] = {e: 0 for e in ENGS}
            gs["dsem"] = []
            gs["doff"] = []
        kind = {}
        for e in ENGS:
            for o in self.ops[e]:
                if o.is_dma and id(o.semkey) not in kind:
                    kind[id(o.semkey)] = 1 if e == "pool" else 0
        self.dma_keys.sort(key=lambda k: kind.get(id(k), 0))
        npool = sum(1 for k in self.dma_keys if kind.get(id(k), 0) == 1)
        nsp = len(self.dma_keys) - npool
        for nm, need in (("dsem_sp", nsp), ("dsem_pl", npool)):
            gs.setdefault(nm, [])
            gs.setdefault(nm + "_off", [])
            while len(gs[nm]) < need:
                gs[nm].append(nc.semaphore("g_%s%d" % (nm, len(gs[nm]))).__enter__())
                gs[nm + "_off"].append(0)
        gs["dsem"] = gs["dsem_sp"][:nsp] + gs["dsem_pl"][:npool]
        gs["doff"] = gs["dsem_sp_off"][:nsp] + gs["dsem_pl_off"][:npool]
        eoff = gs["eoff"]
        for e in ENGS:
            for o in self.ops[e]:
                if o.is_dma:
                    continue
                if o.val is not None:
                    o.val += eoff[e]
        for i, k in enumerate(self.dma_keys):
            k.doff = gs["doff"][i]
        for e in ENGS:
            for o in self.ops[e]:
                if o.is_dma:
                    o.val += o.semkey.doff
        for k in self.dma_keys:
            k.cnt += k.doff
        with contextlib.ExitStack() as st:
            esem = gs["esem"]
            for i, k in enumerate(self.dma_keys):
                k.sem = gs["dsem"][i]
            block = st.enter_context(nc.Block())

            def run(e, engobj):
                known = {}
                for o in self.ops[e]:
                    for d in o.deps:
                        sem = d.semkey.sem if d.is_dma else esem[d.eng]
                        key = id(sem)
                        if known.get(key, 0) < d.val:
                            engobj.wait_ge(sem, d.val)
                            known[key] = d.val
                    if o.fn is None:
                        continue
                    ins = o.fn(engobj)
                    if o.is_dma:
                        ins.then_inc(o.semkey.sem, 16)
                    elif o.signal:
                        ins.then_inc(esem[e], 1)
                for f in ENGS:
                    if f in last and known.get(id(esem[f]), 0) < last[f].val:
                        engobj.wait_ge(esem[f], last[f].val)
                for k in self.dma_keys:
                    if known.get(id(k.sem), 0) < k.cnt:
                        engobj.wait_ge(k.sem, k.cnt)

            @block.tensor
            def _(t):
                run("pe", t)

            @block.scalar
            def _(t):
                run("act", t)

            @block.vector
            def _(t):
                run("dve", t)

            @block.gpsimd
            def _(t):
                run("pool", t)

            @block.sync
            def _(t):
                run("sp", t)
        for e in ENGS:
            if e in last:
                gs["eoff"][e] = last[e].val
        for i, k in enumerate(self.dma_keys):
            if i < nsp:
                gs["dsem_sp_off"][i] = k.cnt
            else:
                gs["dsem_pl_off"][i - nsp] = k.cnt
        for k in self.dma_keys:
            k.sem = None
            k.cnt = 0
            k.last_dma = None
        for b in self.bufs:
            b.lw = None
            b.rd = []


class T:
    def __init__(self, h, name):
        self.h = h
        self.b = Buf(name)


C_MPREV, C_MNEXT = 0, 512
C_POOLA = 1024
C_R1 = C_POOLA + 20 * 128
C_M1 = C_R1 + 128
C_R2 = C_M1 + 128
C_M2 = C_R2 + 128
C_NP1 = C_M2 + 128
C_NB = C_NP1 + 128
C_MASKS = C_NB + 128
C_MASKQ = C_MASKS + 256
C_IDENT = C_MASKQ + 512
C_COLS = C_IDENT + 128
C_TOT = C_COLS + 8
POOL_WINDOWS = (2, 4, 8, 16)


def make_consts():
    c = np.zeros((128, C_TOT), np.float32)
    kl = np.arange(128)[:, None]
    ql = np.arange(128)[None, :]
    mprev = np.where(ql <= kl, 0.0, NEG).astype(np.float32)
    mnext = np.where(kl <= ql, 0.0, NEG).astype(np.float32)
    c[:, C_MPREV:C_MPREV + 512] = np.tile(mprev, (1, 4))
    c[:, C_MNEXT:C_MNEXT + 512] = np.tile(mnext, (1, 4))
    Ls = 384
    for g, w in enumerate(POOL_WINDOWS):
        t = np.arange(Ls)
        lo = np.clip(t - w // 2, 0, Ls)
        hi = np.clip(t + w - w // 2, 0, Ls)
        A = np.zeros((Ls, Ls), np.float64)
        for i in range(Ls):
            A[i, lo[i]:hi[i]] = 1.0 / (hi[i] - lo[i])
        A -= np.eye(Ls)
        blk = lambda ti, si: A[ti * 128:(ti + 1) * 128, si * 128:(si + 1) * 128].T
        mats = [blk(0, 0), blk(1, 1), blk(2, 2), blk(1, 0), blk(1, 2)]
        for v, m in enumerate(mats):
            o = C_POOLA + (g * 5 + v) * 128
            c[:, o:o + 128] = m
    m = np.arange(128)[:, None].astype(np.float64)
    n = np.arange(128)[None, :].astype(np.float64)
    c[:, C_R1:C_R1 + 128] = np.maximum(n - m, 0)
    c[:, C_M1:C_M1 + 128] = (n >= m) * RSCALE
    c[:, C_R2:C_R2 + 128] = np.maximum(m - n, 0)
    c[:, C_M2:C_M2 + 128] = (m >= n) * RSCALE
    c[:, C_NP1:C_NP1 + 128] = np.broadcast_to(n + 1, (128, 128))
    c[:, C_NB:C_NB + 128] = np.broadcast_to(128 - n, (128, 128))
    p = np.arange(128)[:, None]
    c[:, C_MASKS:C_MASKS + 256] = (p // 32 == np.arange(256)[None, :] // 64)
    c[:, C_MASKQ:C_MASKQ + 512] = (p // 32 == np.arange(512)[None, :] // 128)
    c[:, C_IDENT:C_IDENT + 128] = np.eye(128)
    c[:, C_COLS + 0] = 127 - np.arange(128)
    c[:, C_COLS + 1] = np.arange(128)
    c[:, C_COLS + 2] = EPS
    c[:, C_COLS + 3] = math.log(RSCALE)
    c[:, C_COLS + 4] = EPS / (ALPHA * ALPHA)
    return c


NSLOT = 24 * 512
RC_TOK, RC_BG, RC_BD, RC_U, RC_ONES = 0, 32, 64, 86, 214
RC_TOT = 342


def make_rconst():
    c = np.zeros((128, RC_TOT), np.float32)
    p = np.arange(128)[:, None]
    c[:, RC_TOK:RC_TOK + 32] = p + 128 * np.arange(32)[None, :] + NCT * 128
    c[:, RC_BG:RC_BG + 32] = p + 128 * (np.arange(32)[None, :] % 4)
    c[:, RC_BD:RC_BD + 22] = p + 128 * np.arange(22)[None, :]
    c[:, RC_U:RC_U + 128] = (p < np.arange(128)[None, :])
    c[:, RC_ONES:RC_ONES + 128] = 1.0
    return c


def make_slot_init():
    a = np.zeros((NSLOT, 4), np.float32)
    a[:, 2] = 2 * L + (np.arange(NSLOT) % 128)
    return a


def make_rope():
    pos = np.arange(L)
    row = (pos // 64).astype(np.float64)
    col = (pos % 64).astype(np.float64)
    inv = 10000.0 ** (-np.arange(16, dtype=np.float64) / 16)
    ar = row[:, None] * inv[None, :]
    ac = col[:, None] * inv[None, :]
    C = np.concatenate([np.cos(ar), np.cos(ar), np.cos(ac), np.cos(ac)], 1)
    S = np.concatenate([-np.sin(ar), np.sin(ar), -np.sin(ac), np.sin(ac)], 1)
    return np.concatenate([C, S], 1).astype(np.float32)


def win_perm():
    q = []
    for j in range(4):
        q += list(range(j * 64, (j + 1) * 64)) + list(range((4 + j) * 64, (5 + j) * 64))
    k = list(range(512, 640)); v = list(range(640, 768)); u = list(range(768, 1024))
    rq = list(range(1024, 1152)); rk = list(range(1152, 1280)); rv = list(range(1280, 1536)); rg = list(range(1536, 1792))
    return np.array(q + k + rq + rk + v + u + rv + rg)


def build(n_layers=2, moe_experts=NEXP, dbg=False, stop=None):
    nc = bass.Bass("TRN2", target_bir_lowering=False)

    def din(name, shape, dt=F32):
        return nc.dram_tensor(name, list(shape), dt, kind="ExternalInput").ap()

    xall = din("xall", [NT * 128, D])
    cvec = din("cvec", [128, 16])
    w_mod = din("w_mod", [2, D, 6 * D])
    b_modT = din("b_modT", [2, 128, 48])
    b_modr = din("b_modr", [2, 1, 6 * D])
    w_in = din("w_in", [2, D, 1792])
    sink = din("sink", [2, 1, 8])
    pool_w = din("pool_w", [2, 4, 64, 64])
    pool_scale = din("pool_scale", [2, 1, 256])
    lgp = din("lgp", [2, 128, 2])
    lgrow = din("lgrow", [2, 1, 256])
    lgb = din("lgb", [2, 1, 8])
    w_out = din("w_out", [2, D, D])
    lnp = din("lnp", [2, 4, D])
    ffn_wg = din("ffn_wg", [1, D, FF])
    ffn_wu = din("ffn_wu", [1, D, FF])
    ffn_wd = din("ffn_wd", [1, FF, D])
    if n_layers > 1:
        router = din("router", [D, NEXP])
        moe_wg = din("moe_wg", [NEXP * 4 * 128, 8 * 768])
        moe_wu = din("moe_wu", [NEXP * 4 * 128, 8 * 768])
        moe_wd = din("moe_wd", [NEXP * 4 * 128, 6 * D])
    import os as _os
    SKIP = set(_os.environ.get("K_SKIP", "").split(","))
    SPARSE = n_layers > 1 and _os.environ.get("K_DENSE", "") == ""
    if n_layers > 1:
        rconst = din("rconst", [128, RC_TOT])
        slot_init = din("slot_init", [NSLOT, 4])
        slotinfo = nc.dram_tensor("slotinfo", [NSLOT, 4], F32).ap()
        Fsc = nc.dram_tensor("Fsc", [2 * L + 128, D], F32).ap()
        wg2d = nc.dram_tensor("wg16", [NEXP * 4 * 128, 8 * 768], BF16).ap()
        wu2d = nc.dram_tensor("wu16", [NEXP * 4 * 128, 8 * 768], BF16).ap()
        wd2d = nc.dram_tensor("wd16", [NEXP * 4 * 128, 6 * D], BF16).ap()
    consts = din("consts", [128, C_TOT])
    ropecs = din("ropecs", [L, 128])
    out = nc.dram_tensor("out", [L, D], F32, kind="ExternalOutput").ap()
    kind = "ExternalOutput" if dbg else "Internal"
    xs1 = nc.dram_tensor("xs1", [NT * 128, D], F32, kind=kind).ap()
    xs2 = nc.dram_tensor("xs2", [NT * 128, D], F32, kind=kind).ap()
    qbun = nc.dram_tensor("qbun", [NT, 128, 1024], BF16).ap()
    gscr = nc.dram_tensor("gscr", [2, 2048], F32).ap()
    if dbg:
        dbg_modT = nc.dram_tensor("dbg_modT", [128, 96], F32, kind="ExternalOutput").ap()

    xs1_b = [Buf("xs1_%d" % t) for t in range(NT)]
    xs2_b = [Buf("xs2_%d" % t) for t in range(NT)]
    qbun_b = [Buf("qbun_%d" % t) for t in range(NT)]
    out_b = [Buf("out_%d" % t) for t in range(NT)]
    gscr_b = Buf("gscr")

    G = contextlib.ExitStack()
    with G:
        cnt = [0]

        def sb(st, name, shape, dt):
            cnt[0] += 1
            return T(st.enter_context(nc.sbuf_tensor("%s_%d" % (name, cnt[0]), list(shape), dt)), name)

        def ps(st, name, shape, dt):
            cnt[0] += 1
            return T(st.enter_context(nc.psum_tensor("%s_%d" % (name, cnt[0]), list(shape), dt)), name)

        cb = sb(G, "cb", [128, C_R1], BF16)
        cf = sb(G, "cf", [128, C_MASKS - C_R1], F32)
        cm = sb(G, "cm", [128, 768 + 128], BF16)
        cc = sb(G, "cc", [128, 8], F32)
        ones_t = sb(G, "ones", [128, 8], F32)
        ident = cm.h[:, 768:896]
        maskS = cm.h[:, 0:256]
        maskQ = cm.h[:, 256:768]
        mprev = cb.h[:, C_MPREV:C_MPREV + 512]
        mnext = cb.h[:, C_MNEXT:C_MNEXT + 512]

        def PA(g, v):
            o = C_POOLA + (g * 5 + v) * 128
            return cb.h[:, o:o + 128]

        cfo = lambda c0: cf.h[:, c0 - C_R1:c0 - C_R1 + 128]
        KT_b = [Buf("KT%d" % t) for t in range(NT)]
        VX_b = [Buf("VX%d" % t) for t in range(NT)]
        UR_b = [Buf("UR%d" % t) for t in range(NT)]
        RKT_b = [Buf("RKT%d" % t) for t in range(NT)]
        SBD_b = [Buf("SBD%d" % t) for t in range(NT)]
        modT = sb(G, "modT", [128, 48, 2], F32)
        Gt = [sb(G, "G%d" % i, [128, D], F32) for i in range(4)]
        LNt = [sb(G, "LN%d" % i, [128, D], F32) for i in range(4)]
        DsumT = sb(G, "DsumT", [128, 512], F32)
        DFB = sb(G, "DFB", [128, 256], F32)
        WFB = sb(G, "WFB", [128, 256], F32)
        gC = sb(G, "gC", [128, 2], F32)
        esink = sb(G, "esink", [128, 8], F32)
        PW = sb(G, "PW", [64, 256], BF16)

        S = Sched(nc, "c0")
        S.op("pool", lambda e: e.dma_start(out=cb.h[:], in_=consts[:, 0:C_R1]), writes=[cb.b], dma_key=cb.b)
        S.op("pool", lambda e: e.dma_start(out=cm.h[:], in_=consts[:, C_MASKS:C_COLS]), writes=[cm.b], dma_key=cm.b)
        S.op("sp", lambda e: e.dma_start(out=cf.h[:], in_=consts[:, C_R1:C_MASKS]), writes=[cf.b], dma_key=cf.b)
        S.op("sp", lambda e: e.dma_start(out=cc.h[:], in_=consts[:, C_COLS:C_TOT]), writes=[cc.b], dma_key=cc.b)
        S.op("pool", lambda e: e.memset(ones_t.h[:], 1.0), writes=[ones_t.b])
        S.emit()
        c127m = cc.h[:, 0:1]
        cmcol = cc.h[:, 1:2]
        epsc = cc.h[:, 2:3]
        lnrs = cc.h[:, 3:4]
        epsln = cc.h[:, 4:5]

        for layer in range(n_layers):
            last = layer == n_layers - 1
            xsrc, xsrc_b = (xall, None) if layer == 0 else (xs2, xs2_b)
            xdst, xdst_b = (xs2, xs2_b) if not last else (None, None)
            p2_tiles = list(range(NT)) if not last else list(range(NCT, NT))

            def xsrc_bufs(t):
                return [xsrc_b[t]] if xsrc_b is not None else []

            with contextlib.ExitStack() as P:
                S = Sched(nc, "L%dp0" % layer)
                cv = sb(P, "cv", [128, 16], F32)
                scv = sb(P, "scv", [128, 8, 2], BF16)
                wmb = [sb(P, "wmb%d" % i, [128, 8, 512], BF16) for i in range(2)]
                bmT = sb(P, "bmT", [128, 48], F32)
                brow = sb(P, "brow", [2, 2048], F32)
                grow = sb(P, "grow", [2, 2048], F32)
                lgbT = sb(P, "lgbT", [128, 8], F32)
                lgpT = sb(P, "lgpT", [128, 2], F32)
                lgrT = sb(P, "lgrT", [128, 256], F32)
                sinkb = sb(P, "sinkb", [128, 8], F32)
                pwf = sb(P, "pwf", [64, 4, 64], F32)
                pscb = sb(P, "pscb", [64, 256], F32)
                tmpA = sb(P, "tmpA", [128, 128], F32)
                tmpB = sb(P, "tmpB", [128, 128], F32)
                psM = ps(P, "psM", [128, 96], F32)
                psG = ps(P, "psG", [2, 2048], F32)
                S.op("sp", lambda e: e.dma_start(out=cv.h[:], in_=cvec), writes=[cv.b], dma_key=cv.b)
                S.op("sp", lambda e: e.dma_start(out=bmT.h[:], in_=b_modT[layer]), writes=[bmT.b], dma_key=bmT.b)
                for r in range(2):
                    S.op("sp", lambda e, r=r: e.dma_start(out=brow.h[r:r + 1, 0:1024], in_=b_modr[layer, :, 2048:3072]), writes=[brow.b], dma_key=brow.b)
                    S.op("sp", lambda e, r=r: e.dma_start(out=brow.h[r:r + 1, 1024:2048], in_=b_modr[layer, :, 5120:6144]), writes=[brow.b], dma_key=brow.b)
                S.op("sp", lambda e: e.dma_start(out=lgbT.h[:], in_=lgb[layer].partition_broadcast(128)), writes=[lgbT.b], dma_key=lgbT.b)
                S.op("sp", lambda e: e.dma_start(out=lgpT.h[:], in_=lgp[layer]), writes=[lgpT.b], dma_key=lgpT.b)
                S.op("sp", lambda e: e.dma_start(out=lgrT.h[:], in_=lgrow[layer].partition_broadcast(128)), writes=[lgrT.b], dma_key=lgrT.b)
                S.op("sp", lambda e: e.dma_start(out=sinkb.h[:], in_=sink[layer].partition_broadcast(128)), writes=[sinkb.b], dma_key=sinkb.b)
                S.op("sp", lambda e: e.dma_start(out=pwf.h[:], in_=pool_w[layer].rearrange("g c d -> c g d")), writes=[pwf.b], dma_key=pwf.b)
                S.op("sp", lambda e: e.dma_start(out=pscb.h[:], in_=pool_scale[layer].partition_broadcast(64)), writes=[pscb.b], dma_key=pscb.b)
                for i in range(4):
                    S.op("sp", lambda e, i=i: e.dma_start(out=LNt[i].h[:], in_=lnp[layer, i:i + 1, :].partition_broadcast(128)), writes=[LNt[i].b], dma_key=LNt[i].b)
                S.op("act", lambda e: e.activation(out=scv.h[:].rearrange("p k r -> p r k"), in_=cv.h[:].rearrange("p (r k) -> p r k", r=2), func=AF.Silu), reads=[cv.b], writes=[scv.b])
                wsrc = w_mod[layer].rearrange("(k p) n -> p k n", p=128)
                for blk in range(12):
                    wb_ = wmb[blk % 2]
                    S.op("pool", lambda e, blk=blk, wb_=wb_: e.dma_start(out=wb_.h[:], in_=wsrc[:, :, blk * 512:(blk + 1) * 512]), writes=[wb_.b], dma_key=wb_.b)
                    for fc in range(4):
                        j = blk * 4 + fc
                        for kc in range(8):
                            S.op("pe", lambda e, j=j, fc=fc, kc=kc, wb_=wb_: e.matmul(psM.h[:, 2 * j:2 * j + 2], lhsT=wb_.h[:, kc, fc * 128:(fc + 1) * 128], rhs=scv.h[:, kc, :], start=(kc == 0), stop=(kc == 7)),
                                 reads=[wb_.b, scv.b], writes=[psM.b])
                    if blk in (4, 5, 10, 11):
                        co = {4: 0, 5: 512, 10: 1024, 11: 1536}[blk]
                        for kc in range(8):
                            S.op("pe", lambda e, co=co, kc=kc, wb_=wb_: e.matmul(psG.h[:, co:co + 512], lhsT=scv.h[:, kc, :], rhs=wb_.h[:, kc, :], start=(kc == 0), stop=(kc == 7)),
                                 reads=[wb_.b, scv.b], writes=[psG.b])
                S.op("dve", lambda e: e.tensor_tensor(out=modT.h[:], in0=psM.h[:].rearrange("p (j r) -> p j r", r=2), in1=bmT.h[:].unsqueeze(2).to_broadcast([128, 48, 2]), op=ALU.add),
                     reads=[psM.b, bmT.b], writes=[modT.b])
                S.op("dve", lambda e: e.tensor_scalar_add(out=modT.h[:, 8:16, :], in0=modT.h[:, 8:16, :], scalar1=1.0), reads=[modT.b], writes=[modT.b])
                S.op("dve", lambda e: e.tensor_scalar_add(out=modT.h[:, 32:40, :], in0=modT.h[:, 32:40, :], scalar1=1.0), reads=[modT.b], writes=[modT.b])
                S.op("dve", lambda e: e.tensor_tensor(out=grow.h[:], in0=psG.h[:], in1=brow.h[:], op=ALU.add), reads=[psG.b, brow.b], writes=[grow.b])
                S.op("sp", lambda e: e.dma_start(out=gscr, in_=grow.h[:]), reads=[grow.b], writes=[gscr_b], dma_key=grow.b)
                for i in range(4):
                    r, w = i // 2, i % 2
                    S.op("sp", lambda e, i=i, r=r, w=w: e.dma_start(out=Gt[i].h[:], in_=gscr[r:r + 1, w * 1024:(w + 1) * 1024].partition_broadcast(128)), reads=[gscr_b], writes=[Gt[i].b], dma_key=Gt[i].b)
                    S.op("dve", lambda e, i=i: e.tensor_scalar_mul(out=Gt[i].h[:], in0=Gt[i].h[:], scalar1=1.0 / ALPHA), reads=[Gt[i].b], writes=[Gt[i].b])
                if dbg and layer == 0:
                    S.op("sp", lambda e: e.dma_start(out=dbg_modT, in_=modT.h[:].rearrange("p j r -> p (j r)")), reads=[modT.b], writes=[Buf("dbgm")], dma_key=modT.b)
                for h in range(4):
                    S.op("act", lambda e, h=h: e.activation(out=tmpA.h[:], in_=cfo(C_R1), func=AF.Exp, scale=lgbT.h[:, h:h + 1]), reads=[cf.b, lgbT.b], writes=[tmpA.b])
                    S.op("act", lambda e, h=h: e.activation(out=tmpB.h[:], in_=cfo(C_R2), func=AF.Exp, scale=lgbT.h[:, 4 + h:5 + h]), reads=[cf.b, lgbT.b], writes=[tmpB.b])
                    S.op("dve", lambda e: e.tensor_tensor(out=tmpA.h[:], in0=tmpA.h[:], in1=cfo(C_M1), op=ALU.mult), reads=[tmpA.b, cf.b], writes=[tmpA.b])
                    S.op("dve", lambda e: e.tensor_tensor(out=tmpB.h[:], in0=tmpB.h[:], in1=cfo(C_M2), op=ALU.mult), reads=[tmpB.b, cf.b], writes=[tmpB.b])
                    S.op("dve", lambda e, h=h: e.tensor_tensor(out=DsumT.h[:, h * 128:(h + 1) * 128], in0=tmpA.h[:], in1=tmpB.h[:], op=ALU.add), reads=[tmpA.b, tmpB.b], writes=[DsumT.b])
                S.op("act", lambda e: e.activation(out=DFB.h[:, 0:128], in_=cfo(C_NP1), func=AF.Exp, scale=lgpT.h[:, 0:1]), reads=[cf.b, lgpT.b], writes=[DFB.b])
                S.op("act", lambda e: e.activation(out=DFB.h[:, 128:256], in_=cfo(C_NB), func=AF.Exp, scale=lgpT.h[:, 1:2]), reads=[cf.b, lgpT.b], writes=[DFB.b])
                S.op("act", lambda e: e.activation(out=WFB.h[:, 0:128], in_=lgrT.h[:, 0:128], func=AF.Exp, scale=c127m, bias=lnrs), reads=[lgrT.b, cc.b], writes=[WFB.b])
                S.op("act", lambda e: e.activation(out=WFB.h[:, 128:256], in_=lgrT.h[:, 128:256], func=AF.Exp, scale=cmcol, bias=lnrs), reads=[lgrT.b, cc.b], writes=[WFB.b])
                S.op("act", lambda e: e.activation(out=gC.h[:], in_=lgpT.h[:], func=AF.Exp, scale=128.0), reads=[lgpT.b], writes=[gC.b])
                S.op("act", lambda e: e.activation(out=esink.h[:], in_=sinkb.h[:], func=AF.Exp), reads=[sinkb.b], writes=[esink.b])
                S.op("dve", lambda e: e.tensor_tensor(out=PW.h[:].rearrange("p (g d) -> p g d", g=4), in0=pwf.h[:], in1=pscb.h[:].rearrange("p (g d) -> p g d", g=4), op=ALU.mult), reads=[pwf.b, pscb.b], writes=[PW.b])
                S.emit()

            A1 = lambda kc, r: modT.h[:, 8 + kc, r:r + 1]
            SH1 = lambda kc, r: modT.h[:, 0 + kc, r:r + 1]
            A2 = lambda kc, r: modT.h[:, 32 + kc, r:r + 1]
            SH2 = lambda kc, r: modT.h[:, 24 + kc, r:r + 1]

            if stop == "p0":
                break
            M = contextlib.ExitStack()
            M.__enter__()
            KT = sb(M, "KT", [128, NT, 128], BF16)
            VX = sb(M, "VX", [128, NT, 2, 65], BF16)
            UR = sb(M, "UR", [128, NT, 512], BF16)
            RKT = sb(M, "RKT", [128, NT, 128], BF16)
            SBD = sb(M, "SBD", [128, NT, 256], BF16)
            with contextlib.ExitStack() as P:
                S = Sched(nc, "L%dp1" % layer)
                S.op("pool", lambda e: e.memset(VX.h[:], 1.0), writes=VX_b)
                win = sb(P, "win", [128, 8, 1792], BF16)
                xb = [sb(P, "xb%d" % i, [128, D], BF16) for i in range(2)]
                hT = [sb(P, "hT%d" % i, [128, 8, 128], BF16) for i in range(2)]
                rcs = [sb(P, "rcs%d" % i, [128, 128], F32) for i in range(2)]
                tmp1 = [sb(P, "tmp1_%d" % i, [128, 640], F32) for i in range(2)]
                tmp2 = [sb(P, "tmp2_%d" % i, [128, 640], F32) for i in range(2)]
                trin = [sb(P, "trin%d" % i, [128, 896], BF16) for i in range(2)]
                qst = [sb(P, "qst%d" % i, [128, 1024], BF16) for i in range(2)]
                psX = [ps(P, "psX%d" % i, [128, 8, 128], BF16) for i in range(2)]
                psP = [ps(P, "psP%d" % i, [128, 512], F32) for i in range(4)]
                psT2 = ps(P, "psT2", [128, 7, 128], BF16)
                psPall = None
                S.op("pool", lambda e: e.dma_start(out=win.h[:], in_=w_in[layer].rearrange("(k p) n -> p k n", p=128)), writes=[win.b], dma_key=win.b)
                for t in range(NT):
                    s = t % 2
                    r = 1 if t < NCT else 0
                    lat = t >= NCT
                    S.op("pool", lambda e, t=t, s=s: e.dma_start(out=xb[s].h[:], in_=xsrc[t * 128:(t + 1) * 128, :]), reads=xsrc_bufs(t), writes=[xb[s].b], dma_key=xb[s].b)
                    if lat:
                        S.op("sp", lambda e, t=t, s=s: e.dma_start(out=rcs[s].h[:], in_=ropecs[(t - NCT) * 128:(t - NCT + 1) * 128, :]), writes=[rcs[s].b], dma_key=rcs[s].b)
                    for kc in range(8):
                        S.op("pe", lambda e, kc=kc, s=s: e.transpose(out=psX[s].h[:, kc, :], in_=xb[s].h[:, kc * 128:(kc + 1) * 128], identity=ident), reads=[xb[s].b, cm.b], writes=[psX[s].b])
                    for kc in range(8):
                        if kc % 2 == 0:
                            S.op("act", lambda e, kc=kc, s=s, r=r: e.activation(out=hT[s].h[:, kc, :], in_=psX[s].h[:, kc, :], func=AF.Identity, bias=SH1(kc, r), scale=A1(kc, r)), reads=[psX[s].b, modT.b], writes=[hT[s].b])
                        else:
                            S.op("dve", lambda e, kc=kc, s=s, r=r: e.tensor_scalar(out=hT[s].h[:, kc, :], in0=psX[s].h[:, kc, :], scalar1=A1(kc, r), scalar2=SH1(kc, r), op0=ALU.mult, op1=ALU.add), reads=[psX[s].b, modT.b], writes=[hT[s].b])
                    for nb in range(4):
                        ncol = 512 if nb < 3 else 256
                        for kc in range(8):
                            S.op("pe", lambda e, nb=nb, kc=kc, s=s, ncol=ncol: e.matmul(psP[nb].h[:, 0:ncol], lhsT=hT[s].h[:, kc, :], rhs=win.h[:, kc, nb * 512:nb * 512 + ncol], start=(kc == 0), stop=(kc == 7)),
                                 reads=[hT[s].b, win.b], writes=[psP[nb].b])
                    if lat:
                        Cq = rcs[s].h[:, 0:64].unsqueeze(1).to_broadcast([128, 8, 64])
                        Ck = rcs[s].h[:, 0:64].unsqueeze(1).to_broadcast([128, 2, 64])
                        Sv = rcs[s].h[:, 64:128].rearrange("p (a b d) -> p a b d", a=2, b=2)
                        S.op("dve", lambda e, s=s, Cq=Cq: e.tensor_tensor(out=tmp1[s].h[:, 0:512].rearrange("p (h f) -> p h f", h=8), in0=psP[0].h[:].rearrange("p (h f) -> p h f", h=8), in1=Cq, op=ALU.mult), reads=[psP[0].b, rcs[s].b], writes=[tmp1[s].b])
                        S.op("dve", lambda e, s=s, Ck=Ck: e.tensor_tensor(out=tmp1[s].h[:, 512:640].rearrange("p (h f) -> p h f", h=2), in0=psP[1].h[:, 0:128].rearrange("p (h f) -> p h f", h=2), in1=Ck, op=ALU.mult), reads=[psP[1].b, rcs[s].b], writes=[tmp1[s].b])
                        for b_ in range(2):
                            S.op("dve", lambda e, s=s, b_=b_, Sv=Sv: e.tensor_tensor(
                                out=tmp2[s].h[:, 0:512].rearrange("p (h a b d) -> p h a b d", h=8, a=2, b=2)[:, :, :, b_, :],
                                in0=psP[0].h[:].rearrange("p (h a b d) -> p h a b d", h=8, a=2, b=2)[:, :, :, 1 - b_, :],
                                in1=Sv[:, :, b_, :].unsqueeze(1).to_broadcast([128, 8, 2, 16]), op=ALU.mult), reads=[psP[0].b, rcs[s].b], writes=[tmp2[s].b])
                            S.op("dve", lambda e, s=s, b_=b_, Sv=Sv: e.tensor_tensor(
                                out=tmp2[s].h[:, 512:640].rearrange("p (h a b d) -> p h a b d", h=2, a=2, b=2)[:, :, :, b_, :],
                                in0=psP[1].h[:, 0:128].rearrange("p (h a b d) -> p h a b d", h=2, a=2, b=2)[:, :, :, 1 - b_, :],
                                in1=Sv[:, :, b_, :].unsqueeze(1).to_broadcast([128, 2, 2, 16]), op=ALU.mult), reads=[psP[1].b, rcs[s].b], writes=[tmp2[s].b])
                        S.op("pool", lambda e, s=s: e.tensor_tensor(out=trin[s].h[:, 0:640], in0=tmp1[s].h[:], in1=tmp2[s].h[:], op=ALU.add), reads=[tmp1[s].b, tmp2[s].b], writes=[trin[s].b])
                    else:
                        S.op("act", lambda e, s=s: e.copy(out=trin[s].h[:, 0:512], in_=psP[0].h[:]), reads=[psP[0].b], writes=[trin[s].b])
                        S.op("act", lambda e, s=s: e.copy(out=trin[s].h[:, 512:640], in_=psP[1].h[:, 0:128]), reads=[psP[1].b], writes=[trin[s].b])
                    S.op("act", lambda e, s=s: e.copy(out=trin[s].h[:, 640:896], in_=psP[1].h[:, 128:384]), reads=[psP[1].b], writes=[trin[s].b])
                    S.op("act", lambda e, t=t: e.copy(out=VX.h[:, t, :, 0:64], in_=psP[1].h[:, 384:512].rearrange("p (k d) -> p k d", k=2)), reads=[psP[1].b], writes=[VX_b[t]])
                    S.op("act", lambda e, t=t: e.copy(out=RKT.h[:, t, :], in_=psP[1].h[:, 256:384]), reads=[psP[1].b], writes=[RKT_b[t]])
                    S.op("dve", lambda e, t=t: e.tensor_copy(out=UR.h[:, t, :], in_=psP[2].h[:]), reads=[psP[2].b], writes=[UR_b[t]])
                    S.op("act", lambda e, s=s: e.activation(out=qst[s].h[:, 768:1024], in_=psP[3].h[:, 0:256], func=AF.Silu), reads=[psP[3].b], writes=[qst[s].b])
                    for c in range(7):
                        S.op("pe", lambda e, c=c, s=s: e.transpose(out=psT2.h[:, c, :], in_=trin[s].h[:, c * 128:(c + 1) * 128], identity=ident), reads=[trin[s].b, cm.b], writes=[psT2.b])
                    S.op("dve", lambda e, s=s: e.tensor_copy(out=qst[s].h[:, 0:512], in_=psT2.h[:, 0:4, :].rearrange("p c n -> p (c n)")), reads=[psT2.b], writes=[qst[s].b])
                    S.op("act", lambda e, s=s: e.copy(out=qst[s].h[:, 512:768], in_=psT2.h[:, 5:7, :].rearrange("p c n -> p (c n)")), reads=[psT2.b], writes=[qst[s].b])
                    S.op("dve", lambda e, t=t: e.tensor_copy(out=KT.h[:, t, :], in_=psT2.h[:, 4, :]), reads=[psT2.b], writes=[KT_b[t]])
                    S.op("sp", lambda e, t=t, s=s: e.dma_start(out=qbun[t], in_=qst[s].h[:]), reads=[qst[s].b], writes=[qbun_b[t]], dma_key=qst[s].b)
                S.emit()

            if stop == "p1":
                M.__exit__(None, None, None)
                break
            with contextlib.ExitStack() as P:
                S = Sched(nc, "L%dp2" % layer)
                wout = sb(P, "wout", [128, 8, D], BF16)
                QB = [sb(P, "QB%d" % i, [128, 1024], BF16) for i in range(2)]
                XR = [sb(P, "XR%d" % i, [128, D], F32) for i in range(2)]
                PT = [sb(P, "PT%d" % i, [128, 512], BF16) for i in range(3)]
                mix = [sb(P, "mix%d" % i, [128, D], BF16) for i in range(2)]
                mixT = [sb(P, "mixT%d" % i, [128, 8, 128], BF16) for i in range(2)]
                den = sb(P, "den", [128, 4], F32)
                rec = sb(P, "rec", [128, 4], F32)
                plT = sb(P, "plT", [64, 512], BF16)
                qbd = [sb(P, "qbd%d" % i, [128, 512], BF16) for i in range(2)]
                PTr = [sb(P, "PTr%d" % i, [128, 512], BF16) for i in range(2)]
                qfb = [sb(P, "qfb%d" % i, [128, 256], BF16) for i in range(2)]
                kdf = [sb(P, "kdf%d" % i, [128, 128], BF16) for i in range(2)]
                SFs = sb(P, "SFs", [128, 256], F32)
                SBs = sb(P, "SBs", [128, 256], F32)
                SFbd = [sb(P, "SFbd%d" % i, [128, 256], BF16) for i in range(2)]
                sq = sb(P, "sq", [128, 256], F32)
                ro = sb(P, "ro", [128, 256], F32)
                cen = sb(P, "cen", [128, 256], F32)
                st8 = sb(P, "st8", [128, 24], F32)
                tA = [sb(P, "tA%d" % i, [128, D], F32) for i in range(2)]
                st6 = sb(P, "st6", [128, 2, 6], F32)
                mv = sb(P, "mv", [128, 4], F32)
                psS = [ps(P, "psS%d" % i, [128, 512], F32) for i in range(2)]
                psO = ps(P, "psO", [128, 512], F32)
                psY = [ps(P, "psY%d" % i, [128, 512], F32) for i in range(1)] * 2
                psMT = ps(P, "psMT", [128, 8, 128], BF16)
                psPL = ps(P, "psPL", [128, 512], F32)
                psMa = ps(P, "psMa", [128, 512], F32)
                psR = ps(P, "psR", [128, 512], F32)
                sctr = [0]

                def next_psS():
                    sctr[0] += 1
                    return psS[sctr[0] % 2]

                pctr = [0]

                def next_PT():
                    pctr[0] += 1
                    return PT[pctr[0] % 3]

                S.op("pool", lambda e: e.dma_start(out=wout.h[:], in_=w_out[layer].rearrange("(k p) n -> p k n", p=128)), writes=[wout.b], dma_key=wout.b)
                if layer == 0 and n_layers > 1 and SPARSE:
                    for src_, dst_ in ((moe_wg, wg2d), (moe_wu, wu2d), (moe_wd, wd2d)):
                        for ch in range(8):
                            S.op("pool", lambda e, src_=src_, dst_=dst_, ch=ch: e.dma_start(out=dst_[ch * 512:(ch + 1) * 512, :], in_=src_[ch * 512:(ch + 1) * 512, :]), writes=[], dma_key=Buf("cv"))
                S.op("pool", lambda e: e.memset(SBs.h[:], 0.0), writes=[SBs.b])
                S.op("pool", lambda e: e.memset(SFs.h[:], 0.0), writes=[SFs.b])
                S.op("pool", lambda e: e.memset(SFbd[0].h[:], 0.0), writes=[SFbd[0].b])
                border = [1, 0] + list(range(NT - 1, NCT - 1, -1))
                for i, t in enumerate(border):
                    S.op("pool", lambda e, t=t: e.tensor_tensor(out=SBD.h[:, t, :], in0=SBs.h[:], in1=maskS, op=ALU.mult), reads=[SBs.b, cm.b], writes=[SBD_b[t]])
                    if i == len(border) - 1:
                        break
                    if "bwd" in SKIP:
                        continue
                    kd = kdf[i % 2]
                    S.op("pool", lambda e, t=t, kd=kd: e.tensor_tensor(out=kd.h[:], in0=RKT.h[:, t, :], in1=WFB.h[:, 128:256], op=ALU.mult), reads=[RKT_b[t], WFB.b], writes=[kd.b])
                    pk = next_psS()
                    S.op("pe", lambda e, t=t, kd=kd, pk=pk: e.matmul(pk.h[:, 0:256], lhsT=kd.h[:], rhs=UR.h[:, t, 256:512], start=True, stop=True), reads=[kd.b, UR_b[t]], writes=[pk.b])
                    S.op("dve", lambda e, pk=pk: e.scalar_tensor_tensor(out=SBs.h[:], in0=SBs.h[:], scalar=gC.h[:, 1:2], in1=pk.h[:, 0:256], op0=ALU.mult, op1=ALU.add), reads=[SBs.b, gC.b, pk.b], writes=[SBs.b])
                class _Rec:
                    def __init__(self):
                        self.cur = None

                    def op(self, *a, **kw):
                        self.cur.append((a, kw))
                S_real = S
                S = _Rec()
                fronts, tails = {}, {}
                sf_cur = 0
                for t in range(NT):
                    S.cur = fronts.setdefault(t, [])
                    s = t % 2
                    is_ctx = t < NCT
                    r = 1 if is_ctx else 0
                    do_out = t in p2_tiles
                    qb_ = QB[s]
                    if do_out:
                        S.op("sp", lambda e, t=t, qb_=qb_: e.dma_start(out=qb_.h[:], in_=qbun[t]), reads=[qbun_b[t]], writes=[qb_.b], dma_key=qb_.b)
                        S.op("sp", lambda e, t=t, s=s: e.dma_start(out=XR[s].h[:], in_=xsrc[t * 128:(t + 1) * 128, :]), reads=xsrc_bufs(t), writes=[XR[s].b], dma_key=XR[s].b)
                        mx = mix[s]
                        if is_ctx:
                            blocks = [(0, None), (1, None)]
                        else:
                            blocks = []
                            if t > NCT:
                                blocks.append((t - 1, mprev))
                            blocks.append((t, None))
                            if t < NT - 1:
                                blocks.append((t + 1, mnext))
                            blocks += [(0, None), (1, None)]
                        first = t in (0, NCT)
                        lastt = t in (NCT - 1, NT - 1)
                        qd = qbd[s]
                        S.op("dve", lambda e, qd=qd, qb_=qb_: e.tensor_tensor(out=qd.h[:].rearrange("p (h n) -> p h n", h=4), in0=maskQ.rearrange("p (h n) -> p h n", h=4), in1=qb_.h[:, 512:640].unsqueeze(1).to_broadcast([128, 4, 128]), op=ALU.mult),
                             reads=[qb_.b, cm.b], writes=[qd.b])
                        qf = qfb[s]
                        S.op("dve", lambda e, qf=qf, qb_=qb_: e.tensor_tensor(out=qf.h[:].rearrange("p (a n) -> p a n", a=2), in0=DFB.h[:].rearrange("p (a n) -> p a n", a=2), in1=qb_.h[:, 512:640].unsqueeze(1).to_broadcast([128, 2, 128]), op=ALU.mult),
                             reads=[qb_.b, DFB.b], writes=[qf.b])
                        for g in range(4):
                            srcs = []
                            if not first:
                                srcs.append((t - 1, 3))
                            srcs.append((t, 0 if first else (2 if lastt else 1)))
                            if not lastt:
                                srcs.append((t + 1, 4))
                            for si, (j, v) in enumerate(srcs):
                                S.op("pe", lambda e, g=g, j=j, v=v, si=si, ns=len(srcs): e.matmul(psPL.h[0:64, g * 128:(g + 1) * 128], lhsT=UR.h[:, j, g * 64:(g + 1) * 64], rhs=PA(g, v), start=(si == 0), stop=(si == ns - 1)),
                                     reads=[UR_b[j], cb.b], writes=[psPL.b])
                        S.op("act", lambda e: e.copy(out=plT.h[:], in_=psPL.h[0:64, :]), reads=[psPL.b], writes=[plT.b])
                        pA = next_psS()
                        S.op("pe", lambda e, pA=pA, qd=qd, qb_=qb_: e.matmul(pA.h[:], lhsT=qb_.h[:, 640:768], rhs=qd.h[:], start=True, stop=True), reads=[qb_.b, qd.b], writes=[pA.b])
                        ptr = PTr[s]
                        S.op("dve", lambda e, pA=pA, ptr=ptr: e.tensor_tensor(out=ptr.h[:], in0=pA.h[:], in1=DsumT.h[:], op=ALU.mult), reads=[pA.b, DsumT.b], writes=[ptr.b])
                    sfc = SFbd[sf_cur]
                    if t < NT - 1:
                        kd = kdf[t % 2]
                        S.op("pool", lambda e, t=t, kd=kd: e.tensor_tensor(out=kd.h[:], in0=RKT.h[:, t, :], in1=WFB.h[:, 0:128], op=ALU.mult), reads=[RKT_b[t], WFB.b], writes=[kd.b])
                        pk = next_psS()
                        S.op("pe", lambda e, t=t, kd=kd, pk=pk: e.matmul(pk.h[:, 0:256], lhsT=kd.h[:], rhs=UR.h[:, t, 256:512], start=True, stop=True), reads=[kd.b, UR_b[t]], writes=[pk.b])
                        S.op("dve", lambda e, pk=pk: e.scalar_tensor_tensor(out=SFs.h[:], in0=SFs.h[:], scalar=gC.h[:, 0:1], in1=pk.h[:, 0:256], op0=ALU.mult, op1=ALU.add), reads=[SFs.b, gC.b, pk.b], writes=[SFs.b])
                        sf_cur = 1 - sf_cur
                        sfn = SFbd[sf_cur]
                        S.op("pool", lambda e, sfn=sfn: e.tensor_tensor(out=sfn.h[:], in0=SFs.h[:], in1=maskS, op=ALU.mult), reads=[SFs.b, cm.b], writes=[sfn.b])
                    if do_out:
                        seq = [(kv, bi, j, mk) for kv in range(2) for bi, (j, mk) in enumerate(blocks)]
                        nb_ = len(blocks)

                        def emit_score(kv, bi, j, mk):
                            pS = next_psS()
                            S.op("pe", lambda e, kv=kv, j=j, mk=mk, pS=pS, qb_=qb_: e.matmul(pS.h[:], lhsT=KT.h[64 * kv:64 * kv + 64, j, :], rhs=qb_.h[64 * kv:64 * kv + 64, 0:512], start=True, stop=(mk is None)),
                                 reads=[KT_b[j], qb_.b], writes=[pS.b])
                            if mk is not None:
                                S.op("pe", lambda e, mk=mk, pS=pS: e.matmul(pS.h[:], lhsT=ident, rhs=mk, start=False, stop=True), reads=[cm.b, cb.b], writes=[pS.b])
                            return pS
                        pend = emit_score(*seq[0])
                        for i, (kv, bi, j, mk) in enumerate(seq):
                            pS = pend
                            if i + 1 < len(seq):
                                pend = emit_score(*seq[i + 1])
                            pt = next_PT()
                            S.op("act", lambda e, pS=pS, pt=pt: e.activation(out=pt.h[:], in_=pS.h[:], func=AF.Exp, scale=0.125), reads=[pS.b], writes=[pt.b])
                            for g in range(4):
                                S.op("pe", lambda e, g=g, j=j, kv=kv, pt=pt, bi=bi, nb_=nb_: e.matmul(psO.h[:, g * 65:(g + 1) * 65], lhsT=pt.h[:, g * 128:(g + 1) * 128], rhs=VX.h[:, j, kv, :], start=(bi == 0 and g == 0), stop=(bi == nb_ - 1 and g == 3), skip_group_check=True),
                                     reads=[pt.b, VX_b[j]], writes=[psO.b])
                            if bi == nb_ - 1:
                                pov = psO.h[:, 0:260].rearrange("p (g c) -> p g c", g=4)
                                S.op("dve", lambda e, kv=kv, pov=pov: e.tensor_tensor(out=den.h[:], in0=pov[:, :, 64], in1=esink.h[:, 4 * kv:4 * kv + 4], op=ALU.add), reads=[psO.b, esink.b], writes=[den.b])
                                S.op("dve", lambda e: e.reciprocal(out=rec.h[:], in_=den.h[:]), reads=[den.b], writes=[rec.b])
                                S.op("dve", lambda e, kv=kv, pov=pov, mx=mx: e.tensor_tensor(out=mx.h[:, kv * 256:(kv + 1) * 256].rearrange("p (g d) -> p g d", g=4), in0=pov[:, :, 0:64], in1=rec.h[:].unsqueeze(2).to_broadcast([128, 4, 64]), op=ALU.mult),
                                     reads=[psO.b, rec.b], writes=[mx.b])
                        for g in range(4):
                            S.op("pe", lambda e, g=g: e.matmul(psMa.h[:, g * 64:(g + 1) * 64], lhsT=plT.h[:, g * 128:(g + 1) * 128], rhs=PW.h[:, g * 64:(g + 1) * 64], start=True, stop=True), reads=[plT.b, PW.b], writes=[psMa.b])
                        S.op("act", lambda e, mx=mx: e.copy(out=mx.h[:, 512:768], in_=psMa.h[:, 0:256]), reads=[psMa.b], writes=[mx.b])
                        S.op("pe", lambda e, qf=qf, sfc=sfc: e.matmul(psR.h[:, 0:256], lhsT=qf.h[:, 0:128], rhs=sfc.h[:], start=True, stop=False), reads=[qf.b, sfc.b], writes=[psR.b])
                        S.op("pe", lambda e, qf=qf, t=t: e.matmul(psR.h[:, 0:256], lhsT=qf.h[:, 128:256], rhs=SBD.h[:, t, :], start=False, stop=False), reads=[qf.b, SBD_b[t]], writes=[psR.b])
                        for h in range(4):
                            S.op("pe", lambda e, h=h, ptr=ptr, t=t: e.matmul(psR.h[:, h * 64:(h + 1) * 64], lhsT=ptr.h[:, h * 128:(h + 1) * 128], rhs=UR.h[:, t, 256 + h * 64:256 + (h + 1) * 64], start=False, stop=(h == 3)),
                                 reads=[ptr.b, UR_b[t]], writes=[psR.b])
                    if not do_out:
                        continue
                    S.cur = tails.setdefault(t, [])
                    if "ret" in SKIP:
                        pass
                    else:
                        S.op("act", lambda e: e.copy(out=ro.h[:], in_=psR.h[:, 0:256]), reads=[psR.b], writes=[ro.b])
                        prv = ro.h[:].rearrange("p (h d) -> p h d", h=4)
                        S.op("dve", lambda e, prv=prv: e.tensor_reduce(out=st8.h[:, 0:4], in_=prv, axis=AX.X, op=ALU.add), reads=[ro.b], writes=[st8.b])
                        S.op("act", lambda e: e.activation(out=sq.h[:], in_=ro.h[:], func=AF.Square), reads=[ro.b], writes=[sq.b])
                        S.op("dve", lambda e: e.tensor_reduce(out=st8.h[:, 4:8], in_=sq.h[:].rearrange("p (h d) -> p h d", h=4), axis=AX.X, op=ALU.add), reads=[sq.b], writes=[st8.b])
                        S.op("dve", lambda e: e.tensor_scalar_mul(out=st8.h[:, 8:12], in0=st8.h[:, 0:4], scalar1=1.0 / 64), reads=[st8.b], writes=[st8.b])
                        S.op("dve", lambda e: e.tensor_tensor(out=st8.h[:, 12:16], in0=st8.h[:, 8:12], in1=st8.h[:, 8:12], op=ALU.mult), reads=[st8.b], writes=[st8.b])
                        S.op("dve", lambda e: e.scalar_tensor_tensor(out=st8.h[:, 16:20], in0=st8.h[:, 4:8], scalar=1.0 / 64, in1=st8.h[:, 12:16], op0=ALU.mult, op1=ALU.subtract), reads=[st8.b], writes=[st8.b])
                        S.op("act", lambda e: e.activation(out=st8.h[:, 20:24], in_=st8.h[:, 16:20], func=AF.Ln, bias=epsc, scale=1.0), reads=[st8.b, cc.b], writes=[st8.b])
                        S.op("act", lambda e: e.activation(out=st8.h[:, 20:24], in_=st8.h[:, 20:24], func=AF.Exp, scale=-0.5), reads=[st8.b], writes=[st8.b])
                        S.op("dve", lambda e, prv=prv: e.tensor_tensor(out=cen.h[:].rearrange("p (h d) -> p h d", h=4), in0=prv, in1=st8.h[:, 8:12].unsqueeze(2).to_broadcast([128, 4, 64]), op=ALU.subtract), reads=[ro.b, st8.b], writes=[cen.b])
                        S.op("dve", lambda e: e.tensor_tensor(out=cen.h[:].rearrange("p (h d) -> p h d", h=4), in0=cen.h[:].rearrange("p (h d) -> p h d", h=4), in1=st8.h[:, 20:24].unsqueeze(2).to_broadcast([128, 4, 64]), op=ALU.mult), reads=[cen.b, st8.b], writes=[cen.b])
                        S.op("pool", lambda e, mx=mx, qb_=qb_: e.tensor_tensor(out=mx.h[:, 768:1024], in0=cen.h[:], in1=qb_.h[:, 768:1024], op=ALU.mult), reads=[cen.b, qb_.b], writes=[mx.b])
                    if "oproj" in SKIP:
                        continue
                    for kc in range(8):
                        S.op("pe", lambda e, kc=kc, mx=mx: e.transpose(out=psMT.h[:, kc, :], in_=mx.h[:, kc * 128:(kc + 1) * 128], identity=ident), reads=[mx.b, cm.b], writes=[psMT.b])
                    mt = mixT[s]
                    S.op("act", lambda e, mt=mt: e.copy(out=mt.h[:, 0:4, :], in_=psMT.h[:, 0:4, :]), reads=[psMT.b], writes=[mt.b])
                    S.op("dve", lambda e, mt=mt: e.tensor_copy(out=mt.h[:, 4:8, :], in_=psMT.h[:, 4:8, :]), reads=[psMT.b], writes=[mt.b])
                    ta = tA[s]
                    gi = 2 if is_ctx else 0
                    for nh in range(2):
                        for kc in range(8):
                            S.op("pe", lambda e, nh=nh, kc=kc, mt=mt: e.matmul(psY[nh].h[:], lhsT=mt.h[:, kc, :], rhs=wout.h[:, kc, nh * 512:(nh + 1) * 512], start=(kc == 0), stop=(kc == 7)), reads=[mt.b, wout.b], writes=[psY[nh].b])
                        S.op("dve", lambda e, nh=nh, ta=ta, gi=gi: e.tensor_tensor(out=ta.h[:, nh * 512:(nh + 1) * 512], in0=psY[nh].h[:], in1=Gt[gi].h[:, nh * 512:(nh + 1) * 512], op=ALU.mult), reads=[psY[nh].b, Gt[gi].b], writes=[ta.b])
                    emit_ln_epilogue(S, ta, XR[s], LNt[0], LNt[1], st6, mv, epsln, cc,
                                     xs1[t * 128:(t + 1) * 128, :], xs1_b[t])
                S = S_real
                prev_tail = None
                for t in range(NT):
                    Fl = fronts.get(t, [])
                    if prev_tail:
                        j = 0
                        for idx, (a, kw) in enumerate(prev_tail):
                            S.op(*a, **kw)
                            j2 = (idx + 1) * len(Fl) // len(prev_tail)
                            for (a2, kw2) in Fl[j:j2]:
                                S.op(*a2, **kw2)
                            j = j2
                        for (a2, kw2) in Fl[j:]:
                            S.op(*a2, **kw2)
                    else:
                        for (a2, kw2) in Fl:
                            S.op(*a2, **kw2)
                    prev_tail = tails.get(t)
                if prev_tail:
                    for (a, kw) in prev_tail:
                        S.op(*a, **kw)
                S.emit()

            M.__exit__(None, None, None)
            if stop == "p2":
                break

            if (layer % 2 == 1) and stop == "m":
                break
            if (layer % 2 == 1) and SPARSE:
                I32 = mybir.dt.int32
                NK = 24
                xlat = xs1[NCT * 128:, :]
                slot_b = Buf("slotinfo")
                F_b = Buf("Fsc")
                IW = sb(G, "IW%d" % layer, [128, NK, 56], I32)
                with contextlib.ExitStack() as P:
                    S = Sched(nc, "L%dr" % layer)
                    rc = sb(P, "rc", [128, RC_TOT], F32)
                    idf = sb(P, "idf", [128, 128], F32)
                    wr = sb(P, "wr", [128, 8, NEXP], F32)
                    XGr = [sb(P, "XGr%d" % i, [128, D], F32) for i in range(2)]
                    t32 = [sb(P, "t32r%d" % i, [128, 8, 128], F32) for i in range(2)]
                    gt = sb(P, "gtr", [128, 64], F32)
                    M12 = sb(P, "M12", [128, 32, 16], F32)
                    W12 = sb(P, "W12", [128, 32, 2], F32)
                    POS = sb(P, "POS", [128, 32, 8], F32)
                    carry = sb(P, "carry", [128, 8], F32)
                    seg = sb(P, "seg", [128, 32], F32)
                    sl = sb(P, "sl", [128, 32, 2], F32)
                    sli = sb(P, "sli", [128, 32, 2], I32)
                    info = sb(P, "info", [128, 32, 2, 4], F32)
                    tmp8 = sb(P, "tmp8", [128, 16], F32)
                    IWf = sb(P, "IWf", [128, NK, 56], F32)
                    psXf = [ps(P, "psXr%d" % i, [128, 4, 128], F32) for i in range(2)]
                    psL = [ps(P, "psL%d" % i, [128, 512], F32) for i in range(2)]
                    psC = [ps(P, "psC%d" % i, [128, 512], F32) for i in range(2)]
                    S.op("sp", lambda e: e.dma_start(out=rc.h[:], in_=rconst), writes=[rc.b], dma_key=rc.b)
                    S.op("sp", lambda e: e.dma_start(out=idf.h[:], in_=consts[:, C_IDENT:C_IDENT + 128]), writes=[idf.b], dma_key=idf.b)
                    S.op("sp", lambda e: e.dma_start(out=wr.h[:], in_=router.rearrange("(k p) n -> p k n", p=128)), writes=[wr.b], dma_key=wr.b)
                    S.op("sp", lambda e: e.dma_start(out=slotinfo, in_=slot_init), writes=[slot_b], dma_key=slot_b)
                    sck = [Buf("sck%d" % i) for i in range(4)]
                    S.op("pool", lambda e: e.memset(carry.h[:], 0.0), writes=[carry.b])
                    S.op("pool", lambda e: e.memset(info.h[:], 0.0), writes=[info.b])
                    Umat = rc.h[:, RC_U:RC_U + 128]
                    Ones = rc.h[:, RC_ONES:RC_ONES + 128]
                    for ti in range(32):
                        t = NCT + ti
                        xg = XGr[ti % 2]
                        t3 = t32[ti % 2]
                        S.op("sp", lambda e, t=t, xg=xg: e.dma_start(out=xg.h[:], in_=xs1[t * 128:(t + 1) * 128, :]), reads=[xs1_b[t]], writes=[xg.b], dma_key=xg.b)
                        for hf in range(2):
                            px = psXf[hf]
                            for k4 in range(4):
                                kc = hf * 4 + k4
                                S.op("pe", lambda e, kc=kc, k4=k4, xg=xg, px=px: e.transpose(out=px.h[:, k4, :], in_=xg.h[:, kc * 128:(kc + 1) * 128], identity=idf.h[:]), reads=[xg.b, idf.b], writes=[px.b])
                            for k4 in range(4):
                                kc = hf * 4 + k4
                                if kc % 2 == 0:
                                    S.op("act", lambda e, kc=kc, k4=k4, px=px, t3=t3: e.activation(out=t3.h[:, kc, :], in_=px.h[:, k4, :], func=AF.Identity, bias=SH2(kc, 0), scale=A2(kc, 0)), reads=[px.b, modT.b], writes=[t3.b])
                                else:
                                    S.op("dve", lambda e, kc=kc, k4=k4, px=px, t3=t3: e.tensor_scalar(out=t3.h[:, kc, :], in0=px.h[:, k4, :], scalar1=A2(kc, 0), scalar2=SH2(kc, 0), op0=ALU.mult, op1=ALU.add), reads=[px.b, modT.b], writes=[t3.b])
                        pr = psL[ti % 2]
                        for kc in range(8):
                            S.op("pe", lambda e, kc=kc, t3=t3, pr=pr: e.matmul(pr.h[:, 0:NEXP], lhsT=t3.h[:, kc, :], rhs=wr.h[:, kc, :], start=(kc == 0), stop=(kc == 7)), reads=[t3.b, wr.b], writes=[pr.b])
                        lg_ = gt.h[:, 0:8]; m1 = gt.h[:, 8:9]; l2 = gt.h[:, 24:32]; m2 = gt.h[:, 9:10]
                        dd = gt.h[:, 10:11]; ee = gt.h[:, 11:12]
                        k1 = M12.h[:, ti, 0:8]; k2 = M12.h[:, ti, 8:16]
                        w1 = W12.h[:, ti, 0:1]; w2 = W12.h[:, ti, 1:2]
                        gb = [gt.b, M12.b, W12.b]
                        S.op("dve", lambda e, pr=pr, lg_=lg_: e.tensor_copy(out=lg_, in_=pr.h[:, 0:NEXP]), reads=[pr.b], writes=gb)
                        S.op("dve", lambda e, lg_=lg_, m1=m1: e.tensor_reduce(out=m1, in_=lg_, axis=AX.X, op=ALU.max), reads=gb, writes=gb)
                        S.op("dve", lambda e, lg_=lg_, m1=m1, k1=k1: e.tensor_scalar(out=k1, in0=lg_, scalar1=m1, scalar2=None, op0=ALU.is_equal), reads=gb, writes=gb)
                        S.op("dve", lambda e, lg_=lg_, k1=k1, l2=l2: e.scalar_tensor_tensor(out=l2, in0=k1, scalar=-1e30, in1=lg_, op0=ALU.mult, op1=ALU.add), reads=gb, writes=gb)
                        S.op("dve", lambda e, l2=l2, m2=m2: e.tensor_reduce(out=m2, in_=l2, axis=AX.X, op=ALU.max), reads=gb, writes=gb)
                        S.op("dve", lambda e, l2=l2, m2=m2, k2=k2: e.tensor_scalar(out=k2, in0=l2, scalar1=m2, scalar2=None, op0=ALU.is_equal), reads=gb, writes=gb)
                        S.op("dve", lambda e, dd=dd, m1=m1, m2=m2: e.tensor_tensor(out=dd, in0=m2, in1=m1, op=ALU.subtract), reads=gb, writes=gb)
                        S.op("act", lambda e, dd=dd, ee=ee: e.activation(out=ee, in_=dd, func=AF.Exp), reads=gb, writes=gb)
                        S.op("dve", lambda e, ee=ee, w1=w1: e.tensor_scalar_add(out=w1, in0=ee, scalar1=1.0), reads=gb, writes=gb)
                        S.op("dve", lambda e, w1=w1: e.reciprocal(out=w1, in_=w1), reads=gb, writes=gb)
                        S.op("dve", lambda e, ee=ee, w1=w1, w2=w2: e.tensor_tensor(out=w2, in0=ee, in1=w1, op=ALU.mult), reads=gb, writes=gb)
                        ma = tmp8.h[:, 0:8]
                        S.op("dve", lambda e, k1=k1, k2=k2, ma=ma: e.tensor_tensor(out=ma, in0=k1, in1=k2, op=ALU.add), reads=gb, writes=[tmp8.b])
                        pc = psC[ti % 2]
                        S.op("pe", lambda e, pc=pc, ma=ma: e.matmul(pc.h[:, 0:8], lhsT=Umat, rhs=ma, start=True, stop=True), reads=[rc.b, tmp8.b], writes=[pc.b])
                        S.op("pe", lambda e, pc=pc, ma=ma: e.matmul(pc.h[:, 8:16], lhsT=Ones, rhs=ma, start=True, stop=True), reads=[rc.b, tmp8.b], writes=[pc.b])
                        S.op("dve", lambda e, pc=pc, ti=ti: e.tensor_tensor(out=POS.h[:, ti, :], in0=pc.h[:, 0:8], in1=carry.h[:], op=ALU.add), reads=[pc.b, carry.b], writes=[POS.b])
                        S.op("dve", lambda e, pc=pc: e.tensor_tensor(out=carry.h[:], in0=pc.h[:, 8:16], in1=carry.h[:], op=ALU.add), reads=[pc.b, carry.b], writes=[carry.b])
                    tl = seg.h[:, 0:8]; se = seg.h[:, 8:16]; ss = seg.h[:, 16:24]
                    sgb = [seg.b]
                    S.op("dve", lambda e: e.tensor_scalar(out=tl, in0=carry.h[:], scalar1=0.0, scalar2=None, op0=ALU.is_gt), reads=[carry.b], writes=sgb)
                    for j in range(1, 8):
                        S.op("dve", lambda e, j=j: e.scalar_tensor_tensor(out=tl, in0=carry.h[:], scalar=512.0 * j, in1=tl, op0=ALU.is_gt, op1=ALU.add), reads=[carry.b] + sgb, writes=sgb)
                    S.op("dve", lambda e: e.tensor_copy(out=se[:, 0:1], in_=tl[:, 0:1]), reads=sgb, writes=sgb)
                    for j in range(1, 8):
                        S.op("dve", lambda e, j=j: e.tensor_tensor(out=se[:, j:j + 1], in0=se[:, j - 1:j], in1=tl[:, j:j + 1], op=ALU.add), reads=sgb, writes=sgb)
                    S.op("dve", lambda e: e.tensor_tensor(out=ss, in0=se, in1=tl, op=ALU.subtract), reads=sgb, writes=sgb)
                    S.op("dve", lambda e: e.tensor_scalar_mul(out=seg.h[:, 8:24], in0=seg.h[:, 8:24], scalar1=512.0), reads=sgb, writes=sgb)
                    for ti in range(32):
                        for k in range(2):
                            tq = tmp8.h[:, 8:16]
                            S.op("dve", lambda e, ti=ti, tq=tq: e.tensor_tensor(out=tq, in0=POS.h[:, ti, :], in1=ss, op=ALU.add), reads=[POS.b] + sgb, writes=[tmp8.b])
                            S.op("dve", lambda e, ti=ti, k=k, tq=tq: e.tensor_tensor(out=tq, in0=tq, in1=M12.h[:, ti, 8 * k:8 * k + 8], op=ALU.mult), reads=[tmp8.b, M12.b], writes=[tmp8.b])
                            S.op("dve", lambda e, ti=ti, k=k, tq=tq: e.tensor_reduce(out=sl.h[:, ti, k:k + 1], in_=tq, axis=AX.X, op=ALU.add), reads=[tmp8.b], writes=[sl.b])
                    S.op("dve", lambda e: e.tensor_copy(out=sli.h[:], in_=sl.h[:]), reads=[sl.b], writes=[sli.b])
                    tokv = rc.h[:, RC_TOK:RC_TOK + 32]
                    for k in range(2):
                        S.op("act", lambda e, k=k: e.copy(out=info.h[:, :, k, 0], in_=tokv), reads=[rc.b], writes=[info.b])
                        S.op("act", lambda e, k=k: e.copy(out=info.h[:, :, k, 1], in_=W12.h[:, :, k]), reads=[W12.b], writes=[info.b])
                        S.op("dve", lambda e, k=k: e.tensor_scalar_add(out=info.h[:, :, k, 2], in0=tokv, scalar1=float(k * L - NCT * 128)), reads=[rc.b], writes=[info.b])
                    for ti in range(32):
                        for k in range(2):
                            S.op("pool", lambda e, ti=ti, k=k: e.indirect_dma_start(out=slotinfo, out_offset=bass.IndirectOffsetOnAxis(ap=sli.h[:, ti, k:k + 1], axis=0), in_=info.h[:, ti, k, :], in_offset=None),
                                 reads=[sli.b, info.b, slot_b], writes=[], dma_key=sck[(2 * ti + k) % 4])
                    for k in range(NK):
                        c8 = tmp8.h[:, 0:8]; ek = tmp8.h[:, 8:9]; e1 = tmp8.h[:, 9:10]; e2 = tmp8.h[:, 10:11]
                        tb = [tmp8.b]
                        S.op("dve", lambda e, k=k, c8=c8: e.tensor_scalar(out=c8, in0=se, scalar1=512.0 * k, scalar2=None, op0=ALU.is_le), reads=sgb, writes=tb)
                        S.op("dve", lambda e, c8=c8, ek=ek: e.tensor_reduce(out=ek, in_=c8, axis=AX.X, op=ALU.add), reads=tb, writes=tb)
                        S.op("dve", lambda e, ek=ek: e.tensor_scalar_min(out=ek, in0=ek, scalar1=7.0), reads=tb, writes=tb)
                        S.op("dve", lambda e, ek=ek, e1=e1: e.tensor_scalar_mul(out=e1, in0=ek, scalar1=512.0), reads=tb, writes=tb)
                        S.op("dve", lambda e, ek=ek, e2=e2: e.tensor_scalar_mul(out=e2, in0=ek, scalar1=2816.0), reads=tb, writes=tb)
                        S.op("dve", lambda e, k=k, e1=e1: e.tensor_scalar(out=IWf.h[:, k, 0:32], in0=rc.h[:, RC_BG:RC_BG + 32], scalar1=e1, scalar2=None, op0=ALU.add), reads=tb + [rc.b], writes=[IWf.b])
                        S.op("dve", lambda e, k=k, e2=e2: e.tensor_scalar(out=IWf.h[:, k, 32:54], in0=rc.h[:, RC_BD:RC_BD + 22], scalar1=e2, scalar2=None, op0=ALU.add), reads=tb + [rc.b], writes=[IWf.b])
                    S.op("pool", lambda e: e.memset(IWf.h[:, :, 54:56], 0.0), writes=[IWf.b])
                    S.op("dve", lambda e: e.tensor_copy(out=IW.h[:], in_=IWf.h[:]), reads=[IWf.b], writes=[IW.b])
                    S.emit()
                if stop == "r":
                    break
                with contextlib.ExitStack() as P:
                    S = Sched(nc, "L%dx" % layer)
                    units = []
                    c0 = 0
                    for ncu in (6, 6, 5, 5):
                        units.append((c0, ncu))
                        c0 += ncu
                    WG = [sb(P, "WG%d" % i, [128, 8, 768], BF16) for i in range(2)]
                    WU = [sb(P, "WU%d" % i, [128, 8, 768], BF16) for i in range(2)]
                    WD = [sb(P, "WD%d" % i, [128, 6, D], BF16) for i in range(2)]
                    XG = sb(P, "XGx", [128, 4, D], F32)
                    XG_b = [Buf("XGx%d" % j) for j in range(4)]
                    tTs = [sb(P, "tTx%d" % i, [128, 8, 512], BF16) for i in range(2)]
                    hid = [sb(P, "hidx%d" % i, [128, 6, 512], BF16) for i in range(2)]
                    sg = [sb(P, "sgx%d" % i, [128, 512], BF16) for i in range(2)]
                    accs = [sb(P, "accx%d" % i, [128, 4, D], F32) for i in range(2)]
                    accs_b = [[Buf("accx%d_%d" % (i, j)) for j in range(4)] for i in range(2)]
                    SI = [sb(P, "SI%d" % i, [128, 4, 4], F32) for i in range(2)]
                    TI = [sb(P, "TI%d" % i, [128, 4, 4], I32) for i in range(2)]
                    idf = sb(P, "idfx", [128, 128], F32)
                    psXf = [ps(P, "psXx%d" % i, [128, 4, 128], F32) for i in range(2)]
                    psGa = [ps(P, "psGx%d" % i, [128, 512], F32) for i in range(2)]
                    psUa = [ps(P, "psUx%d" % i, [128, 512], F32) for i in range(2)]
                    psY = [ps(P, "psYx%d" % i, [128, 512], F32) for i in range(2)]
                    S.op("sp", lambda e: e.dma_start(out=idf.h[:], in_=consts[:, C_IDENT:C_IDENT + 128]), writes=[idf.b], dma_key=idf.b)
                    yc = [0]

                    def prep(k):
                        si = SI[k % 2]; tix = TI[k % 2]; tT = tTs[k % 2]
                        S.op("sp", lambda e, k=k, si=si: e.dma_start(out=si.h[:], in_=slotinfo[k * 512:(k + 1) * 512, :].rearrange("(s p) c -> p s c", p=128)), reads=[slot_b], writes=[si.b], dma_key=si.b)
                        S.op("dve", lambda e, si=si, tix=tix: e.tensor_copy(out=tix.h[:], in_=si.h[:]), reads=[si.b], writes=[tix.b])
                        for sub in range(4):
                            S.op("pool", lambda e, sub=sub, tix=tix: e.indirect_dma_start(out=XG.h[:, sub, :], out_offset=None, in_=xs1, in_offset=bass.IndirectOffsetOnAxis(ap=tix.h[:, sub, 0:1], axis=0)),
                                 reads=[tix.b] + xs1_b, writes=[XG_b[sub]], dma_key=XG_b[sub])
                        for sub in range(4):
                            for hf in range(2):
                                px = psXf[hf]
                                for k4 in range(4):
                                    kc = hf * 4 + k4
                                    S.op("pe", lambda e, kc=kc, k4=k4, sub=sub, px=px: e.transpose(out=px.h[:, k4, :], in_=XG.h[:, sub, kc * 128:(kc + 1) * 128], identity=idf.h[:]), reads=[XG_b[sub], idf.b], writes=[px.b])
                                for k4 in range(4):
                                    kc = hf * 4 + k4
                                    if kc % 2 == 0:
                                        S.op("act", lambda e, kc=kc, k4=k4, px=px, sub=sub, tT=tT: e.activation(out=tT.h[:, kc, sub * 128:(sub + 1) * 128], in_=px.h[:, k4, :], func=AF.Identity, bias=SH2(kc, 0), scale=A2(kc, 0)), reads=[px.b, modT.b], writes=[tT.b])
                                    else:
                                        S.op("dve", lambda e, kc=kc, k4=k4, px=px, sub=sub, tT=tT: e.tensor_scalar(out=tT.h[:, kc, sub * 128:(sub + 1) * 128], in0=px.h[:, k4, :], scalar1=A2(kc, 0), scalar2=SH2(kc, 0), op0=ALU.mult, op1=ALU.add), reads=[px.b, modT.b], writes=[tT.b])

                    def gather(k, ui):
                        us = (k * 4 + ui) % 2
                        iw = lambda k=k, ui=ui: bass.IndirectOffsetOnAxis(ap=IW.h[:, k, ui:ui + 1], axis=0)
                        S.op("pool", lambda e, us=us, iw=iw: e.indirect_dma_start(out=WG[us].h[:].rearrange("p k n -> p (k n)"), out_offset=None, in_=wg2d, in_offset=iw()), reads=[IW.b], writes=[WG[us].b], dma_key=WG[us].b)
                        S.op("pool", lambda e, us=us, iw=iw: e.indirect_dma_start(out=WU[us].h[:].rearrange("p k n -> p (k n)"), out_offset=None, in_=wu2d, in_offset=iw()), reads=[IW.b], writes=[WU[us].b], dma_key=WU[us].b)
                        S.op("pool", lambda e, us=us, iw=iw: e.indirect_dma_start(out=WD[us].h[:].rearrange("p c n -> p (c n)"), out_offset=None, in_=wd2d, in_offset=iw()), reads=[IW.b], writes=[WD[us].b], dma_key=WD[us].b)

                    def compute(k, ui):
                        c0, ncu = units[ui]
                        us = (k * 4 + ui) % 2
                        tT = tTs[k % 2]
                        acc = accs[k % 2]; acc_b = accs_b[k % 2]
                        hd = hid[us]
                        for c in range(ncu):
                            pg = psGa[c % 2]
                            pu = psUa[c % 2]
                            for kc in range(8):
                                S.op("pe", lambda e, c=c, kc=kc, us=us, pg=pg, tT=tT: e.matmul(pg.h[:], lhsT=WG[us].h[:, kc, c * 128:(c + 1) * 128], rhs=tT.h[:, kc, :], start=(kc == 0), stop=(kc == 7)), reads=[WG[us].b, tT.b], writes=[pg.b])
                            for kc in range(8):
                                S.op("pe", lambda e, c=c, kc=kc, us=us, pu=pu, tT=tT: e.matmul(pu.h[:], lhsT=WU[us].h[:, kc, c * 128:(c + 1) * 128], rhs=tT.h[:, kc, :], start=(kc == 0), stop=(kc == 7)), reads=[WU[us].b, tT.b], writes=[pu.b])
                            sg_ = sg[c % 2]
                            S.op("act", lambda e, pg=pg, sg_=sg_: e.activation(out=sg_.h[:], in_=pg.h[:], func=AF.Silu), reads=[pg.b], writes=[sg_.b])
                            S.op("dve", lambda e, c=c, pu=pu, sg_=sg_, hd=hd: e.tensor_tensor(out=hd.h[:, c, :], in0=pu.h[:], in1=sg_.h[:], op=ALU.mult), reads=[pu.b, sg_.b], writes=[hd.b])
                        for sub in range(4):
                            for nh in range(2):
                                py = psY[yc[0] % 2]
                                yc[0] += 1
                                for c in range(ncu):
                                    S.op("pe", lambda e, c=c, sub=sub, nh=nh, us=us, hd=hd, py=py, ncu=ncu: e.matmul(py.h[:], lhsT=hd.h[:, c, sub * 128:(sub + 1) * 128], rhs=WD[us].h[:, c, nh * 512:(nh + 1) * 512], start=(c == 0), stop=(c == ncu - 1)), reads=[hd.b, WD[us].b], writes=[py.b])
                                ao = acc.h[:, sub, nh * 512:(nh + 1) * 512]
                                if ui == 0:
                                    S.op("act", lambda e, ao=ao, py=py: e.copy(out=ao, in_=py.h[:]), reads=[py.b], writes=[acc_b[sub]])
                                else:
                                    S.op("dve", lambda e, ao=ao, py=py: e.tensor_tensor(out=ao, in0=py.h[:], in1=ao, op=ALU.add), reads=[py.b, acc_b[sub]], writes=[acc_b[sub]])

                    def fin(k):
                        si = SI[k % 2]; tix = TI[k % 2]
                        acc = accs[k % 2]; acc_b = accs_b[k % 2]
                        for sub in range(4):
                            S.op("act", lambda e, sub=sub, si=si, acc=acc: e.activation(out=acc.h[:, sub, :], in_=acc.h[:, sub, :], func=AF.Identity, scale=si.h[:, sub, 1:2]), reads=[acc_b[sub], si.b], writes=[acc_b[sub]])
                            S.op("pool", lambda e, sub=sub, tix=tix, acc=acc: e.indirect_dma_start(out=Fsc, out_offset=bass.IndirectOffsetOnAxis(ap=tix.h[:, sub, 2:3], axis=0), in_=acc.h[:, sub, :], in_offset=None),
                                 reads=[acc_b[sub], tix.b], writes=[], dma_key=acc_b[sub])

                    prep(0)
                    gather(0, 0)
                    gather(0, 1)
                    for k in range(NK):
                        for ui in range(4):
                            compute(k, ui)
                            nk, nu = (k, ui + 2) if ui + 2 < 4 else (k + 1, ui - 2)
                            if nk < NK:
                                gather(nk, nu)
                            if ui == 1 and k + 1 < NK:
                                prep(k + 1)
                        fin(k)
                    S.emit()
                if stop == "x":
                    break
                with contextlib.ExitStack() as P:
                    S = Sched(nc, "L%dz" % layer)
                    F0 = [sb(P, "F0_%d" % i, [128, D], F32) for i in range(2)]
                    F1 = [sb(P, "F1_%d" % i, [128, D], F32) for i in range(2)]
                    XE = [sb(P, "XE%d" % i, [128, D], F32) for i in range(2)]
                    st6 = sb(P, "st6z", [128, 2, 6], F32)
                    mv = sb(P, "mvz", [128, 4], F32)
                    for ti in range(32):
                        t = NCT + ti
                        s_ = ti % 2
                        S.op("sp", lambda e, ti=ti, s_=s_: e.dma_start(out=F0[s_].h[:], in_=Fsc[ti * 128:(ti + 1) * 128, :]), reads=[F_b], writes=[F0[s_].b], dma_key=F0[s_].b)
                        S.op("sp", lambda e, ti=ti, s_=s_: e.dma_start(out=F1[s_].h[:], in_=Fsc[L + ti * 128:L + (ti + 1) * 128, :]), reads=[F_b], writes=[F1[s_].b], dma_key=F1[s_].b)
                        S.op("sp", lambda e, t=t, s_=s_: e.dma_start(out=XE[s_].h[:], in_=xs1[t * 128:(t + 1) * 128, :]), reads=[xs1_b[t]], writes=[XE[s_].b], dma_key=XE[s_].b)
                        S.op("pool", lambda e, s_=s_: e.tensor_tensor(out=F0[s_].h[:], in0=F0[s_].h[:], in1=F1[s_].h[:], op=ALU.add), reads=[F0[s_].b, F1[s_].b], writes=[F0[s_].b])
                        S.op("dve", lambda e, s_=s_: e.tensor_tensor(out=F0[s_].h[:], in0=F0[s_].h[:], in1=Gt[1].h[:], op=ALU.mult), reads=[F0[s_].b, Gt[1].b], writes=[F0[s_].b])
                        if last:
                            dst, dstb = out[ti * 128:(ti + 1) * 128, :], out_b[t]
                        else:
                            dst, dstb = xs2[t * 128:(t + 1) * 128, :], xs2_b[t]
                        emit_ln_epilogue(S, F0[s_], XE[s_], LNt[2], LNt[3], st6, mv, epsln, cc, dst, dstb)
                    S.emit()
                continue
            with contextlib.ExitStack() as P:
                S = Sched(nc, "L%dp3" % layer)
                moe = (layer % 2 == 1)
                nexp = moe_experts if moe else 1
                if moe:
                    wg_src = lambda e_: moe_wg[e_]
                    wu_src = lambda e_: moe_wu[e_]
                    wd_src = lambda e_: moe_wd[e_]
                else:
                    wg_src = lambda e_: ffn_wg[layer // 2]
                    wu_src = lambda e_: ffn_wu[layer // 2]
                    wd_src = lambda e_: ffn_wd[layer // 2]
                units = []
                for e_ in range(nexp):
                    c0 = 0
                    for ncu in (6, 6, 5, 5):
                        units.append((e_, c0, ncu))
                        c0 += ncu
                GT = 4
                tiles = list(range(NT)) if not last else list(range(NCT, NT))
                groups = []
                if not last:
                    groups.append([0, 1])
                for g0 in range(NCT, NT, GT):
                    groups.append(list(range(g0, g0 + GT)))
                WG = [sb(P, "WG%d" % i, [128, 8, 768], BF16) for i in range(2)]
                WU = [sb(P, "WU%d" % i, [128, 8, 768], BF16) for i in range(2)]
                WD = [sb(P, "WD%d" % i, [128, 6, D], BF16) for i in range(2)]
                XG = [sb(P, "XG%d" % i, [128, GT, D], F32) for i in range(1)]
                tT = [sb(P, "tT%d" % i, [128, 8, GT * 128], BF16) for i in range(1)]
                hid = [sb(P, "hid%d" % i, [128, 6, GT * 128], BF16) for i in range(2)]
                sg = [sb(P, "sg%d" % i, [128, GT * 128], BF16) for i in range(2)]
                acc = sb(P, "acc", [128, GT, D], F32)
                acc_b = [Buf("acc%d" % i) for i in range(GT)]
                XG_b = [[Buf("XG%d_%d" % (i, j)) for j in range(GT)] for i in range(1)]
                st6 = sb(P, "st6b", [128, 2, 6], F32)
                mv = sb(P, "mvb", [128, 4], F32)
                psXf = [ps(P, "psXf%d" % i, [128, 4, 128], F32) for i in range(2)]
                psGa = [ps(P, "psGa%d" % i, [128, 512], F32) for i in range(2)]
                psUa = [ps(P, "psUa%d" % i, [128, 512], F32) for i in range(2)]
                psY = [ps(P, "psYb%d" % i, [128, 512], F32) for i in range(2)]
                idf = sb(P, "idf", [128, 128], F32)
                S.op("sp", lambda e: e.dma_start(out=idf.h[:], in_=consts[:, C_IDENT:C_IDENT + 128]), writes=[idf.b], dma_key=idf.b)
                if moe:
                    wr = sb(P, "wr", [128, 8, NEXP], F32)
                    t32 = [sb(P, "t32_%d" % i, [128, 8, 128], F32) for i in range(2)]
                    gates = sb(P, "gates", [128, GT, NEXP], F32)
                    gates_b = [Buf("gates%d" % i) for i in range(GT)]
                    gt = sb(P, "gt", [128, 64], F32)
                    S.op("sp", lambda e: e.dma_start(out=wr.h[:], in_=router.rearrange("(k p) n -> p k n", p=128)), writes=[wr.b], dma_key=wr.b)
                uctr = 0
                yctr = 0
                for gi_, grp in enumerate(groups):
                    gs = 0
                    r = 1 if grp[0] < NCT else 0
                    N = len(grp) * 128
                    for ti, t in enumerate(grp):
                        S.op("sp", lambda e, t=t, ti=ti, gs=gs: e.dma_start(out=XG[gs].h[:, ti, :], in_=xs1[t * 128:(t + 1) * 128, :]), reads=[xs1_b[t]], writes=[XG_b[gs][ti]], dma_key=XG_b[gs][ti])
                        for hf in range(2):
                            px = psXf[hf]
                            for k4 in range(4):
                                kc = hf * 4 + k4
                                S.op("pe", lambda e, kc=kc, k4=k4, ti=ti, gs=gs, px=px: e.transpose(out=px.h[:, k4, :], in_=XG[gs].h[:, ti, kc * 128:(kc + 1) * 128], identity=idf.h[:]), reads=[XG_b[gs][ti], idf.b], writes=[px.b])
                            for k4 in range(4):
                                kc = hf * 4 + k4
                                if moe:
                                    t3 = t32[ti % 2]
                                    S.op("act", lambda e, kc=kc, k4=k4, px=px, t3=t3, r=r: e.activation(out=t3.h[:, kc, :], in_=px.h[:, k4, :], func=AF.Identity, bias=SH2(kc, r), scale=A2(kc, r)), reads=[px.b, modT.b], writes=[t3.b])
                                    S.op("dve", lambda e, kc=kc, ti=ti, gs=gs, t3=t3: e.tensor_copy(out=tT[gs].h[:, kc, ti * 128:(ti + 1) * 128], in_=t3.h[:, kc, :]), reads=[t3.b], writes=[tT[gs].b])
                                elif kc % 2 == 0:
                                    S.op("act", lambda e, kc=kc, k4=k4, px=px, ti=ti, gs=gs, r=r: e.activation(out=tT[gs].h[:, kc, ti * 128:(ti + 1) * 128], in_=px.h[:, k4, :], func=AF.Identity, bias=SH2(kc, r), scale=A2(kc, r)), reads=[px.b, modT.b], writes=[tT[gs].b])
                                else:
                                    S.op("dve", lambda e, kc=kc, k4=k4, px=px, ti=ti, gs=gs, r=r: e.tensor_scalar(out=tT[gs].h[:, kc, ti * 128:(ti + 1) * 128], in0=px.h[:, k4, :], scalar1=A2(kc, r), scalar2=SH2(kc, r), op0=ALU.mult, op1=ALU.add), reads=[px.b, modT.b], writes=[tT[gs].b])
                        if moe:
                            t3 = t32[ti % 2]
                            pr = psY[yctr % 2]
                            yctr += 1
                            for kc in range(8):
                                S.op("pe", lambda e, kc=kc, t3=t3, pr=pr: e.matmul(pr.h[:, 0:NEXP], lhsT=t3.h[:, kc, :], rhs=wr.h[:, kc, :], start=(kc == 0), stop=(kc == 7)), reads=[t3.b, wr.b], writes=[pr.b])
                            lg_ = gt.h[:, 0:8]; m1 = gt.h[:, 8:9]; k1 = gt.h[:, 16:24]; l2 = gt.h[:, 24:32]; m2 = gt.h[:, 9:10]; k2 = gt.h[:, 32:40]
                            dd = gt.h[:, 10:11]; ee = gt.h[:, 11:12]; w1 = gt.h[:, 12:13]; w2 = gt.h[:, 13:14]
                            gb = [gt.b]
                            S.op("dve", lambda e, pr=pr, lg_=lg_: e.tensor_copy(out=lg_, in_=pr.h[:, 0:NEXP]), reads=[pr.b], writes=gb)
                            S.op("dve", lambda e, lg_=lg_, m1=m1: e.tensor_reduce(out=m1, in_=lg_, axis=AX.X, op=ALU.max), reads=gb, writes=gb)
                            S.op("dve", lambda e, lg_=lg_, m1=m1, k1=k1: e.tensor_scalar(out=k1, in0=lg_, scalar1=m1, scalar2=None, op0=ALU.is_equal), reads=gb, writes=gb)
                            S.op("dve", lambda e, lg_=lg_, k1=k1, l2=l2: e.scalar_tensor_tensor(out=l2, in0=k1, scalar=-1e30, in1=lg_, op0=ALU.mult, op1=ALU.add), reads=gb, writes=gb)
                            S.op("dve", lambda e, l2=l2, m2=m2: e.tensor_reduce(out=m2, in_=l2, axis=AX.X, op=ALU.max), reads=gb, writes=gb)
                            S.op("dve", lambda e, l2=l2, m2=m2, k2=k2: e.tensor_scalar(out=k2, in0=l2, scalar1=m2, scalar2=None, op0=ALU.is_equal), reads=gb, writes=gb)
                            S.op("dve", lambda e, dd=dd, m1=m1, m2=m2: e.tensor_tensor(out=dd, in0=m2, in1=m1, op=ALU.subtract), reads=gb, writes=gb)
                            S.op("act", lambda e, dd=dd, ee=ee: e.activation(out=ee, in_=dd, func=AF.Exp), reads=gb, writes=gb)
                            S.op("dve", lambda e, ee=ee, w1=w1: e.tensor_scalar_add(out=w1, in0=ee, scalar1=1.0), reads=gb, writes=gb)
                            S.op("dve", lambda e, w1=w1: e.reciprocal(out=w1, in_=w1), reads=gb, writes=gb)
                            S.op("dve", lambda e, ee=ee, w1=w1, w2=w2: e.tensor_tensor(out=w2, in0=ee, in1=w1, op=ALU.mult), reads=gb, writes=gb)
                            S.op("dve", lambda e, k1=k1, w1=w1: e.tensor_scalar(out=k1, in0=k1, scalar1=w1, scalar2=None, op0=ALU.mult), reads=gb, writes=gb)
                            S.op("dve", lambda e, k1=k1, k2=k2, w2=w2, ti=ti: e.scalar_tensor_tensor(out=gates.h[:, ti, :], in0=k2, scalar=w2, in1=k1, op0=ALU.mult, op1=ALU.add), reads=gb, writes=[gates_b[ti]])
                    for ui, (e_, c0, ncu) in enumerate(units):
                        us = uctr % 2
                        uctr += 1
                        wgs = wg_src(e_).rearrange("(k p) n -> p k n", p=128)
                        wus = wu_src(e_).rearrange("(k p) n -> p k n", p=128)
                        wds = wd_src(e_)[c0 * 128:(c0 + ncu) * 128, :].rearrange("(c p) n -> p c n", p=128)
                        S.op("pool", lambda e, us=us, wgs=wgs, c0=c0, ncu=ncu: e.dma_start(out=WG[us].h[:, :, 0:ncu * 128], in_=wgs[:, :, c0 * 128:(c0 + ncu) * 128]), writes=[WG[us].b], dma_key=WG[us].b)
                        S.op("pool", lambda e, us=us, wus=wus, c0=c0, ncu=ncu: e.dma_start(out=WU[us].h[:, :, 0:ncu * 128], in_=wus[:, :, c0 * 128:(c0 + ncu) * 128]), writes=[WU[us].b], dma_key=WU[us].b)
                        S.op("pool", lambda e, us=us, wds=wds, ncu=ncu: e.dma_start(out=WD[us].h[:, 0:ncu, :], in_=wds), writes=[WD[us].b], dma_key=WD[us].b)
                        hd = hid[us]
                        for c in range(ncu):
                            pg = psGa[c % 2]
                            pu = psUa[c % 2]
                            for kc in range(8):
                                S.op("pe", lambda e, c=c, kc=kc, us=us, gs=gs, pg=pg, N=N: e.matmul(pg.h[:, 0:N], lhsT=WG[us].h[:, kc, c * 128:(c + 1) * 128], rhs=tT[gs].h[:, kc, 0:N], start=(kc == 0), stop=(kc == 7)), reads=[WG[us].b, tT[gs].b], writes=[pg.b])
                            for kc in range(8):
                                S.op("pe", lambda e, c=c, kc=kc, us=us, gs=gs, pu=pu, N=N: e.matmul(pu.h[:, 0:N], lhsT=WU[us].h[:, kc, c * 128:(c + 1) * 128], rhs=tT[gs].h[:, kc, 0:N], start=(kc == 0), stop=(kc == 7)), reads=[WU[us].b, tT[gs].b], writes=[pu.b])
                            sg_ = sg[c % 2]
                            S.op("act", lambda e, pg=pg, sg_=sg_, N=N: e.activation(out=sg_.h[:, 0:N], in_=pg.h[:, 0:N], func=AF.Silu), reads=[pg.b], writes=[sg_.b])
                            S.op("dve", lambda e, c=c, pu=pu, sg_=sg_, hd=hd, N=N: e.tensor_tensor(out=hd.h[:, c, 0:N], in0=pu.h[:, 0:N], in1=sg_.h[:, 0:N], op=ALU.mult), reads=[pu.b, sg_.b], writes=[hd.b])
                        for ti, t in enumerate(grp):
                            for nh in range(2):
                                py = psY[yctr % 2]
                                yctr += 1
                                for c in range(ncu):
                                    S.op("pe", lambda e, c=c, ti=ti, nh=nh, us=us, hd=hd, py=py, ncu=ncu: e.matmul(py.h[:], lhsT=hd.h[:, c, ti * 128:(ti + 1) * 128], rhs=WD[us].h[:, c, nh * 512:(nh + 1) * 512], start=(c == 0), stop=(c == ncu - 1)), reads=[hd.b, WD[us].b], writes=[py.b])
                                ao = acc.h[:, ti, nh * 512:(nh + 1) * 512]
                                eng_ = "dve" if nh == 0 else "pool"
                                if moe:
                                    gsc = gates.h[:, ti, e_:e_ + 1]
                                    if ui == 0:
                                        S.op("dve", lambda e, ao=ao, py=py, gsc=gsc: e.tensor_scalar(out=ao, in0=py.h[:], scalar1=gsc, scalar2=None, op0=ALU.mult), reads=[py.b, gates_b[ti]], writes=[acc_b[ti]])
                                    else:
                                        S.op("dve", lambda e, ao=ao, py=py, gsc=gsc: e.scalar_tensor_tensor(out=ao, in0=py.h[:], scalar=gsc, in1=ao, op0=ALU.mult, op1=ALU.add), reads=[py.b, gates_b[ti], acc_b[ti]], writes=[acc_b[ti]])
                                else:
                                    if ui == 0:
                                        S.op("act", lambda e, ao=ao, py=py: e.copy(out=ao, in_=py.h[:]), reads=[py.b], writes=[acc_b[ti]])
                                    else:
                                        S.op("dve", lambda e, ao=ao, py=py: e.tensor_tensor(out=ao, in0=py.h[:], in1=ao, op=ALU.add), reads=[py.b, acc_b[ti]], writes=[acc_b[ti]])
                    for ti, t in enumerate(grp):
                        gidx = 3 if r == 1 else 1
                        av = acc.h[:, ti, :]
                        S.op("pool", lambda e, av=av, gidx=gidx: e.tensor_tensor(out=av, in0=av, in1=Gt[gidx].h[:], op=ALU.mult), reads=[acc_b[ti], Gt[gidx].b], writes=[acc_b[ti]])
                        if last:
                            dst, dstb = out[(t - NCT) * 128:(t - NCT + 1) * 128, :], out_b[t]
                        else:
                            dst, dstb = xs2[t * 128:(t + 1) * 128, :], xs2_b[t]
                        emit_ln_epilogue(S, T_view(av, acc_b[ti]), T_view(XG[gs].h[:, ti, :], XG_b[gs][ti]), LNt[2], LNt[3], st6, mv, epsln, cc, dst, dstb)
                S.emit()
    return nc


class T_view:
    def __init__(self, ap, b):
        self.ap = ap
        self.b = b


def _ap(x):
    return x.ap if isinstance(x, T_view) else x.h[:]


def emit_ln_epilogue(S, ta, xr, lng, lnb, st6, mv, epsc, cc, dst, dstb):
    a = _ap(ta)
    x = _ap(xr)
    S.op("pool", lambda e: e.tensor_tensor(out=a, in0=x, in1=a, op=ALU.add), reads=[xr.b, ta.b], writes=[ta.b])
    for c in range(2):
        S.op("dve", lambda e, c=c: e.bn_stats(out=st6.h[:, c, :], in_=a[:, c * 512:(c + 1) * 512]), reads=[ta.b], writes=[st6.b])
    S.op("dve", lambda e: e.bn_aggr(out=mv.h[:, 0:2], in_=st6.h[:]), reads=[st6.b], writes=[mv.b])
    S.op("act", lambda e: e.activation(out=mv.h[:, 2:3], in_=mv.h[:, 1:2], func=AF.Ln, bias=epsc, scale=1.0), reads=[mv.b, cc.b], writes=[mv.b])
    S.op("act", lambda e: e.activation(out=mv.h[:, 2:3], in_=mv.h[:, 2:3], func=AF.Exp, scale=-0.5), reads=[mv.b], writes=[mv.b])
    S.op("dve", lambda e: e.scalar_tensor_tensor(out=mv.h[:, 3:4], in0=mv.h[:, 0:1], scalar=-1.0, in1=mv.h[:, 2:3], op0=ALU.mult, op1=ALU.mult), reads=[mv.b], writes=[mv.b])
    S.op("act", lambda e: e.activation(out=a, in_=a, func=AF.Identity, bias=mv.h[:, 3:4], scale=mv.h[:, 2:3]), reads=[ta.b, mv.b], writes=[ta.b])
    S.op("pool", lambda e: e.tensor_tensor(out=a, in0=a, in1=lng.h[:], op=ALU.mult), reads=[ta.b, lng.b], writes=[ta.b])
    S.op("pool", lambda e: e.tensor_tensor(out=a, in0=a, in1=lnb.h[:], op=ALU.add), reads=[ta.b, lnb.b], writes=[ta.b])
    S.op("sp", lambda e: e.dma_start(out=dst, in_=a), reads=[ta.b], writes=[dstb], dma_key=ta.b)


_CACHE = {}


def prep_shared(inp):
    f = lambda a: np.ascontiguousarray(np.asarray(a, dtype=np.float32))
    perm = win_perm()
    sh = {}
    sh["w_mod"] = f(inp["w_mod"])
    bm = f(inp["b_mod"])
    sh["b_modT"] = f(bm.reshape(2, 48, 128).transpose(0, 2, 1))
    sh["b_modr"] = f(bm.reshape(2, 1, 6 * D))
    sh["w_in"] = f(np.asarray(inp["w_in"])[:, :, perm])
    sh["sink"] = f(np.asarray(inp["attn_sink"]).reshape(2, 1, 8))
    sh["pool_w"] = f(inp["pool_w"])
    sh["pool_scale"] = f(np.asarray(inp["pool_scale"]).reshape(2, 1, 256))
    lf = np.asarray(inp["ret_log_decay_fwd"], dtype=np.float32)
    lb = np.asarray(inp["ret_log_decay_bwd"], dtype=np.float32)
    sh["lgp"] = f(np.stack([np.repeat(lf, 32, axis=1), np.repeat(lb, 32, axis=1)], axis=2))
    sh["lgrow"] = f(np.concatenate([np.repeat(lf, 32, axis=1), np.repeat(lb, 32, axis=1)], axis=1).reshape(2, 1, 256))
    sh["lgb"] = f(np.concatenate([lf, lb], axis=1).reshape(2, 1, 8))
    sh["w_out"] = f(inp["w_out"])
    sh["lnp"] = f(np.stack([inp["ln1_g"], inp["ln1_b"], inp["ln2_g"], inp["ln2_b"]], axis=1))
    sh["ffn_wg"] = f(inp["ffn_w_gate"])
    sh["ffn_wu"] = f(inp["ffn_w_up"])
    sh["ffn_wd"] = f(inp["ffn_w_down"])
    sh["router"] = f(np.asarray(inp["moe_router"])[0])
    def unit_layout(w):
        w = np.asarray(w, dtype=np.float32)[0]
        o = np.zeros((NEXP, 4, 128, 8, 768), np.float32)
        wv = w.reshape(NEXP, 8, 128, FF)
        c0 = 0
        for u, ncu in enumerate((6, 6, 5, 5)):
            o[:, u, :, :, 0:ncu * 128] = wv[:, :, :, c0 * 128:(c0 + ncu) * 128].transpose(0, 2, 1, 3)
            c0 += ncu
        return o.reshape(NEXP * 4 * 128, 8 * 768)

    def down_layout(w):
        w = np.asarray(w, dtype=np.float32)[0]
        o = np.zeros((NEXP, 4, 128, 6, D), np.float32)
        wv = w.reshape(NEXP, NCH, 128, D)
        c0 = 0
        for u, ncu in enumerate((6, 6, 5, 5)):
            o[:, u, :, 0:ncu, :] = wv[:, c0:c0 + ncu, :, :].transpose(0, 2, 1, 3)
            c0 += ncu
        return o.reshape(NEXP * 4 * 128, 6 * D)
    sh["moe_wg"] = unit_layout(inp["moe_w_gate"])
    sh["moe_wu"] = unit_layout(inp["moe_w_up"])
    sh["moe_wd"] = down_layout(inp["moe_w_down"])
    sh["consts"] = make_consts()
    sh["ropecs"] = make_rope()
    sh["rconst"] = make_rconst()
    sh["slot_init"] = make_slot_init()
    return sh


def prep_core(inp, b):
    x = np.asarray(inp["x"], dtype=np.float32)
    ctx = np.asarray(inp["ctx"], dtype=np.float32)
    c = np.asarray(inp["c"], dtype=np.float32)
    cc = np.asarray(inp["c_ctx"], dtype=np.float32)
    d = {}
    d["xall"] = np.ascontiguousarray(np.concatenate([ctx[b], x[b]], axis=0))
    d["cvec"] = np.ascontiguousarray(np.concatenate([c[b].reshape(8, 128).T, cc.reshape(8, 128).T], axis=1))
    return d


def kernel(**inputs):
    n = 8
    if "nc" not in _CACHE:
        _CACHE["nc"] = build()
    nc = _CACHE["nc"]
    sh = prep_shared(inputs)
    in_maps = []
    for b in range(n):
        m = dict(sh)
        m.update(prep_core(inputs, b))
        in_maps.append(m)
    res = run_bass_kernel_spmd(nc, in_maps, core_ids=list(range(n)))
    return np.stack([np.asarray(r["out"]) for r in res.results], axis=0).astype(np.float32)
```
